# Optimizing a Trainium2 kernel written in Bass

```python
import math
import jax, jax.numpy as jnp
from jax import lax
import numpy as np

D_MODEL = 1024
BATCH = 4
SEQ = 8192
DEPTH = 1

N_META = 16
CONV_WIDTH = D_MODEL // 2
CONV_K = 3
N_HEADS = 4
HEAD_DIM = 64
V_DIM = 2 * HEAD_DIM
QK_WIDTH = N_HEADS * 2 * HEAD_DIM
ATTN_WIDTH = N_HEADS * V_DIM
MIX_WIDTH = CONV_WIDTH + ATTN_WIDTH
IN_COLS = 3 * CONV_WIDTH + 2 * QK_WIDTH + ATTN_WIDTH
ROPE_DIM = HEAD_DIM // 4
ROPE_THETA = 500000.0
Q_BLOCK = 128
N_GROUPS = 4
EXPERTS_PER_GROUP = 8
N_EXPERTS = N_GROUPS * EXPERTS_PER_GROUP
TOP_K = 2
D_FF_EXPERT = D_MODEL // 2
MOE_BLOCK = 256
EPS = 1e-6

kernel_name = "hymba_conv_diffattn_hiermoe_layer"


def rms_norm(x, g):
    xf = x.astype(jnp.float32)
    y = xf * lax.rsqrt(jnp.mean(xf * xf, axis=-1, keepdims=True) + EPS)
    return y.astype(x.dtype) * g.astype(x.dtype)


def partial_rope(t, cos, sin):
    half = ROPE_DIM // 2
    r1 = t[..., :half]
    r2 = t[..., half:ROPE_DIM]
    rest = t[..., ROPE_DIM:]
    return jnp.concatenate([r1 * cos - r2 * sin, r2 * cos + r1 * sin, rest], axis=-1)


def causal_depthwise_conv(z, w):
    return lax.conv_general_dilated(
        z, w[:, None, :].astype(z.dtype), window_strides=(1,),
        padding=[(CONV_K - 1, 0)], dimension_numbers=("NWC", "WIO", "NWC"),
        feature_group_count=z.shape[-1])


def diff_attention(q, k, v, lam):
    _, b, h, lp, _ = q.shape
    nb = lp // Q_BLOCK
    scale = HEAD_DIM ** -0.5
    qb = q.reshape(2, b, h, nb, Q_BLOCK, HEAD_DIM).transpose(3, 0, 1, 2, 4, 5)
    kpos = jnp.arange(lp)

    def one_block(args):
        qblk, i = args
        qpos = i * Q_BLOCK + jnp.arange(Q_BLOCK)
        mask = qpos[:, None] >= kpos[None, :]
        s = jnp.einsum("nbhqd,nbhkd->nbhqk", qblk, k).astype(jnp.float32) * scale
        p = jax.nn.softmax(jnp.where(mask, s, -jnp.inf), axis=-1)
        w = p[0] - lam * p[1]
        return jnp.einsum("bhqk,bhkd->bhqd", w.astype(v.dtype), v)

    out = lax.map(one_block, (qb, jnp.arange(nb)))
    return out.transpose(1, 2, 0, 3, 4).reshape(b, h, lp, V_DIM)


def hier_moe(t, w_rg, b_rg, w_re, b_re, w_gate, w_up, w_down):
    n = t.shape[0]
    tf = t.astype(jnp.float32)
    g_prob = jax.nn.softmax(tf @ w_rg.astype(jnp.float32) + b_rg.astype(jnp.float32), axis=-1)
    g_val, g_idx = lax.top_k(g_prob, 1)
    e_logits = (tf @ w_re.astype(jnp.float32) + b_re.astype(jnp.float32)).reshape(n, N_GROUPS, EXPERTS_PER_GROUP)
    e_sel = jnp.take_along_axis(e_logits, g_idx[:, :, None], axis=1)[:, 0]
    e_val, e_idx = lax.top_k(jax.nn.softmax(e_sel, axis=-1), TOP_K)
    gates = (g_val * e_val / jnp.sum(e_val, axis=-1, keepdims=True)).reshape(-1).astype(t.dtype)
    expert_ids = (g_idx * EXPERTS_PER_GROUP + e_idx).reshape(-1).astype(jnp.int32)
    token_ids = jnp.repeat(jnp.arange(n, dtype=jnp.int32), TOP_K)

    a = n * TOP_K
    n_blocks = -(-a // MOE_BLOCK) + N_EXPERTS
    p_rows = n_blocks * MOE_BLOCK
    order = jnp.argsort(expert_ids, stable=True)
    sorted_e = expert_ids[order]
    counts = jnp.bincount(expert_ids, length=N_EXPERTS)
    padded = (counts + MOE_BLOCK - 1) // MOE_BLOCK * MOE_BLOCK
    starts = jnp.cumsum(counts) - counts
    pends = jnp.cumsum(padded)
    pstarts = pends - padded
    dest = pstarts[sorted_e] + (jnp.arange(a) - starts[sorted_e])
    buf_tok = jnp.full((p_rows,), n, jnp.int32).at[dest].set(token_ids[order])
    buf_gate = jnp.zeros((p_rows,), t.dtype).at[dest].set(gates[order])
    block_e = jnp.minimum(jnp.searchsorted(pends, jnp.arange(n_blocks) * MOE_BLOCK, side="right"),
                          N_EXPERTS - 1)
    t_pad = jnp.concatenate([t, jnp.zeros((1, t.shape[1]), t.dtype)], axis=0)
    xb = t_pad[buf_tok].reshape(n_blocks, MOE_BLOCK, t.shape[1])

    def expert_block(args):
        xe, e = args
        hid = jax.nn.silu(xe @ w_gate[e]) * (xe @ w_up[e])
        return hid @ w_down[e]

    yb = lax.map(expert_block, (xb, block_e)).reshape(p_rows, t.shape[1])
    out = jnp.zeros((n + 1, t.shape[1]), t.dtype).at[buf_tok].add(yb * buf_gate[:, None])
    return out[:n]


def setup_inputs(seed: int = 0) -> dict:
    key = jax.random.key(seed)
    ks = jax.random.split(key, 24)
    f32 = jnp.float32
    nrm = lambda k, shape, s: jax.random.normal(k, shape, f32) * s
    return {
        "x": nrm(ks[0], (BATCH, SEQ, D_MODEL), 1.0),
        "meta_tokens": nrm(ks[1], (N_META, D_MODEL), 1.0),
        "norm1_g": 1.0 + nrm(ks[2], (DEPTH, D_MODEL), 0.02),
        "w_in": nrm(ks[3], (DEPTH, D_MODEL, IN_COLS), D_MODEL ** -0.5),
        "conv_w": nrm(ks[4], (DEPTH, CONV_K, CONV_WIDTH), CONV_K ** -0.5),
        "q_norm_g": 1.0 + nrm(ks[5], (DEPTH, HEAD_DIM), 0.02),
        "k_norm_g": 1.0 + nrm(ks[6], (DEPTH, HEAD_DIM), 0.02),
        "lambda_q1": nrm(ks[7], (DEPTH, HEAD_DIM), 0.1),
        "lambda_k1": nrm(ks[8], (DEPTH, HEAD_DIM), 0.1),
        "lambda_q2": nrm(ks[9], (DEPTH, HEAD_DIM), 0.1),
        "lambda_k2": nrm(ks[10], (DEPTH, HEAD_DIM), 0.1),
        "subln_g": 1.0 + nrm(ks[11], (DEPTH, V_DIM), 0.02),
        "w_out": nrm(ks[12], (DEPTH, MIX_WIDTH, D_MODEL), MIX_WIDTH ** -0.5),
        "norm2_g": 1.0 + nrm(ks[13], (DEPTH, D_MODEL), 0.02),
        "w_router_group": nrm(ks[14], (DEPTH, D_MODEL, N_GROUPS), D_MODEL ** -0.5),
        "b_router_group": nrm(ks[15], (DEPTH, N_GROUPS), 0.01),
        "w_router_expert": nrm(ks[16], (DEPTH, D_MODEL, N_EXPERTS), D_MODEL ** -0.5),
        "b_router_expert": nrm(ks[17], (DEPTH, N_EXPERTS), 0.01),
        "w_gate": nrm(ks[18], (DEPTH, N_EXPERTS, D_MODEL, D_FF_EXPERT), D_MODEL ** -0.5),
        "w_up": nrm(ks[19], (DEPTH, N_EXPERTS, D_MODEL, D_FF_EXPERT), D_MODEL ** -0.5),
        "w_down": nrm(ks[20], (DEPTH, N_EXPERTS, D_FF_EXPERT, D_MODEL), D_FF_EXPERT ** -0.5),
    }


def reference(x, meta_tokens, norm1_g, w_in, conv_w, q_norm_g, k_norm_g, lambda_q1, lambda_k1,
              lambda_q2, lambda_k2, subln_g, w_out, norm2_g, w_router_group, b_router_group,
              w_router_expert, b_router_expert, w_gate, w_up, w_down):
    b, s, d = x.shape
    h = jnp.concatenate([jnp.broadcast_to(meta_tokens[None].astype(x.dtype), (b, N_META, d)), x], axis=1)
    length = s + N_META
    lp = -(-length // Q_BLOCK) * Q_BLOCK

    pos = jnp.arange(length, dtype=jnp.float32)
    inv_freq = ROPE_THETA ** (-jnp.arange(0, ROPE_DIM, 2, dtype=jnp.float32) / ROPE_DIM)
    ang = pos[:, None] * inv_freq[None, :]
    cos = jnp.cos(ang).astype(x.dtype)
    sin = jnp.sin(ang).astype(x.dtype)

    for l in range(DEPTH):
        lam_init = 0.8 - 0.6 * math.exp(-0.3 * l)
        xn = rms_norm(h, norm1_g[l])
        u = xn @ w_in[l]
        c_b, c_c, c_x, q, k, v = jnp.split(
            u, [CONV_WIDTH, 2 * CONV_WIDTH, 3 * CONV_WIDTH, 3 * CONV_WIDTH + QK_WIDTH,
                3 * CONV_WIDTH + 2 * QK_WIDTH], axis=-1)
        conv_y = c_b * causal_depthwise_conv(c_c * c_x, conv_w[l])

        q = q.reshape(b, length, N_HEADS, 2, HEAD_DIM).transpose(3, 0, 2, 1, 4)
        k = k.reshape(b, length, N_HEADS, 2, HEAD_DIM).transpose(3, 0, 2, 1, 4)
        q = partial_rope(rms_norm(q, q_norm_g[l]), cos, sin)
        k = partial_rope(rms_norm(k, k_norm_g[l]), cos, sin)
        v = v.reshape(b, length, N_HEADS, V_DIM).transpose(0, 2, 1, 3)
        pad = lp - length
        q = jnp.pad(q, ((0, 0), (0, 0), (0, 0), (0, pad), (0, 0)))
        k = jnp.pad(k, ((0, 0), (0, 0), (0, 0), (0, pad), (0, 0)))
        v = jnp.pad(v, ((0, 0), (0, 0), (0, pad), (0, 0)))
        lam = (jnp.exp(jnp.sum(lambda_q1[l].astype(jnp.float32) * lambda_k1[l].astype(jnp.float32)))
               - jnp.exp(jnp.sum(lambda_q2[l].astype(jnp.float32) * lambda_k2[l].astype(jnp.float32)))
               + lam_init)
        o = diff_attention(q, k, v, lam)[:, :, :length]
        o = rms_norm(o, subln_g[l]) * (1.0 - lam_init)
        o = o.transpose(0, 2, 1, 3).reshape(b, length, ATTN_WIDTH)
        h = h + jnp.concatenate([conv_y, o], axis=-1) @ w_out[l]

        xn2 = rms_norm(h, norm2_g[l]).reshape(b * length, d)
        h = h + hier_moe(xn2, w_router_group[l], b_router_group[l], w_router_expert[l],
                         b_router_expert[l], w_gate[l], w_up[l], w_down[l]).reshape(b, length, d)

    return h[:, N_META:]
```

```python
import os
from contextlib import ExitStack
import numpy as np
import ml_dtypes
import concourse.bass as bass
import concourse.mybir as mybir
from concourse.bass_utils import run_bass_kernel_spmd

F32 = mybir.dt.float32
BF16 = mybir.dt.bfloat16
I32 = mybir.dt.int32
ALU = mybir.AluOpType
AF = mybir.ActivationFunctionType
AX = mybir.AxisListType

NCORES = 8
D = 1024
SEQ = 8192
NMETA = 16
NT_ALL = 65
NG = 8
GT = 512
NOWN = NG * GT
NOT_ = NOWN // 128
EPS = 1e-6
NE = 32
NBLK = 64
BLK = 256
NSLOT = NBLK * BLK
GROUPS = [[0, 3, 4, 7, 8, 11, 12, 15], [1, 2, 5, 6, 9, 10, 13, 14]]

STOP_AFTER = os.environ.get("HK_STOP", "")
DEBUG = os.environ.get("HK_DEBUG", "") == "1"
STATIC_E = os.environ.get("HK_STATIC_E", "") == "1"


class Prog:
    ENGS = ["sync", "scalar", "vector", "gpsimd", "tensor"]

    def __init__(self, nc, es):
        self.nc, self.es = nc, es
        self.ops = {e: [] for e in self.ENGS}
        self.sems, self.cnt, self.owner = {}, {}, {}
        for e in self.ENGS[1:]:
            self.newsem("E_" + e, e)
        self.last = {e: None for e in self.ENGS}
        self.pending = {e: [] for e in self.ENGS}

    def barrier(self):
        toks = [self.last[e] for e in self.ENGS[1:] if self.last[e] is not None]
        toks += [(k, v) for k, v in self.cnt.items() if self.owner.get(k) is None and v > 0]
        for e in self.ENGS:
            self.pending[e] = list(toks)

    def _w(self, eng, waits):
        w = self.pending[eng] + flat(list(waits))
        self.pending[eng] = []
        return w

    def newsem(self, name, owner=None):
        self.sems[name] = self.es.enter_context(self.nc.semaphore(name))
        self.cnt[name] = 0
        self.owner[name] = owner
        return name

    SERIAL = ("scalar", "vector", "gpsimd")

    def op(self, eng, fn, waits=(), sig=True, noself=False):
        tok = None
        name = None
        w = self._w(eng, waits)
        if eng in self.SERIAL:
            sig = True
            if self.cnt["E_" + eng] > 0 and not noself:
                w = w + [("E_" + eng, self.cnt["E_" + eng], "self")]
        if sig:
            name = "E_" + eng
            self.cnt[name] += 1
            tok = (name, self.cnt[name])
        self.ops[eng].append((freeze(fn), w, name, 1))
        if tok is not None:
            self.last[eng] = tok
        return tok

    def dma(self, eng, fn, sem, waits=()):
        self.cnt[sem] += 16
        tok = (sem, self.cnt[sem])
        self.ops[eng].append((freeze(fn), self._w(eng, waits), sem, 16))
        return tok

    def replay(self, eng, e):
        waited = {}
        for fn, waits, sig, inc in self.ops[eng]:
            for (k, v) in waits:
                if self.owner.get(k) == eng:
                    continue
                if waited.get(k, 0) >= v:
                    continue
                e.wait_ge(self.sems[k], v)
                waited[k] = v
            ins = fn(e)
            if sig is not None:
                ins.then_inc(self.sems[sig], inc)


import types


def freeze(fn):
    if fn is None or fn.__closure__ is None:
        return fn
    cells = []
    for c in fn.__closure__:
        try:
            cells.append(types.CellType(c.cell_contents))
        except ValueError:
            cells.append(c)
    return types.FunctionType(fn.__code__, fn.__globals__, fn.__name__, fn.__defaults__, tuple(cells))


def flat(toks):
    out = []
    for t in toks:
        if t is None:
            continue
        if isinstance(t, list):
            out.extend(flat(t))
        else:
            out.append(t)
    return out


def build():
    nc = bass.Bass("TRN2", target_bir_lowering=False)
    dt_in = lambda n, s, d=F32: nc.dram_tensor(n, s, d, kind="ExternalInput").ap()
    xall = dt_in("xall", [NT_ALL * 128, D])
    xown = dt_in("xown", [NOWN, D])
    xhalo = dt_in("xhalo", [NG * 128, D])
    cosA = dt_in("cosA", [128, NT_ALL * 8]); sinA = dt_in("sinA", [128, NT_ALL * 8])
    cosO = dt_in("cosO", [128, NOT_ * 8]); sinO = dt_in("sinO", [128, NOT_ * 8])
    masks_d = dt_in("masks", [128, 16 * 512], BF16)
    thr_d = dt_in("thr", [1, NBLK]); iota_d = dt_in("iota", [1, 32]); pidx_d = dt_in("pidx", [128, 12])
    norm1_g = dt_in("norm1_g", [1, D]); norm2_g = dt_in("norm2_g", [1, D])
    w_in = dt_in("w_in", [D, 3072]); w_out = dt_in("w_out", [D, D])
    conv_wT = dt_in("conv_wT", [512, 3])
    q_g = dt_in("q_norm_g", [1, 64]); k_g = dt_in("k_norm_g", [1, 64])
    lq1 = dt_in("lambda_q1", [1, 64]); lk1 = dt_in("lambda_k1", [1, 64])
    lq2 = dt_in("lambda_q2", [1, 64]); lk2 = dt_in("lambda_k2", [1, 64])
    subln_g = dt_in("subln_g", [1, 128])
    w_r = dt_in("w_r", [D, 36]); b_r = dt_in("b_r", [1, 36])
    w_gate = dt_in("w_gate", [NE, D, 512]); w_up = dt_in("w_up", [NE, D, 512]); w_down = dt_in("w_down", [NE, 512, D])
    out_d = nc.dram_tensor("out", [NOWN, D], F32, kind="ExternalOutput").ap()
    scr_kind = "ExternalOutput" if DEBUG else "Internal"
    h1buf = nc.dram_tensor("h1buf", [NOWN, D], F32, kind=scr_kind).ap()
    xn2buf = nc.dram_tensor("xn2buf", [NOWN, D], BF16, kind="Internal").ap()
    otbuf = nc.dram_tensor("otbuf", [128, 4 * NOWN], BF16, kind=scr_kind).ap()
    cacheA = nc.dram_tensor("cacheA", [NT_ALL, 128, D], BF16, kind="Internal").ap()
    cacheO = nc.dram_tensor("cacheO", [NOT_, 128, D], BF16, kind="Internal").ap()
    xebuf = nc.dram_tensor("xebuf", [NSLOT, D], BF16, kind="Internal").ap()
    ybuf = nc.dram_tensor("ybuf", [NSLOT, D], F32, kind="Internal").ap()
    if DEBUG:
        dbg_qt = nc.dram_tensor("dbg_qt", [128, 2 * NOWN], BF16, kind="ExternalOutput").ap()
        dbg_kt = nc.dram_tensor("dbg_kt", [128, 2 * NT_ALL * 128], BF16, kind="ExternalOutput").ap()
        dbg_v = nc.dram_tensor("dbg_v", [128, NT_ALL * 2 * 130], BF16, kind="ExternalOutput").ap()
        dbg_rt = nc.dram_tensor("dbg_rt", [128, 1024], F32, kind="ExternalOutput").ap()

    with ExitStack() as es:
        def sb(name, shape, dt):
            return es.enter_context(nc.sbuf_tensor("s_" + name, shape, dt))
        P = Prog(nc, es)
        for i in range(48):
            P.newsem(f"D{i}")
        ident = sb("ident", [128, 128], BF16)
        Ubf = sb("Ubf", [128, 128], BF16)
        ones_bf = sb("ones_bf", [128, 128], BF16)
        g1_t = sb("g1_t", [128, D], F32)
        gq_t = sb("gq_t", [128, 64], F32)
        gk_t = sb("gk_t", [128, 64], F32)
        gsub_t = sb("gsub_t", [128, 128], F32)
        lam_w = sb("lam_w", [128, 8], F32)
        nlam = sb("nlam", [128, 1], F32)
        EPS_T = sb("EPS_T", [128, 1], F32)
        masks = sb("masks", [128, 16, 512], BF16)
        cosA_t = sb("cosA_t", [128, NT_ALL, 8], F32); sinA_t = sb("sinA_t", [128, NT_ALL, 8], F32)
        cosO_t = sb("cosO_t", [128, NOT_, 8], F32); sinO_t = sb("sinO_t", [128, NOT_, 8], F32)
        Wr = sb("Wr", [128, 8, 36], BF16)
        br_t = sb("br_t", [128, 36], F32)
        convw = sb("convw", [128, 4, 3], F32)
        thr_t = sb("thr_t", [128, NBLK], F32)
        iota_t = sb("iota_t", [128, 32], F32)
        QT = sb("QT", [128, 2, NOWN], BF16)
        eid_all = sb("eid_all", [128, NOT_, 2], F32)
        rank_all = sb("rank_all", [128, NOT_, 2], F32)
        gate_all = sb("gate_all", [128, NOT_, 2], F32)
        slot_f = sb("slot_f", [128, NOT_, 2], F32)
        slot_i = sb("slot_i", [128, NOT_, 2], I32)
        base = sb("base", [128, 32], F32)
        pst = sb("pst", [128, 4, 32], F32)
        pst_i = sb("pst_i", [128, 32], I32)
        Ej_f = sb("Ej_f", [128, NBLK], F32)
        Ej_i = sb("Ej_i", [128, NBLK], I32)
        pidx_t = sb("pidx_t", [128, 12], F32)
        idxW_i = sb("idxW_i", [128, NBLK, 12], I32)
        regs = {}
        def breg(e, bound):
            key = (id(e), bound)
            if key not in regs:
                regs[key] = e.to_reg(bound)
            return regs[key]
        ARENA_B = 118 * 1024
        arena = sb("arena", [128, ARENA_B // 2], BF16)

        class Carver:
            def __init__(self):
                self.off = 0
            def reset(self):
                self.off = 0
            def get(self, shape, dt):
                n = int(np.prod(shape[1:]))
                nb = n * (4 if dt in (F32, I32) else 2)
                nb_al = (nb + 63) // 64 * 64
                a = arena[:, self.off // 2:(self.off + nb) // 2]
                self.off += nb_al
                assert self.off <= ARENA_B, (self.off, ARENA_B)
                if dt != BF16:
                    a = a.bitcast(dt)
                if len(shape) == 3:
                    a = a.rearrange("p (a b) -> p a b", b=shape[2])
                elif len(shape) == 4:
                    a = a.rearrange("p (a b c) -> p a b c", b=shape[2], c=shape[3])
                return a
        cv = Carver()

        psum = es.enter_context(nc.psum_tensor("psum", [128, 4096], F32))
        def pbank(b, nb=1):
            return psum[:, b * 512:(b + nb) * 512]
        def pbank_bf(b, nb=1):
            return psum[:, b * 512:(b + nb) * 512].bitcast(BF16)

        w_in_v = w_in.rearrange("(c p) n -> p c n", p=128)
        w_out_v = w_out.rearrange("(c p) n -> p c n", p=128)
        out_toks = []

        cv.reset()
        ident_f = cv.get([128, 128], F32)
        U_f = cv.get([128, 128], F32)
        lam_in = cv.get([128, 4, 64], F32)
        ztile = cv.get([128, 1024], BF16)
        P.op("gpsimd", lambda e: e.memset(ident_f, 0.0), sig=False)
        P.op("gpsimd", lambda e: e.affine_select(out=ident_f, in_=ident_f, pattern=[[-1, 128]], compare_op=ALU.not_equal,
                                                  fill=1.0, base=0, channel_multiplier=1), sig=False)
        P.op("gpsimd", lambda e: e.memset(U_f, 1.0), sig=False)
        P.op("gpsimd", lambda e: e.affine_select(out=U_f, in_=U_f, pattern=[[1, 128]], compare_op=ALU.is_gt,
                                                  fill=0.0, base=0, channel_multiplier=-1), sig=False)
        P.op("gpsimd", lambda e: e.tensor_copy(out=ident[:], in_=ident_f), sig=False)
        P.op("gpsimd", lambda e: e.tensor_copy(out=Ubf[:], in_=U_f), sig=False)
        P.op("gpsimd", lambda e: e.memset(base[:], 0.0), sig=False)
        P.op("gpsimd", lambda e: e.memset(EPS_T[:], EPS), sig=False)
        P.op("gpsimd", lambda e: e.memset(ones_bf[:], 1.0), sig=False)
        tkz = P.op("gpsimd", lambda e: e.memset(ztile, 0.0))
        def cdma(out, in_):
            P.dma("sync", lambda e, o=out, i=in_: e.dma_start(out=o, in_=i), "D0")
        cdma(g1_t[:], norm1_g.partition_broadcast(128))
        cdma(gq_t[:], q_g.partition_broadcast(128))
        cdma(gk_t[:], k_g.partition_broadcast(128))
        cdma(gsub_t[:], subln_g.partition_broadcast(128))
        cdma(lam_in[:, 0, :], lq1.partition_broadcast(128))
        cdma(lam_in[:, 1, :], lk1.partition_broadcast(128))
        cdma(lam_in[:, 2, :], lq2.partition_broadcast(128))
        cdma(lam_in[:, 3, :], lk2.partition_broadcast(128))
        cdma(masks[:], masks_d.rearrange("p (a b) -> p a b", b=512))
        cdma(cosA_t[:], cosA.rearrange("p (a b) -> p a b", b=8))
        cdma(sinA_t[:], sinA.rearrange("p (a b) -> p a b", b=8))
        cdma(cosO_t[:], cosO.rearrange("p (a b) -> p a b", b=8))
        cdma(sinO_t[:], sinO.rearrange("p (a b) -> p a b", b=8))
        cdma(br_t[:], b_r.partition_broadcast(128))
        cdma(convw[:], conv_wT.rearrange("(c p) k -> p c k", p=128))
        cdma(thr_t[:], thr_d.partition_broadcast(128))
        cdma(iota_t[:], iota_d.partition_broadcast(128))
        cdma(pidx_t[:], pidx_d)
        tk_cd = ("D0", P.cnt["D0"])
        P.dma("gpsimd", lambda e: e.dma_start(out=Wr[:], in_=w_r.rearrange("(c p) n -> p c n", p=128)), "D1")
        P.op("vector", lambda e: e.tensor_tensor(out=lam_in[:, 0, :], in0=lam_in[:, 0, :], in1=lam_in[:, 1, :], op=ALU.mult), waits=[tk_cd], sig=False)
        P.op("vector", lambda e: e.tensor_tensor(out=lam_in[:, 2, :], in0=lam_in[:, 2, :], in1=lam_in[:, 3, :], op=ALU.mult), sig=False)
        P.op("vector", lambda e: e.reduce_sum(out=lam_w[:, 0:1], in_=lam_in[:, 0, :], axis=AX.X), sig=False)
        tk = P.op("vector", lambda e: e.reduce_sum(out=lam_w[:, 1:2], in_=lam_in[:, 2, :], axis=AX.X))
        tk = P.op("scalar", lambda e: e.activation(out=lam_w[:, 2:4], in_=lam_w[:, 0:2], func=AF.Exp), waits=[tk])
        P.op("vector", lambda e: e.tensor_tensor(out=lam_w[:, 4:5], in0=lam_w[:, 3:4], in1=lam_w[:, 2:3], op=ALU.subtract), waits=[tk], sig=False)
        P.op("vector", lambda e: e.tensor_scalar(out=nlam[:], in0=lam_w[:, 4:5], scalar1=-0.2, scalar2=None, op0=ALU.add), sig=False)
        P.op("vector", lambda e: e.tensor_scalar(out=gsub_t[:], in0=gsub_t[:], scalar1=0.8, scalar2=None, op0=ALU.mult))
        xe_v = xebuf.rearrange("(p r) d -> p r d", p=128)
        for q in range(8):
            P.dma("gpsimd", lambda e, q=q: e.dma_start(out=xe_v[:, q * 16:(q + 1) * 16, :],
                                                        in_=ztile.unsqueeze(1).broadcast_to([128, 16, 1024])), "D2", waits=[tkz])
        P.barrier()

        def carve_front():
            cv.reset()
            B = {}
            B["xt"] = [cv.get([128, D], F32) for _ in range(2)]
            B["junk"] = cv.get([128, D], BF16)
            B["xn"] = [cv.get([128, D], BF16) for _ in range(2)]
            B["xnT"] = [cv.get([128, 8, 128], BF16) for _ in range(2)]
            B["st"] = [cv.get([128, 16], F32) for _ in range(2)]
            B["sq2"] = [cv.get([128, 256], F32) for _ in range(2)]
            B["stq"] = [cv.get([128, 16], F32) for _ in range(2)]
            B["t"] = cv.get([128, 512], F32)
            B["rp"] = cv.get([128, 4, 8, 8], F32)
            B["qb"] = [cv.get([128, 512], BF16) for _ in range(2)]
            B["W"] = cv.get([128, 8, 512], BF16)
            return B

        state = {"n": 0, "save_tok": [None, None]}
        def reset_state():
            for k in ("xt_free", "xn_free", "ptr_free"):
                state[k] = [None, None]
            state["sq2_free"] = [None, None]; state["stq_free"] = [None, None]

        def x_front(B, src_rows, dst, ncol, dst_free, xt=None, xt_free=None, xt_sem=None, defer_copy=False, save_to=None, load_from=None, load_sem=None):
            n = state["n"]; state["n"] += 1
            b = n % 2
            if load_from is not None:
                sem = load_sem or f"D{38 + b}"
                tl = P.dma("sync", lambda e: e.dma_start(out=dst, in_=load_from.rearrange("p (c k) -> p c k", k=128)[:, :, 0:ncol]), sem, waits=[dst_free])
                if defer_copy:
                    return (lambda: tl), None, b
                return tl, None, b
            if xt is None:
                xt = B["xt"][b]; xt_free = state["xt_free"][b]; xt_sem = ["D4", "D5"][b]
            xn, st = B["xn"][b], B["st"][b]
            tl = P.dma("sync", lambda e: e.dma_start(out=xt, in_=src_rows), xt_sem, waits=[xt_free])
            P.op("scalar", lambda e: e.activation(out=B["junk"], in_=xt, func=AF.Square, accum_out=st[:, 0:1]), waits=[tl], sig=False)
            t2 = P.op("scalar", lambda e: e.activation(out=st[:, 1:2], in_=st[:, 0:1], func=AF.Sqrt, scale=1.0 / D, bias=EPS_T[:, 0:1]))
            P.op("vector", lambda e: e.reciprocal(out=st[:, 2:3], in_=st[:, 1:2]), waits=[t2], sig=False)
            t3 = P.op("vector", lambda e: e.scalar_tensor_tensor(out=xn, in0=xt, scalar=st[:, 2:3], in1=g1_t[:], op0=ALU.mult, op1=ALU.mult),
                      waits=[state["xn_free"][b]])
            state["xt_free"][b] = t3
            ptr = pbank_bf(b).rearrange("p (a b) -> p a b", b=128)
            tt = None
            for c in range(8):
                tt = P.op("tensor", lambda e, c=c: e.transpose(out=ptr[:, c, :], in_=xn[:, c * 128:(c + 1) * 128], identity=ident[:]),
                          waits=[t3, state["ptr_free"][b]] if c == 0 else [], sig=(c == 7))
            state["xn_free"][b] = tt
            def do_copy():
                t4 = P.op("scalar", lambda e: e.copy(out=dst, in_=ptr[:, :, 0:ncol]), waits=[tt, dst_free, state["save_tok"][b]])
                state["ptr_free"][b] = t4
                if save_to is not None:
                    state["save_tok"][b] = P.dma("sync", lambda e: e.dma_start(out=save_to.rearrange("p (c k) -> p c k", k=128), in_=dst), f"D{40 + b}", waits=[t4])
                return t4
            if defer_copy:
                return do_copy, t3, b
            return do_copy(), t3, b

        def qk_stats_a(B, pin, ncol, waits, par):
            sq = B["sq2"][par]
            return P.op("scalar", lambda e: e.activation(out=sq[:, 0:ncol], in_=pin, func=AF.Square), waits=list(waits) + [state["sq2_free"][par]])

        def qk_stats_b(B, ncol, ta, par):
            nh = ncol // 64
            sq = B["sq2"][par]; stq = B["stq"][par]
            tb = P.op("vector", lambda e: e.reduce_sum(out=stq[:, 0:nh], in_=sq[:, 0:ncol].rearrange("p (a b) -> p a b", b=64), axis=AX.X), waits=[ta, state["stq_free"][par]])
            state["sq2_free"][par] = tb
            tc_ = P.op("scalar", lambda e: e.activation(out=stq[:, 0:nh], in_=stq[:, 0:nh], func=AF.Sqrt, scale=1.0 / 64, bias=EPS_T[:, 0:1]), waits=[tb])
            return P.op("vector", lambda e: e.reciprocal(out=stq[:, 0:nh], in_=stq[:, 0:nh]), waits=[tc_])

        def qk_apply(B, pin, ncol, g_t, cos_t, sin_t, outb, tr, out_free, par):
            nh = ncol // 64
            t, rp = B["t"], B["rp"]
            stq = B["stq"][par]
            t3v = t[:, 0:ncol].rearrange("p (a b) -> p a b", b=64)
            P.op("vector", lambda e: e.tensor_tensor(out=t3v, in0=pin.rearrange("p (a b) -> p a b", b=64),
                                                     in1=stq[:, 0:nh].unsqueeze(2).broadcast_to([128, nh, 64]), op=ALU.mult), waits=[tr], sig=False)
            state["stq_free"][par] = P.op("vector", lambda e: e.tensor_tensor(out=t3v, in0=t3v, in1=g_t[:].unsqueeze(1).broadcast_to([128, nh, 64]), op=ALU.mult), sig=False)
            ob3 = outb.rearrange("p (a b) -> p a b", b=64)
            P.op("vector", lambda e: e.tensor_copy(out=ob3[:, :, 16:64], in_=t3v[:, :, 16:64]), waits=[out_free], sig=False)
            cosb = cos_t.unsqueeze(1).broadcast_to([128, nh, 8]); sinb = sin_t.unsqueeze(1).broadcast_to([128, nh, 8])
            r1 = t3v[:, :, 0:8]; r2 = t3v[:, :, 8:16]
            P.op("vector", lambda e: e.tensor_tensor(out=rp[:, 0, 0:nh, :], in0=r1, in1=cosb, op=ALU.mult), sig=False)
            P.op("vector", lambda e: e.tensor_tensor(out=rp[:, 1, 0:nh, :], in0=r2, in1=sinb, op=ALU.mult), sig=False)
            P.op("vector", lambda e: e.tensor_tensor(out=rp[:, 2, 0:nh, :], in0=r2, in1=cosb, op=ALU.mult), sig=False)
            P.op("vector", lambda e: e.tensor_tensor(out=rp[:, 3, 0:nh, :], in0=r1, in1=sinb, op=ALU.mult), sig=False)
            P.op("vector", lambda e: e.tensor_tensor(out=ob3[:, :, 0:8], in0=rp[:, 0, 0:nh, :], in1=rp[:, 1, 0:nh, :], op=ALU.subtract), sig=False)
            return P.op("vector", lambda e: e.tensor_tensor(out=ob3[:, :, 8:16], in0=rp[:, 2, 0:nh, :], in1=rp[:, 3, 0:nh, :], op=ALU.add))

        n_pass = 2
        for p in range(n_pass):
            h0 = 2 * p
            B = carve_front()
            KT = cv.get([128, 2, NT_ALL * 128], BF16)
            Vs = cv.get([128, NT_ALL, 2, 130], BF16)
            Ebuf = [cv.get([128, 1024], BF16) for _ in range(2)]
            ev_t1 = cv.get([128, 4, 128], F32)
            ev_o = cv.get([128, 4, 128], F32)
            ev_sq = cv.get([128, 128], F32)
            ev_st = cv.get([128, 16], F32)
            ev_ob = cv.get([128, 4, 128], BF16)
            ot_st = [cv.get([128, GT], BF16) for _ in range(2)]
            reset_state()
            tk_w = P.dma("gpsimd", lambda e: e.dma_start(out=B["W"][:, :, 0:256], in_=w_in_v[:, :, 1536 + h0 * 128:1536 + h0 * 128 + 256]), "D3")
            tk_ones = P.op("gpsimd", lambda e: e.memset(Vs[:, :, :, 128:129], 1.0))
            def proj_phase(T, src, ncw, g_t, cos_t, sin_t, dst, is_kv, cache):
                pf = {"proj": [None, None], "tr": None, "qb": [None, None], "xnT": [None, None]}
                info = {}
                def stageF(t):
                    b = state["n"] % 2
                    xnT = B["xnT"][b]
                    cp, _, b = x_front(B, src[t * 128:(t + 1) * 128, :], xnT, 128, pf["xnT"][b], defer_copy=True,
                                       save_to=(cache[t] if p == 0 else None), load_from=(cache[t] if p == 1 else None))
                    info[t] = {"b": b, "xnT": xnT, "cp": cp}
                def stageF2(t):
                    info[t]["t4"] = info[t]["cp"]()
                def stagePa(t):
                    d = info[t]; b = d["b"]; xnT = d["xnT"]
                    pk = pbank(2 + b)
                    tm = None
                    for c in range(8):
                        tm = P.op("tensor", lambda e, c=c, pk=pk, xnT=xnT: e.matmul(pk[:, 0:ncw], lhsT=xnT[:, c, :], rhs=B["W"][:, c, 0:ncw], start=(c == 0), stop=(c == 7)),
                                  waits=[d["t4"], tk_w, pf["proj"][b]] if c == 0 else [], sig=(c == 7))
                    pf["xnT"][b] = tm
                    d["tm"] = tm; d["pk"] = pk
                    d["tv"] = None
                def stagePb(t):
                    d = info[t]; pk = d["pk"]
                    if is_kv:
                        d["tv"] = P.op("scalar", lambda e, t=t, pk=pk: e.copy(out=Vs[:, t, :, 0:128], in_=pk[:, 256:512].rearrange("p (a b) -> p a b", b=128)), waits=[d["tm"]])
                def stageN1a(t):
                    d = info[t]; b = d["b"]
                    d["ta"] = qk_stats_a(B, d["pk"][:, 0:256], 256, [d["tm"], d["tv"]], b)
                def stageN1b(t):
                    d = info[t]; b = d["b"]
                    d["tr"] = qk_stats_b(B, 256, d["ta"], b)
                def stageN2(t):
                    d = info.pop(t); b = d["b"]; pk = d["pk"]
                    kb = B["qb"][b][:, 0:256]
                    tq = qk_apply(B, pk[:, 0:256], 256, g_t, cos_t[:, t, :], sin_t[:, t, :], kb, d["tr"], pf["qb"][b], b)
                    pf["proj"][b] = [tq, d["tv"]]
                    pkt = pbank_bf(4).rearrange("p (a b) -> p a b", b=128)[:, 0:2, :]
                    tt = None
                    for hl in range(2):
                        tt = P.op("tensor", lambda e, hl=hl, kb=kb: e.transpose(out=pkt[:, hl, :], in_=kb[:, hl * 128:(hl + 1) * 128], identity=ident[:]),
                                  waits=[tq, pf["tr"]] if hl == 0 else [], sig=(hl == 1))
                    pf["qb"][b] = tt
                    pf["tr"] = P.op("vector", lambda e, t=t: e.tensor_copy(out=dst[:, :, t * 128:(t + 1) * 128], in_=pkt), waits=[tt])
                for k in range(T + 2):
                    if 0 <= k - 1 < T:
                        stagePa(k - 1)
                    if k < T:
                        stageF(k)
                    if 0 <= k - 1 < T:
                        stagePb(k - 1)
                        stageN1a(k - 1)
                    if 0 <= k - 2 < T:
                        stageN2(k - 2)
                    if 0 <= k - 1 < T:
                        stageN1b(k - 1)
                    if k < T:
                        stageF2(k)
                return pf["tr"]
            qtr_free = proj_phase(NOT_, xown, 256, gq_t, cosO_t, sinO_t, QT, False, cacheO)
            if DEBUG and p == 0:
                out_toks.append(P.dma("sync", lambda e: e.dma_start(out=dbg_qt, in_=QT[:].rearrange("p a b -> p (a b)")), "D6", waits=[qtr_free]))
            P.barrier()
            if STOP_AFTER == "O":
                break
            P.dma("gpsimd", lambda e: e.dma_start(out=B["W"][:, :, 0:256], in_=w_in_v[:, :, 2048 + h0 * 128:2048 + h0 * 128 + 256]), "D3")
            tk_w = P.dma("gpsimd", lambda e: e.dma_start(out=B["W"][:, :, 256:512], in_=w_in_v[:, :, 2560 + h0 * 128:2560 + h0 * 128 + 256]), "D3")
            reset_state()
            proj_phase(int(os.environ.get("HK_NTA", NT_ALL)), xall, 512, gk_t, cosA_t, sinA_t, KT, True, cacheA)
            if DEBUG and p == 0:
                P.barrier()
                out_toks.append(P.dma("sync", lambda e: e.dma_start(out=dbg_kt, in_=KT.rearrange("p a b -> p (a b)")), "D6"))
                out_toks.append(P.dma("sync", lambda e: e.dma_start(out=dbg_v, in_=Vs.rearrange("p a b c -> p (a b c)")), "D6"))
            P.barrier()
            if STOP_AFTER == "A":
                break
            accs = []
            for a in range(8):
                bk, r = divmod(a, 3)
                accs.append(psum[:, (4 + bk) * 512 + r * 132:(4 + bk) * 512 + r * 132 + 129])
            AST = {"S_free": [None, None], "E_free": [None, None], "acc_free": None, "otr_free": None, "n_ot": 0,
                   "ot_st_free": [None, None]}
            def emit_S_exp(i, hl, u, un):
                nkb = 8 * i + 8
                si = un % 2
                nk = 16 if u == 0 else 128
                ps = pbank(2 * si, 2)
                ts = None
                for m in range(2):
                    ts = P.op("tensor", lambda e, m=m, ps=ps, u=u, nk=nk, hl=hl, i=i: e.matmul(
                        ps[0:nk, m * 512:(m + 1) * 512], lhsT=KT[m * 64:(m + 1) * 64, hl, u * 128:u * 128 + nk],
                        rhs=QT[m * 64:(m + 1) * 64, hl, i * GT:(i + 1) * GT], start=True, stop=True),
                        waits=[AST["S_free"][si]] if m == 0 else [], sig=(m == 1))
                Eb = Ebuf[si]
                te = P.op("scalar", lambda e, ps=ps, Eb=Eb, nk=nk: e.activation(out=Eb[0:nk, :], in_=ps[0:nk, :], func=AF.Exp, scale=0.125),
                          waits=[ts, AST["E_free"][si]], noself=True)
                AST["S_free"][si] = te
                if u > nkb - 8:
                    r = u - 1 - (nkb - 8)
                    mi = (i % 2) * 8 + r
                    te = P.op("vector", lambda e, Eb=Eb, mi=mi: e.tensor_tensor(
                        out=Eb.rearrange("p (a b) -> p a b", b=512), in0=Eb.rearrange("p (a b) -> p a b", b=512),
                        in1=masks[:, mi, :].unsqueeze(1).broadcast_to([128, 2, 512]), op=ALU.mult), waits=[te])
                return {"i": i, "hl": hl, "u": u, "si": si, "nk": nk, "Eb": Eb, "te": te, "nkb": nkb}

            def emit_PV(d):
                i, hl, u, si, nk, Eb, te, nkb = d["i"], d["hl"], d["u"], d["si"], d["nk"], d["Eb"], d["te"], d["nkb"]
                tp = None
                for m in range(2):
                    for s_ in range(4):
                        a = m * 4 + s_
                        tp = P.op("tensor", lambda e, a=a, m=m, s_=s_, Eb=Eb, nk=nk, u=u, hl=hl, nkb=nkb: e.matmul(
                            accs[a], lhsT=Eb[0:nk, m * 512 + s_ * 128:m * 512 + (s_ + 1) * 128], rhs=Vs[0:nk, u, hl, 0:129],
                            start=(u == 0 and a % 3 == 0), stop=(u == nkb), skip_group_check=True),
                            waits=[te, AST["acc_free"] if u == 0 else None] if a == 0 else [], sig=(a == 7))
                AST["E_free"][si] = tp
                if u == nkb:
                    emit_evac(i, hl, tp)

            def emit_evac(i, hl, last_pv):
                h = 2 * p + hl
                st = ev_st
                acc_free = None
                for s in range(4):
                    a1, a2 = accs[s], accs[4 + s]
                    P.op("vector", lambda e, a1=a1, s=s: e.reciprocal(out=st[:, s:s + 1], in_=a1[:, 128:129]), waits=[last_pv] if s == 0 else [], sig=False)
                    P.op("vector", lambda e, a2=a2, s=s: e.reciprocal(out=st[:, 4 + s:5 + s], in_=a2[:, 128:129]), sig=False)
                    P.op("vector", lambda e, s=s: e.tensor_tensor(out=st[:, 4 + s:5 + s], in0=st[:, 4 + s:5 + s], in1=nlam[:], op=ALU.mult), sig=False)
                    P.op("vector", lambda e, a1=a1, s=s: e.tensor_scalar(out=ev_t1[:, s, :], in0=a1[:, 0:128], scalar1=st[:, s:s + 1], scalar2=None, op0=ALU.mult), sig=False)
                    acc_free = P.op("vector", lambda e, a2=a2, s=s: e.scalar_tensor_tensor(out=ev_o[:, s, :], in0=a2[:, 0:128], scalar=st[:, 4 + s:5 + s], in1=ev_t1[:, s, :],
                                                                                            op0=ALU.mult, op1=ALU.add), sig=False)
                AST["acc_free"] = acc_free
                tss = None
                for s in range(4):
                    P.op("vector", lambda e, s=s: e.tensor_tensor(out=ev_sq, in0=ev_o[:, s, :], in1=ev_o[:, s, :], op=ALU.mult), sig=False)
                    tss = P.op("vector", lambda e, s=s: e.reduce_sum(out=st[:, 8 + s:9 + s], in_=ev_sq, axis=AX.X))
                tsq = P.op("scalar", lambda e: e.activation(out=st[:, 8:12], in_=st[:, 8:12], func=AF.Sqrt, scale=1.0 / 128, bias=EPS_T[:, 0:1]), waits=[tss])
                P.op("vector", lambda e: e.reciprocal(out=st[:, 8:12], in_=st[:, 8:12]), waits=[tsq], sig=False)
                tob = None
                for s in range(4):
                    tob = P.op("vector", lambda e, s=s: e.scalar_tensor_tensor(out=ev_ob[:, s, :], in0=ev_o[:, s, :], scalar=st[:, 8 + s:9 + s], in1=gsub_t[:],
                                                                                op0=ALU.mult, op1=ALU.mult), waits=[AST["otr_free"]] if s == 0 else [])
                pot = pbank_bf(7).rearrange("p (a b) -> p a b", b=128)[:, 0:4, :]
                tt = None
                for s in range(4):
                    tt = P.op("tensor", lambda e, s=s: e.transpose(out=pot[:, s, :], in_=ev_ob[:, s, :], identity=ident[:]),
                              waits=[tob, AST["otr_free"]] if s == 0 else [], sig=(s == 3))
                n_ot = AST["n_ot"]
                osb = ot_st[n_ot % 2]
                otr = P.op("vector", lambda e, osb=osb: e.tensor_copy(out=osb, in_=pot.rearrange("p a b -> p (a b)")), waits=[tt, AST["ot_st_free"][n_ot % 2]])
                AST["otr_free"] = otr
                AST["ot_st_free"][n_ot % 2] = P.dma("sync", lambda e, osb=osb, h=h, i=i: e.dma_start(out=otbuf[:, h * NOWN + i * GT:h * NOWN + (i + 1) * GT], in_=osb),
                                                    f"D{33 + n_ot % 2}", waits=[otr])
                AST["n_ot"] = n_ot + 1

            units = [(i, hl, u) for i in range(NG) for hl in range(2) for u in range(8 * i + 9)]
            prev = None
            for un, (i, hl, u) in enumerate(units):
                d = emit_S_exp(i, hl, u, un)
                if prev is not None:
                    emit_PV(prev)
                prev = d
            emit_PV(prev)
            P.barrier()
        if STOP_AFTER not in ("O", "A", "B"):
            V = lambda fn, waits=(): P.op("vector", fn, waits)
            A = lambda fn, waits=(): P.op("scalar", fn, waits)
            T = lambda fn, waits=(), sig=True: P.op("tensor", fn, waits, sig)
            cv.reset()
            xt4 = cv.get([128, 4, D], F32)
            B = {}
            B["junk"] = cv.get([128, D], BF16)
            B["xn"] = [cv.get([128, D], BF16) for _ in range(2)]
            B["st"] = [cv.get([128, 16], F32) for _ in range(2)]
            hal_xt = cv.get([128, D], F32)
            xnTg = cv.get([128, 8, 516], BF16)
            Wc = cv.get([128, 8, 1536], BF16)
            Wo = cv.get([128, 8, D], BF16)
            g2_t = cv.get([128, D], F32)
            cc = cv.get([128, 516], F32)
            z = cv.get([128, 516], F32)
            yv = cv.get([128, 512], F32)
            mixc = cv.get([128, 4, 512], BF16)
            h1 = [cv.get([128, D], F32) for _ in range(2)]
            xn2 = [cv.get([128, D], BF16) for _ in range(2)]
            xn2T = cv.get([128, 8, 128], BF16)
            R = cv.get([128, 1024], F32)
            OTg = [cv.get([128, 4, GT], BF16) for _ in range(2)]
            otg_free = [None, None]
            st2 = cv.get([128, 8], F32)
            ohb = cv.get([128, 4, 32], BF16)
            reset_state()
            P.dma("gpsimd", lambda e: e.dma_start(out=Wc, in_=w_in_v[:, :, 0:1536]), "D16")
            P.dma("gpsimd", lambda e: e.dma_start(out=Wo, in_=w_out_v), "D16")
            tk_g2 = P.dma("sync", lambda e: e.dma_start(out=g2_t, in_=norm2_g.partition_broadcast(128)), "D37")
            tk_wc = [("D16", P.cnt["D16"]), tk_g2]
            ps5 = pbank(5)
            ps_rt = pbank_bf(2).rearrange("p (a b) -> p a b", b=128)
            rtc_free = None; xn2T_free = None
            xt4_tok = [None] * 4
            xt4_free = [None] * 4; hal_free = None; grp_free = None; conv_free = None
            ph_free = [None, None]; h1_free = [None, None]; xn2_free = [None, None]; rt_free = None
            nph = 0
            for i in range(NG):
                fr = []
                t4, t3, _ = x_front(B, xhalo[i * 128:(i + 1) * 128, :], xnTg[:, :, 0:2], 2, grp_free, xt=hal_xt, xt_free=hal_free, xt_sem="D11")
                hal_free = t3; fr.append(t4)
                for s in range(4):
                    xt4_tok[s] = P.dma("sync", lambda e, s=s, i=i: e.dma_start(out=xt4[:, s, :], in_=xown[(4 * i + s) * 128:(4 * i + s + 1) * 128, :]),
                                       f"D{7 + s}", waits=[xt4_free[s]])
                    t4, _, _ = x_front(B, None, xnTg[:, :, 2 + s * 128:2 + (s + 1) * 128], 128, grp_free, load_from=cacheO[4 * i + s], load_sem=f"D{42 + s}")
                    fr.append(t4)
                OTc = OTg[i % 2]
                tk_otg = P.dma("sync", lambda e, OTc=OTc, i=i: e.dma_start(out=OTc, in_=otbuf.rearrange("p (h t) -> p h t", h=4)[:, :, i * GT:(i + 1) * GT]),
                               f"D{35 + i % 2}", waits=[otg_free[i % 2]])
                tmix = None
                tm = None
                for q in range(4):
                    for (blkc, dstp, hcol) in ((q, pbank(2), None), (4 + q, pbank(3), 0), (8 + q, pbank(4), 2)):
                        for c in range(8):
                            tm = T(lambda e, c=c, blkc=blkc, dstp=dstp: e.matmul(dstp, lhsT=Wc[:, c, blkc * 128:(blkc + 1) * 128], rhs=xnTg[:, c, 2:514],
                                                                                 start=(c == 0), stop=(c == 7)),
                                   waits=fr + [tk_wc, conv_free, rt_free] if c == 0 else [], sig=(c == 7))
                        if hcol is not None:
                            for c in range(8):
                                tm = T(lambda e, c=c, blkc=blkc, hcol=hcol: e.matmul(ps5[:, hcol:hcol + 2], lhsT=Wc[:, c, blkc * 128:(blkc + 1) * 128], rhs=xnTg[:, c, 0:2],
                                                                                     start=(c == 0), stop=(c == 7)), sig=(c == 7))
                    A(lambda e: e.copy(out=cc[:, 2:514], in_=pbank(3)), waits=[tm])
                    ta = A(lambda e: e.copy(out=cc[:, 0:2], in_=ps5[:, 0:2]))
                    V(lambda e: e.tensor_tensor(out=z[:, 2:514], in0=cc[:, 2:514], in1=pbank(4), op=ALU.mult), waits=[ta, tm])
                    V(lambda e: e.tensor_tensor(out=z[:, 0:2], in0=cc[:, 0:2], in1=ps5[:, 2:4], op=ALU.mult))
                    V(lambda e, q=q: e.tensor_scalar(out=yv, in0=z[:, 0:512], scalar1=convw[:, q, 0:1], scalar2=None, op0=ALU.mult))
                    V(lambda e, q=q: e.scalar_tensor_tensor(out=yv, in0=z[:, 1:513], scalar=convw[:, q, 1:2], in1=yv, op0=ALU.mult, op1=ALU.add))
                    V(lambda e, q=q: e.scalar_tensor_tensor(out=yv, in0=z[:, 2:514], scalar=convw[:, q, 2:3], in1=yv, op0=ALU.mult, op1=ALU.add))
                    conv_free = V(lambda e, q=q: e.tensor_tensor(out=mixc[:, q, :], in0=pbank(2), in1=yv, op=ALU.mult), waits=[grp_free])
                tmix = conv_free
                grp_free = tm
                for s in range(4):
                    ot = 4 * i + s
                    par = ot % 2
                    hb = h1[par]; xb = xn2[par]
                    for hf in range(2):
                        ph = pbank(6 + nph % 2); pfree = ph_free[nph % 2]
                        tw = None
                        for kk in range(8):
                            lhsT = mixc[:, kk, s * 128:(s + 1) * 128] if kk < 4 else OTc[:, kk - 4, s * 128:(s + 1) * 128]
                            tw = T(lambda e, kk=kk, lhsT=lhsT, ph=ph, hf=hf: e.matmul(ph, lhsT=lhsT, rhs=Wo[:, kk, hf * 512:(hf + 1) * 512], start=(kk == 0), stop=(kk == 7)),
                                   waits=[tmix, pfree, tk_otg] if kk == 0 else [], sig=(kk == 7))
                        th = V(lambda e, hb=hb, ph=ph, s=s, hf=hf: e.tensor_tensor(out=hb[:, hf * 512:(hf + 1) * 512], in0=ph, in1=xt4[:, s, hf * 512:(hf + 1) * 512], op=ALU.add),
                               waits=[tw, h1_free[par], xt4_tok[s]])
                        ph_free[nph % 2] = th
                        nph += 1
                    xt4_free[s] = th
                    if s == 3:
                        otg_free[i % 2] = tw
                    A(lambda e, hb=hb: e.activation(out=B["junk"], in_=hb, func=AF.Square, accum_out=st2[:, 0:1]), waits=[th])
                    ta = A(lambda e: e.activation(out=st2[:, 1:2], in_=st2[:, 0:1], func=AF.Sqrt, scale=1.0 / D, bias=EPS_T[:, 0:1]))
                    V(lambda e: e.reciprocal(out=st2[:, 2:3], in_=st2[:, 1:2]), waits=[ta])
                    tx = V(lambda e, hb=hb, xb=xb: e.scalar_tensor_tensor(out=xb, in0=hb, scalar=st2[:, 2:3], in1=g2_t, op0=ALU.mult, op1=ALU.mult), waits=[xn2_free[par]])
                    d1 = P.dma("sync", lambda e, hb=hb, ot=ot: e.dma_start(out=h1buf[ot * 128:(ot + 1) * 128, :], in_=hb), f"D{12 + par}", waits=[th])
                    d2 = P.dma("sync", lambda e, xb=xb, ot=ot: e.dma_start(out=xn2buf[ot * 128:(ot + 1) * 128, :], in_=xb), f"D{14 + par}", waits=[tx])
                    h1_free[par] = [d1, tx]
                    tt = None
                    for c in range(8):
                        tt = T(lambda e, c=c, xb=xb: e.transpose(out=ps_rt[:, c, :], in_=xb[:, c * 128:(c + 1) * 128], identity=ident[:]),
                               waits=[tx, rtc_free, conv_free] if c == 0 else [], sig=(c == 7))
                    rtc_free = A(lambda e: e.copy(out=xn2T, in_=ps_rt), waits=[tt, xn2T_free])
                    xn2_free[par] = [d2, tt]
                    tl = None
                    for c in range(8):
                        tl = T(lambda e, c=c, s=s: e.matmul(ps5[:, 16 + 36 * s:52 + 36 * s], lhsT=xn2T[:, c, :], rhs=Wr[:, c, :], start=(c == 0), stop=(c == 7)),
                               waits=[rtc_free, rt_free] if c == 0 else [], sig=(c == 7))
                    xn2T_free = tl
                o4 = 4 * i
                def R3(a, n, k):
                    return R[:, a:a + 4 * k].rearrange("p (s k) -> p s k", k=k)
                def R2(a):
                    return R[:, a:a + 4]
                def bc(ap2, k):
                    return ap2.unsqueeze(2).broadcast_to([128, 4, k])
                LG = R3(0, 4, 36)
                V(lambda e: e.tensor_tensor(out=LG, in0=ps5[:, 16:160].rearrange("p (s k) -> p s k", k=36), in1=br_t[:].unsqueeze(1).broadcast_to([128, 4, 36]), op=ALU.add), waits=[tl])
                LGg = LG[:, :, 0:4]
                V(lambda e: e.tensor_reduce(out=R2(144), in_=LGg, axis=AX.X, op=ALU.max))
                V(lambda e: e.tensor_tensor(out=R3(148, 4, 4), in0=LGg, in1=bc(R2(144), 4), op=ALU.is_equal))
                tg = V(lambda e: e.tensor_tensor(out=R3(164, 4, 4), in0=LGg, in1=bc(R2(144), 4), op=ALU.subtract))
                tge = A(lambda e: e.activation(out=R[:, 180:196], in_=R[:, 164:180], func=AF.Exp), waits=[tg])
                V(lambda e: e.reduce_sum(out=R2(196), in_=R3(180, 4, 4), axis=AX.X), waits=[tge])
                V(lambda e: e.reciprocal(out=R2(200), in_=R2(196)))
                V(lambda e: e.tensor_tensor(out=R3(164, 4, 4), in0=R3(148, 4, 4), in1=iota_t[:, 0:4].unsqueeze(1).broadcast_to([128, 4, 4]), op=ALU.mult))
                V(lambda e: e.reduce_sum(out=R2(204), in_=R3(164, 4, 4), axis=AX.X))
                PR = R[:, 208:336].rearrange("p (s g j) -> p s g j", g=4, j=8)
                V(lambda e: e.tensor_tensor(out=PR, in0=LG[:, :, 4:36].rearrange("p s (g j) -> p s g j", j=8),
                                            in1=R3(148, 4, 4).unsqueeze(3).broadcast_to([128, 4, 4, 8]), op=ALU.mult))
                ES = R3(336, 4, 8)
                V(lambda e: e.reduce_sum(out=ES, in_=PR.rearrange("p s g j -> p s j g"), axis=AX.X))
                T8 = R3(368, 4, 8)
                for s in range(4):
                    V(lambda e, s=s: e.max(out=T8[:, s, :], in_=ES[:, s, :]))
                OH = [R3(400, 4, 8), R3(432, 4, 8)]
                for k in range(2):
                    V(lambda e, k=k: e.tensor_tensor(out=OH[k], in0=ES, in1=T8[:, :, k:k + 1].broadcast_to([128, 4, 8]), op=ALU.is_equal))
                    V(lambda e, k=k: e.tensor_tensor(out=R3(464, 4, 8), in0=OH[k], in1=iota_t[:, 0:8].unsqueeze(1).broadcast_to([128, 4, 8]), op=ALU.mult))
                    V(lambda e, k=k: e.reduce_sum(out=R2(496 + 4 * k), in_=R3(464, 4, 8), axis=AX.X))
                    V(lambda e, k=k: e.scalar_tensor_tensor(out=eid_all[:, o4:o4 + 4, k], in0=R2(204), scalar=8.0, in1=R2(496 + 4 * k), op0=ALU.mult, op1=ALU.add))
                td = V(lambda e: e.tensor_tensor(out=R2(504), in0=T8[:, :, 1], in1=T8[:, :, 0], op=ALU.subtract))
                tex = A(lambda e: e.activation(out=R2(508), in_=R2(504), func=AF.Exp), waits=[td])
                V(lambda e: e.tensor_scalar(out=R2(512), in0=R2(508), scalar1=1.0, scalar2=None, op0=ALU.add), waits=[tex])
                V(lambda e: e.reciprocal(out=R2(516), in_=R2(512)))
                V(lambda e: e.tensor_tensor(out=gate_all[:, o4:o4 + 4, 0], in0=R2(200), in1=R2(516), op=ALU.mult))
                V(lambda e: e.tensor_tensor(out=gate_all[:, o4:o4 + 4, 1], in0=R2(200), in1=gate_all[:, o4:o4 + 4, 0], op=ALU.subtract))
                O32 = [R3(520, 4, 32), R3(648, 4, 32)]
                for k in range(2):
                    V(lambda e, k=k: e.tensor_tensor(out=O32[k], in0=iota_t[:].unsqueeze(1).broadcast_to([128, 4, 32]),
                                                     in1=eid_all[:, o4:o4 + 4, k:k + 1].broadcast_to([128, 4, 32]), op=ALU.is_equal))
                toh = V(lambda e: e.tensor_tensor(out=ohb, in0=O32[0], in1=O32[1], op=ALU.add))
                tcn = None
                for s in range(4):
                    T(lambda e, s=s: e.matmul(ps5[:, 160 + 32 * s:192 + 32 * s], lhsT=Ubf[:], rhs=ohb[:, s, :], start=True, stop=True), waits=[toh] if s == 0 else [], sig=False)
                    tcn = T(lambda e, s=s: e.matmul(ps5[:, 288 + 32 * s:320 + 32 * s], lhsT=ones_bf[:], rhs=ohb[:, s, :], start=True, stop=True))
                BV = R3(776, 4, 32)
                V(lambda e: e.tensor_copy(out=BV[:, 0, :], in_=base[:]), waits=[tcn])
                for s in range(1, 4):
                    V(lambda e, s=s: e.tensor_tensor(out=BV[:, s, :], in0=BV[:, s - 1, :], in1=ps5[:, 288 + 32 * (s - 1):320 + 32 * (s - 1)], op=ALU.add))
                V(lambda e: e.tensor_tensor(out=base[:], in0=BV[:, 3, :], in1=ps5[:, 384:416], op=ALU.add))
                V(lambda e: e.tensor_tensor(out=BV, in0=BV, in1=ps5[:, 160:288].rearrange("p (s k) -> p s k", k=32), op=ALU.add))
                for k in range(2):
                    V(lambda e, k=k: e.tensor_tensor(out=O32[k], in0=O32[k], in1=BV, op=ALU.mult))
                    rt_free = V(lambda e, k=k: e.reduce_sum(out=rank_all[:, o4:o4 + 4, k], in_=O32[k], axis=AX.X))
            if DEBUG:
                V(lambda e: e.tensor_copy(out=R[:, 0:64], in_=eid_all[:].rearrange("p a b -> p (a b)")))
            cv.reset()
            cmpn = cv.get([128, 32, 32], F32)
            V(lambda e: e.tensor_tensor(out=cmpn, in0=base[:].unsqueeze(2).broadcast_to([128, 32, 32]),
                                        in1=thr_t[:, 0:32].unsqueeze(1).broadcast_to([128, 32, 32]), op=ALU.is_gt))
            V(lambda e: e.reduce_sum(out=pst[:, 3, :], in_=cmpn, axis=AX.X))
            V(lambda e: e.tensor_scalar(out=pst[:, 0, :], in0=pst[:, 3, :], scalar1=256.0, scalar2=None, op0=ALU.mult))
            V(lambda e: e.tensor_copy(out=pst[:, 1, 0:1], in_=pst[:, 0, 0:1]))
            for ee in range(1, 32):
                V(lambda e, ee=ee: e.tensor_tensor(out=pst[:, 1, ee:ee + 1], in0=pst[:, 1, ee - 1:ee], in1=pst[:, 0, ee:ee + 1], op=ALU.add))
            V(lambda e: e.tensor_tensor(out=pst[:, 2, :], in0=pst[:, 1, :], in1=pst[:, 0, :], op=ALU.subtract))
            cmp3 = cv.get([128, NBLK, 32], F32)
            V(lambda e: e.tensor_tensor(out=cmp3, in0=pst[:, 1, :].unsqueeze(1).broadcast_to([128, NBLK, 32]),
                                        in1=thr_t[:].unsqueeze(2).broadcast_to([128, NBLK, 32]), op=ALU.is_le))
            V(lambda e: e.reduce_sum(out=Ej_f[:], in_=cmp3, axis=AX.X))
            V(lambda e: e.tensor_scalar(out=Ej_f[:], in0=Ej_f[:], scalar1=31.0, scalar2=None, op0=ALU.min))
            tk_ej = V(lambda e: e.tensor_copy(out=Ej_i[:], in_=Ej_f[:]))
            unused = cv.get([128, NBLK], F32)
            V(lambda e: e.tensor_scalar(out=unused, in0=thr_t[:], scalar1=pst[:, 1, 31:32], scalar2=None, op0=ALU.is_ge))
            V(lambda e: e.scalar_tensor_tensor(out=Ej_f[:], in0=unused, scalar=8192.0, in1=Ej_f[:], op0=ALU.mult, op1=ALU.add))
            idxW_f = cv.get([128, NBLK, 12], F32)
            for c in range(12):
                V(lambda e, c=c: e.tensor_scalar(out=idxW_f[:, :, c], in0=Ej_f[:], scalar1=(128.0 if c < 8 else 512.0), scalar2=pidx_t[:, c:c + 1],
                                                 op0=ALU.mult, op1=ALU.add))
            tk_ej = V(lambda e: e.tensor_copy(out=idxW_i[:], in_=idxW_f))
            for ot in range(NOT_):
                for k in range(2):
                    V(lambda e, ot=ot, k=k: e.tensor_scalar(out=R[:, 96:128], in0=iota_t[:], scalar1=eid_all[:, ot, k:k + 1], scalar2=None, op0=ALU.is_equal))
                    V(lambda e: e.tensor_tensor(out=R[:, 96:128], in0=R[:, 96:128], in1=pst[:, 2, :], op=ALU.mult))
                    V(lambda e: e.reduce_sum(out=R[:, 240:241], in_=R[:, 96:128], axis=AX.X))
                    V(lambda e, ot=ot, k=k: e.tensor_tensor(out=slot_f[:, ot, k:k + 1], in0=R[:, 240:241], in1=rank_all[:, ot, k:k + 1], op=ALU.add))
            tk_slot = V(lambda e: e.tensor_copy(out=slot_i[:], in_=slot_f[:]))
            if DEBUG:
                V(lambda e: e.tensor_copy(out=R[:, 64:128], in_=slot_f[:].rearrange("p a b -> p (a b)")))
                V(lambda e: e.tensor_copy(out=R[:, 128:192], in_=gate_all[:].rearrange("p a b -> p (a b)")))
                V(lambda e: e.tensor_copy(out=R[:, 192:256], in_=Ej_f[:]))
                out_toks.append(P.dma("sync", lambda e: e.dma_start(out=dbg_rt[:, 0:256], in_=R), "D6", waits=[P.last["vector"]]))
            P.barrier()
            cv.reset()
            NXS = 4
            xs = [cv.get([128, D], BF16) for _ in range(NXS)]
            xs_free = [None] * NXS
            xs_sem = ["D17", "D18", "D38", "D39"]
            sc_sem = ["D19", "D20", "D46", "D47"]
            for ot in range(NOT_):
                par = ot % NXS
                tl = P.dma("sync", lambda e, ot=ot, par=par: e.dma_start(out=xs[par], in_=xn2buf[ot * 128:(ot + 1) * 128, :]), xs_sem[par], waits=[xs_free[par]])
                tsc = None
                for k in range(2):
                    tsc = P.dma("gpsimd", lambda e, ot=ot, k=k, par=par: e.indirect_dma_start(
                        out=xebuf, out_offset=bass.IndirectOffsetOnAxis(ap=slot_i[:, ot, k:k + 1], axis=0), in_=xs[par], in_offset=None,
                        bounds_check=breg(e, NSLOT - 1), oob_is_err=False), sc_sem[par], waits=[tl])
                xs_free[par] = tsc
            P.barrier()
            if STOP_AFTER != "C":
                cv.reset()
                Wg = [cv.get([128, 8, 512], BF16) for _ in range(2)]
                Wu = [cv.get([128, 8, 512], BF16) for _ in range(2)]
                Wd = [cv.get([128, 4, D], BF16) for _ in range(2)]
                xe = [cv.get([128, 2, D], BF16) for _ in range(2)]
                xeT = cv.get([128, 8, 256], BF16)
                sg = [cv.get([128, 256], F32) for _ in range(2)]
                hidT = cv.get([128, 4, 256], BF16)
                ysb = [cv.get([128, D], F32) for _ in range(2)]
                w_free = [None, None]; xe_free = [None, None]; xeT_free = None; pxe_free = None
                sg_free = [None, None]; pgu_free = [None, None]; hid_free = None; py_free = [None, None]; ysb_free = [None, None]
                ny = 0
                wg_v = w_gate.rearrange("e (c p) n -> p e c n", p=128)
                wu_v = w_up.rearrange("e (c p) n -> p e c n", p=128)
                wd_v = w_down.rearrange("e (c p) n -> p e c n", p=128)
                wg_rows = w_gate.rearrange("e (p c) n -> (e p) (c n)", c=8)
                wu_rows = w_up.rearrange("e (p c) n -> (e p) (c n)", c=8)
                wd_rows = w_down.rearrange("e f n -> (e f) n")
                def emit_wload(j):
                    par = j % 2
                    tok = P.dma("gpsimd", lambda e: e.indirect_dma_start(
                        out=Wg[par].rearrange("p c n -> p (c n)"), out_offset=None, in_=wg_rows, in_offset=bass.IndirectOffsetOnAxis(ap=idxW_i[:, j, 0:1], axis=0),
                        bounds_check=breg(e, NE * 128 - 1), oob_is_err=False), f"D{21 + par}", waits=[w_free[par], tk_ej])
                    tok = P.dma("gpsimd", lambda e: e.indirect_dma_start(
                        out=Wu[par].rearrange("p c n -> p (c n)"), out_offset=None, in_=wu_rows, in_offset=bass.IndirectOffsetOnAxis(ap=idxW_i[:, j, 0:1], axis=0),
                        bounds_check=breg(e, NE * 128 - 1), oob_is_err=False), f"D{21 + par}")
                    for c in range(4):
                        tok = P.dma("gpsimd", lambda e, c=c: e.indirect_dma_start(
                            out=Wd[par][:, c, :], out_offset=None, in_=wd_rows, in_offset=bass.IndirectOffsetOnAxis(ap=idxW_i[:, j, 8 + c:9 + c], axis=0),
                            bounds_check=breg(e, NE * 512 - 1), oob_is_err=False), f"D{21 + par}")
                    return tok
                def emit_xload(j):
                    par = j % 2
                    return P.dma("sync", lambda e: e.dma_start(out=xe[par], in_=xebuf[j * BLK:(j + 1) * BLK, :].rearrange("(t p) d -> p t d", p=128)),
                                 f"D{23 + par}", waits=[xe_free[par]])
                tkw = {0: emit_wload(0)}; tkx = {0: emit_xload(0)}
                ptrx = pbank_bf(0, 2).rearrange("p (a b) -> p a b", b=256)
                for j in range(NBLK):
                    par = j % 2
                    if j + 1 < NBLK:
                        tkw[j + 1] = emit_wload(j + 1); tkx[j + 1] = emit_xload(j + 1)
                    tt = None
                    for t2 in range(2):
                        for c in range(8):
                            tt = T(lambda e, t2=t2, c=c, par=par: e.transpose(out=ptrx[:, c, t2 * 128:(t2 + 1) * 128], in_=xe[par][:, t2, c::8], identity=ident[:]),
                                   waits=[tkx[j], pxe_free] if (t2 == 0 and c == 0) else [], sig=(t2 == 1 and c == 7))
                    xe_free[par] = tt
                    pxe_free = A(lambda e: e.copy(out=xeT, in_=ptrx), waits=[tt, xeT_free])
                    for fc in range(4):
                        pp = fc % 2
                        pgu = pbank(2 + pp)
                        tg = None
                        for (Wsrc, c0) in ((Wg[par], 0), (Wu[par], 256)):
                            for c in range(8):
                                tg = T(lambda e, c=c, Wsrc=Wsrc, c0=c0, pgu=pgu, fc=fc: e.matmul(pgu[:, c0:c0 + 256], lhsT=Wsrc[:, c, fc * 128:(fc + 1) * 128], rhs=xeT[:, c, :],
                                                                                                 start=(c == 0), stop=(c == 7)),
                                       waits=[pxe_free, tkw[j], pgu_free[pp]] if (c == 0 and c0 == 0) else [], sig=(c == 7))
                        tsg = A(lambda e, pgu=pgu, pp=pp: e.activation(out=sg[pp], in_=pgu[:, 0:256], func=AF.Silu), waits=[tg, sg_free[pp]])
                        th = V(lambda e, pgu=pgu, pp=pp, fc=fc: e.tensor_tensor(out=hidT[:, fc, :], in0=sg[pp], in1=pgu[:, 256:512], op=ALU.mult), waits=[tsg, hid_free] if fc == 0 else [tsg])
                        sg_free[pp] = th; pgu_free[pp] = th
                    xeT_free = tg
                    tyl = None
                    for t2 in range(2):
                        yb = ysb[t2]
                        tcs = []
                        for hf in range(2):
                            py = pbank(4 + ny % 2)
                            ty = None
                            for fc in range(4):
                                ty = T(lambda e, fc=fc, t2=t2, hf=hf, py=py, par=par: e.matmul(py, lhsT=hidT[:, fc, t2 * 128:(t2 + 1) * 128], rhs=Wd[par][:, fc, hf * 512:(hf + 1) * 512],
                                                                                       start=(fc == 0), stop=(fc == 3)),
                                       waits=[th, py_free[ny % 2]] if fc == 0 else [], sig=(fc == 3))
                            tcp = A(lambda e, yb=yb, py=py, hf=hf: e.copy(out=yb[:, hf * 512:(hf + 1) * 512], in_=py), waits=[ty, ysb_free[t2]])
                            py_free[ny % 2] = tcp
                            tcs.append(tcp)
                            ny += 1
                            tyl = ty
                        ysb_free[t2] = P.dma("sync", lambda e, yb=yb, j=j, t2=t2: e.dma_start(out=ybuf[j * BLK + t2 * 128:j * BLK + (t2 + 1) * 128, :], in_=yb),
                                             f"D{25 + t2}", waits=tcs)
                    hid_free = tyl
                    w_free[par] = tyl
                P.barrier()
                cv.reset()
                NCB = 4
                y0 = [cv.get([128, D], F32) for _ in range(NCB)]
                y1 = [cv.get([128, D], F32) for _ in range(NCB)]
                hc = [cv.get([128, D], F32) for _ in range(NCB)]
                cb_free = [None] * NCB
                g_sem = ["D27", "D28", "D19", "D20"]
                h_sem = ["D29", "D30", "D23", "D24"]
                o_sem = ["D31", "D32", "D25", "D26"]
                for ot in range(NOT_):
                    par = ot % NCB
                    tg0 = P.dma("gpsimd", lambda e, ot=ot, par=par: e.indirect_dma_start(
                        out=y0[par], out_offset=None, in_=ybuf, in_offset=bass.IndirectOffsetOnAxis(ap=slot_i[:, ot, 0:1], axis=0),
                        bounds_check=breg(e, NSLOT - 1), oob_is_err=False), g_sem[par], waits=[cb_free[par]])
                    tg1 = P.dma("gpsimd", lambda e, ot=ot, par=par: e.indirect_dma_start(
                        out=y1[par], out_offset=None, in_=ybuf, in_offset=bass.IndirectOffsetOnAxis(ap=slot_i[:, ot, 1:2], axis=0),
                        bounds_check=breg(e, NSLOT - 1), oob_is_err=False), g_sem[par])
                    tlh = P.dma("sync", lambda e, ot=ot, par=par: e.dma_start(out=hc[par], in_=h1buf[ot * 128:(ot + 1) * 128, :]), h_sem[par], waits=[cb_free[par]])
                    V(lambda e, ot=ot, par=par: e.scalar_tensor_tensor(out=hc[par], in0=y0[par], scalar=gate_all[:, ot, 0:1], in1=hc[par], op0=ALU.mult, op1=ALU.add), waits=[tg1, tlh])
                    tv = V(lambda e, ot=ot, par=par: e.scalar_tensor_tensor(out=hc[par], in0=y1[par], scalar=gate_all[:, ot, 1:2], in1=hc[par], op0=ALU.mult, op1=ALU.add))
                    cb_free[par] = P.dma("sync", lambda e, ot=ot, par=par: e.dma_start(out=out_d[ot * 128:(ot + 1) * 128, :], in_=hc[par]), o_sem[par], waits=[tv])
        P.barrier()
        P.ops["sync"].append((None, P._w("sync", []), None, 0))

        with nc.Block() as blk:
            def mk(engname):
                def body(e):
                    waited = {}
                    for fn, waits, sig, inc in P.ops[engname]:
                        for wv in waits:
                            k, v = wv[0], wv[1]
                            if P.owner.get(k) == engname and len(wv) == 2 and engname == "tensor":
                                continue
                            if waited.get(k, 0) >= v:
                                continue
                            e.wait_ge(P.sems[k], v)
                            waited[k] = v
                        if fn is None:
                            continue
                        ins = fn(e)
                        if sig is not None:
                            ins.then_inc(P.sems[sig], inc)
                return body
            blk.sync(mk("sync"))
            blk.scalar(mk("scalar"))
            blk.vector(mk("vector"))
            blk.gpsimd(mk("gpsimd"))
            blk.tensor(mk("tensor"))
    return nc


def _rope_tables(pos):
    inv = (500000.0 ** (-np.arange(0, 16, 2, dtype=np.float32) / np.float32(16))).astype(np.float32)
    ang = pos.astype(np.float32)[:, None] * inv[None, :]
    return np.cos(ang).astype(np.float32), np.sin(ang).astype(np.float32)


def _masks(variant_large):
    m = np.zeros((8, 128, 512), np.float32)
    tri = (np.arange(128)[:, None] <= np.arange(128)[None, :]).astype(np.float32)
    for r in range(8):
        for s in range(4):
            if variant_large:
                if r < 4: v = 1.0
                elif s > r - 4: v = 1.0
                elif s == r - 4: v = tri
                else: v = 0.0
            else:
                if r >= 4: v = 0.0
                elif s > r: v = 1.0
                elif s == r: v = tri
                else: v = 0.0
            m[r, :, s * 128:(s + 1) * 128] = v
    return m


_NC_CACHE = {}


def kernel(x, meta_tokens, norm1_g, w_in, conv_w, q_norm_g, k_norm_g, lambda_q1, lambda_k1, lambda_q2, lambda_k2,
           subln_g, w_out, norm2_g, w_router_group, b_router_group, w_router_expert, b_router_expert,
           w_gate, w_up, w_down):
    x = np.asarray(x, np.float32)
    f = lambda a: np.ascontiguousarray(np.asarray(a, np.float32))
    if "nc" not in _NC_CACHE:
        _NC_CACHE["nc"] = build()
    nc = _NC_CACHE["nc"]
    meta = f(meta_tokens)
    w_r = np.ascontiguousarray(np.concatenate([f(w_router_group)[0], f(w_router_expert)[0]], axis=1))
    b_r = np.ascontiguousarray(np.concatenate([f(b_router_group)[0], f(b_router_expert)[0]], axis=0)[None, :])
    shared = {
        "norm1_g": f(norm1_g), "norm2_g": f(norm2_g), "w_in": f(w_in)[0], "w_out": f(w_out)[0],
        "conv_wT": np.ascontiguousarray(f(conv_w)[0].T), "q_norm_g": f(q_norm_g), "k_norm_g": f(k_norm_g),
        "lambda_q1": f(lambda_q1), "lambda_k1": f(lambda_k1), "lambda_q2": f(lambda_q2), "lambda_k2": f(lambda_k2),
        "subln_g": f(subln_g), "w_r": w_r, "b_r": b_r,
        "w_gate": f(w_gate)[0], "w_up": f(w_up)[0], "w_down": f(w_down)[0],
        "thr": (np.arange(NBLK, dtype=np.float32) * BLK)[None, :],
        "iota": np.arange(32, dtype=np.float32)[None, :],
        "pidx": np.ascontiguousarray(np.concatenate([np.arange(8)[None, :] * 128 + np.arange(128)[:, None],
                                                     np.arange(4)[None, :] * 128 + np.arange(128)[:, None]], 1).astype(np.float32)),
    }
    posA = np.zeros((NT_ALL, 128), np.float32)
    posA[0, :16] = np.arange(16)
    posA[1:] = 16 + np.arange(SEQ).reshape(64, 128)
    cA, sA = _rope_tables(posA.reshape(-1))
    cosA = np.ascontiguousarray(cA.reshape(NT_ALL, 128, 8).transpose(1, 0, 2).reshape(128, -1))
    sinA = np.ascontiguousarray(sA.reshape(NT_ALL, 128, 8).transpose(1, 0, 2).reshape(128, -1))
    mk_l = _masks(True); mk_s = _masks(False)
    in_maps = []
    for c in range(NCORES):
        b, hf = divmod(c, 2)
        xa = np.zeros((NT_ALL * 128, D), np.float32)
        xa[:NMETA] = meta
        xa[128:] = x[b]
        gl = GROUPS[hf]
        xo = np.concatenate([x[b, g * GT:(g + 1) * GT] for g in gl], 0)
        hal = np.zeros((NG * 128, D), np.float32)
        for i, g in enumerate(gl):
            hal[128 * i:128 * i + 2] = meta[14:16] if g == 0 else x[b, g * GT - 2:g * GT]
        posO = np.concatenate([16 + g * GT + np.arange(GT) for g in gl]).astype(np.float32)
        cO, sO = _rope_tables(posO)
        cosO = np.ascontiguousarray(cO.reshape(NOT_, 128, 8).transpose(1, 0, 2).reshape(128, -1))
        sinO = np.ascontiguousarray(sO.reshape(NOT_, 128, 8).transpose(1, 0, 2).reshape(128, -1))
        mm = np.stack([mk_s if hf == 0 else mk_l, mk_l if hf == 0 else mk_s], 0)
        mm = np.ascontiguousarray(mm.reshape(16, 128, 512).transpose(1, 0, 2).reshape(128, -1)).astype(ml_dtypes.bfloat16)
        d = dict(shared)
        d.update({"xall": xa, "xown": np.ascontiguousarray(xo), "xhalo": hal, "cosA": cosA, "sinA": sinA,
                  "cosO": cosO, "sinO": sinO, "masks": mm})
        in_maps.append(d)
    kernel.last_in_maps = in_maps
    if os.environ.get("HK_NORUN") == "1":
        return None
    res = run_bass_kernel_spmd(nc, in_maps, core_ids=list(range(NCORES)))
    kernel.last_results = res.results
    out = np.zeros((4, SEQ, D), np.float32)
    for c in range(NCORES):
        b, hf = divmod(c, 2)
        o = res.results[c]["out"]
        for i, g in enumerate(GROUPS[hf]):
            out[b, g * GT:(g + 1) * GT] = o[i * GT:(i + 1) * GT]
    return out
```

```python
import os
from contextlib import ExitStack
import numpy as np
import ml_dtypes
import concourse.bass as bass
import concourse.mybir as mybir
from concourse.bass_utils import run_bass_kernel_spmd

F32 = mybir.dt.float32
BF16 = mybir.dt.bfloat16
I32 = mybir.dt.int32
ALU = mybir.AluOpType
AF = mybir.ActivationFunctionType
AX = mybir.AxisListType

NCORES = 8
D = 1024
SEQ = 8192
NMETA = 16
NT_ALL = 65
NG = 8
GT = 512
NOWN = NG * GT
NOT_ = NOWN // 128
EPS = 1e-6
NE = 32
NBLK = 64
BLK = 256
NSLOT = NBLK * BLK
GROUPS = [[0, 3, 4, 7, 8, 11, 12, 15], [1, 2, 5, 6, 9, 10, 13, 14]]

STOP_AFTER = os.environ.get("HK_STOP", "")
DEBUG = os.environ.get("HK_DEBUG", "") == "1"
STATIC_E = os.environ.get("HK_STATIC_E", "") == "1"


class Prog:
    ENGS = ["sync", "scalar", "vector", "gpsimd", "tensor"]

    def __init__(self, nc, es):
        self.nc, self.es = nc, es
        self.ops = {e: [] for e in self.ENGS}
        self.sems, self.cnt, self.owner = {}, {}, {}
        for e in self.ENGS[1:]:
            self.newsem("E_" + e, e)
        self.last = {e: None for e in self.ENGS}
        self.pending = {e: [] for e in self.ENGS}

    def barrier(self):
        toks = [self.last[e] for e in self.ENGS[1:] if self.last[e] is not None]
        toks += [(k, v) for k, v in self.cnt.items() if self.owner.get(k) is None and v > 0]
        for e in self.ENGS:
            self.pending[e] = list(toks)

    def _w(self, eng, waits):
        w = self.pending[eng] + flat(list(waits))
        self.pending[eng] = []
        return w

    def newsem(self, name, owner=None):
        self.sems[name] = self.es.enter_context(self.nc.semaphore(name))
        self.cnt[name] = 0
        self.owner[name] = owner
        return name

    SERIAL = ("scalar", "vector", "gpsimd")

    def op(self, eng, fn, waits=(), sig=True, noself=False):
        tok = None
        name = None
        w = self._w(eng, waits)
        if eng in self.SERIAL:
            sig = True
            if self.cnt["E_" + eng] > 0 and not noself:
                w = w + [("E_" + eng, self.cnt["E_" + eng], "self")]
        if sig:
            name = "E_" + eng
            self.cnt[name] += 1
            tok = (name, self.cnt[name])
        self.ops[eng].append((freeze(fn), w, name, 1))
        if tok is not None:
            self.last[eng] = tok
        return tok

    def dma(self, eng, fn, sem, waits=()):
        self.cnt[sem] += 16
        tok = (sem, self.cnt[sem])
        self.ops[eng].append((freeze(fn), self._w(eng, waits), sem, 16))
        return tok

    def replay(self, eng, e):
        waited = {}
        for fn, waits, sig, inc in self.ops[eng]:
            for (k, v) in waits:
                if self.owner.get(k) == eng:
                    continue
                if waited.get(k, 0) >= v:
                    continue
                e.wait_ge(self.sems[k], v)
                waited[k] = v
            ins = fn(e)
            if sig is not None:
                ins.then_inc(self.sems[sig], inc)


import types


def freeze(fn):
    if fn is None or fn.__closure__ is None:
        return fn
    cells = []
    for c in fn.__closure__:
        try:
            cells.append(types.CellType(c.cell_contents))
        except ValueError:
            cells.append(c)
    return types.FunctionType(fn.__code__, fn.__globals__, fn.__name__, fn.__defaults__, tuple(cells))


def flat(toks):
    out = []
    for t in toks:
        if t is None:
            continue
        if isinstance(t, list):
            out.extend(flat(t))
        else:
            out.append(t)
    return out


def build():
    nc = bass.Bass("TRN2", target_bir_lowering=False)
    dt_in = lambda n, s, d=F32: nc.dram_tensor(n, s, d, kind="ExternalInput").ap()
    xall = dt_in("xall", [NT_ALL * 128, D])
    xown = dt_in("xown", [NOWN, D])
    xhalo = dt_in("xhalo", [NG * 128, D])
    cosA = dt_in("cosA", [128, NT_ALL * 8]); sinA = dt_in("sinA", [128, NT_ALL * 8])
    cosO = dt_in("cosO", [128, NOT_ * 8]); sinO = dt_in("sinO", [128, NOT_ * 8])
    masks_d = dt_in("masks", [128, 16 * 512], BF16)
    thr_d = dt_in("thr", [1, NBLK]); iota_d = dt_in("iota", [1, 32]); pidx_d = dt_in("pidx", [128, 12])
    norm1_g = dt_in("norm1_g", [1, D]); norm2_g = dt_in("norm2_g", [1, D])
    w_in = dt_in("w_in", [D, 3072]); w_out = dt_in("w_out", [D, D])
    conv_wT = dt_in("conv_wT", [512, 3])
    q_g = dt_in("q_norm_g", [1, 64]); k_g = dt_in("k_norm_g", [1, 64])
    lq1 = dt_in("lambda_q1", [1, 64]); lk1 = dt_in("lambda_k1", [1, 64])
    lq2 = dt_in("lambda_q2", [1, 64]); lk2 = dt_in("lambda_k2", [1, 64])
    subln_g = dt_in("subln_g", [1, 128])
    w_r = dt_in("w_r", [D, 36]); b_r = dt_in("b_r", [1, 36])
    w_gate = dt_in("w_gate", [NE, D, 512]); w_up = dt_in("w_up", [NE, D, 512]); w_down = dt_in("w_down", [NE, 512, D])
    out_d = nc.dram_tensor("out", [NOWN, D], F32, kind="ExternalOutput").ap()
    scr_kind = "ExternalOutput" if DEBUG else "Internal"
    h1buf = nc.dram_tensor("h1buf", [NOWN, D], F32, kind=scr_kind).ap()
    xn2buf = nc.dram_tensor("xn2buf", [NOWN, D], BF16, kind="Internal").ap()
    otbuf = nc.dram_tensor("otbuf", [128, 4 * NOWN], BF16, kind=scr_kind).ap()
    cacheA = nc.dram_tensor("cacheA", [NT_ALL, 128, D], BF16, kind="Internal").ap()
    cacheO = nc.dram_tensor("cacheO", [NOT_, 128, D], BF16, kind="Internal").ap()
    xebuf = nc.dram_tensor("xebuf", [NSLOT, D], BF16, kind="Internal").ap()
    ybuf = nc.dram_tensor("ybuf", [NSLOT, D], F32, kind="Internal").ap()
    if DEBUG:
        dbg_qt = nc.dram_tensor("dbg_qt", [128, 2 * NOWN], BF16, kind="ExternalOutput").ap()
        dbg_kt = nc.dram_tensor("dbg_kt", [128, 2 * NT_ALL * 128], BF16, kind="ExternalOutput").ap()
        dbg_v = nc.dram_tensor("dbg_v", [128, NT_ALL * 2 * 130], BF16, kind="ExternalOutput").ap()
        dbg_rt = nc.dram_tensor("dbg_rt", [128, 1024], F32, kind="ExternalOutput").ap()

    with ExitStack() as es:
        def sb(name, shape, dt):
            return es.enter_context(nc.sbuf_tensor("s_" + name, shape, dt))
        P = Prog(nc, es)
        for i in range(48):
            P.newsem(f"D{i}")
        ident = sb("ident", [128, 128], BF16)
        Ubf = sb("Ubf", [128, 128], BF16)
        ones_bf = sb("ones_bf", [128, 128], BF16)
        g1_t = sb("g1_t", [128, D], F32)
        gq_t = sb("gq_t", [128, 64], F32)
        gk_t = sb("gk_t", [128, 64], F32)
        gsub_t = sb("gsub_t", [128, 128], F32)
        lam_w = sb("lam_w", [128, 8], F32)
        nlam = sb("nlam", [128, 1], F32)
        EPS_T = sb("EPS_T", [128, 1], F32)
        masks = sb("masks", [128, 16, 512], BF16)
        cosA_t = sb("cosA_t", [128, NT_ALL, 8], F32); sinA_t = sb("sinA_t", [128, NT_ALL, 8], F32)
        cosO_t = sb("cosO_t", [128, NOT_, 8], F32); sinO_t = sb("sinO_t", [128, NOT_, 8], F32)
        Wr = sb("Wr", [128, 8, 36], BF16)
        br_t = sb("br_t", [128, 36], F32)
        convw = sb("convw", [128, 4, 3], F32)
        thr_t = sb("thr_t", [128, NBLK], F32)
        iota_t = sb("iota_t", [128, 32], F32)
        QT = sb("QT", [128, 2, NOWN], BF16)
        eid_all = sb("eid_all", [128, NOT_, 2], F32)
        rank_all = sb("rank_all", [128, NOT_, 2], F32)
        gate_all = sb("gate_all", [128, NOT_, 2], F32)
        slot_f = sb("slot_f", [128, NOT_, 2], F32)
        slot_i = sb("slot_i", [128, NOT_, 2], I32)
        base = sb("base", [128, 32], F32)
        pst = sb("pst", [128, 4, 32], F32)
        pst_i = sb("pst_i", [128, 32], I32)
        Ej_f = sb("Ej_f", [128, NBLK], F32)
        Ej_i = sb("Ej_i", [128, NBLK], I32)
        pidx_t = sb("pidx_t", [128, 12], F32)
        idxW_i = sb("idxW_i", [128, NBLK, 12], I32)
        regs = {}
        def breg(e, bound):
            key = (id(e), bound)
            if key not in regs:
                regs[key] = e.to_reg(bound)
            return regs[key]
        ARENA_B = 118 * 1024
        arena = sb("arena", [128, ARENA_B // 2], BF16)

        class Carver:
            def __init__(self):
                self.off = 0
            def reset(self):
                self.off = 0
            def get(self, shape, dt):
                n = int(np.prod(shape[1:]))
                nb = n * (4 if dt in (F32, I32) else 2)
                nb_al = (nb + 63) // 64 * 64
                a = arena[:, self.off // 2:(self.off + nb) // 2]
                self.off += nb_al
                assert self.off <= ARENA_B, (self.off, ARENA_B)
                if dt != BF16:
                    a = a.bitcast(dt)
                if len(shape) == 3:
                    a = a.rearrange("p (a b) -> p a b", b=shape[2])
                elif len(shape) == 4:
                    a = a.rearrange("p (a b c) -> p a b c", b=shape[2], c=shape[3])
                return a
        cv = Carver()

        psum = es.enter_context(nc.psum_tensor("psum", [128, 4096], F32))
        def pbank(b, nb=1):
            return psum[:, b * 512:(b + nb) * 512]
        def pbank_bf(b, nb=1):
            return psum[:, b * 512:(b + nb) * 512].bitcast(BF16)

        w_in_v = w_in.rearrange("(c p) n -> p c n", p=128)
        w_out_v = w_out.rearrange("(c p) n -> p c n", p=128)
        out_toks = []

        cv.reset()
        ident_f = cv.get([128, 128], F32)
        U_f = cv.get([128, 128], F32)
        lam_in = cv.get([128, 4, 64], F32)
        ztile = cv.get([128, 1024], BF16)
        P.op("gpsimd", lambda e: e.memset(ident_f, 0.0), sig=False)
        P.op("gpsimd", lambda e: e.affine_select(out=ident_f, in_=ident_f, pattern=[[-1, 128]], compare_op=ALU.not_equal,
                                                  fill=1.0, base=0, channel_multiplier=1), sig=False)
        P.op("gpsimd", lambda e: e.memset(U_f, 1.0), sig=False)
        P.op("gpsimd", lambda e: e.affine_select(out=U_f, in_=U_f, pattern=[[1, 128]], compare_op=ALU.is_gt,
                                                  fill=0.0, base=0, channel_multiplier=-1), sig=False)
        P.op("gpsimd", lambda e: e.tensor_copy(out=ident[:], in_=ident_f), sig=False)
        P.op("gpsimd", lambda e: e.tensor_copy(out=Ubf[:], in_=U_f), sig=False)
        P.op("gpsimd", lambda e: e.memset(base[:], 0.0), sig=False)
        P.op("gpsimd", lambda e: e.memset(EPS_T[:], EPS), sig=False)
        P.op("gpsimd", lambda e: e.memset(ones_bf[:], 1.0), sig=False)
        tkz = P.op("gpsimd", lambda e: e.memset(ztile, 0.0))
        def cdma(out, in_):
            P.dma("sync", lambda e, o=out, i=in_: e.dma_start(out=o, in_=i), "D0")
        cdma(g1_t[:], norm1_g.partition_broadcast(128))
        cdma(gq_t[:], q_g.partition_broadcast(128))
        cdma(gk_t[:], k_g.partition_broadcast(128))
        cdma(gsub_t[:], subln_g.partition_broadcast(128))
        cdma(lam_in[:, 0, :], lq1.partition_broadcast(128))
        cdma(lam_in[:, 1, :], lk1.partition_broadcast(128))
        cdma(lam_in[:, 2, :], lq2.partition_broadcast(128))
        cdma(lam_in[:, 3, :], lk2.partition_broadcast(128))
        cdma(masks[:], masks_d.rearrange("p (a b) -> p a b", b=512))
        cdma(cosA_t[:], cosA.rearrange("p (a b) -> p a b", b=8))
        cdma(sinA_t[:], sinA.rearrange("p (a b) -> p a b", b=8))
        cdma(cosO_t[:], cosO.rearrange("p (a b) -> p a b", b=8))
        cdma(sinO_t[:], sinO.rearrange("p (a b) -> p a b", b=8))
        cdma(br_t[:], b_r.partition_broadcast(128))
        cdma(convw[:], conv_wT.rearrange("(c p) k -> p c k", p=128))
        cdma(thr_t[:], thr_d.partition_broadcast(128))
        cdma(iota_t[:], iota_d.partition_broadcast(128))
        cdma(pidx_t[:], pidx_d)
        tk_cd = ("D0", P.cnt["D0"])
        P.dma("gpsimd", lambda e: e.dma_start(out=Wr[:], in_=w_r.rearrange("(c p) n -> p c n", p=128)), "D1")
        P.op("vector", lambda e: e.tensor_tensor(out=lam_in[:, 0, :], in0=lam_in[:, 0, :], in1=lam_in[:, 1, :], op=ALU.mult), waits=[tk_cd], sig=False)
        P.op("vector", lambda e: e.tensor_tensor(out=lam_in[:, 2, :], in0=lam_in[:, 2, :], in1=lam_in[:, 3, :], op=ALU.mult), sig=False)
        P.op("vector", lambda e: e.reduce_sum(out=lam_w[:, 0:1], in_=lam_in[:, 0, :], axis=AX.X), sig=False)
        tk = P.op("vector", lambda e: e.reduce_sum(out=lam_w[:, 1:2], in_=lam_in[:, 2, :], axis=AX.X))
        tk = P.op("scalar", lambda e: e.activation(out=lam_w[:, 2:4], in_=lam_w[:, 0:2], func=AF.Exp), waits=[tk])
        P.op("vector", lambda e: e.tensor_tensor(out=lam_w[:, 4:5], in0=lam_w[:, 3:4], in1=lam_w[:, 2:3], op=ALU.subtract), waits=[tk], sig=False)
        P.op("vector", lambda e: e.tensor_scalar(out=nlam[:], in0=lam_w[:, 4:5], scalar1=-0.2, scalar2=None, op0=ALU.add), sig=False)
        P.op("vector", lambda e: e.tensor_scalar(out=gsub_t[:], in0=gsub_t[:], scalar1=0.8, scalar2=None, op0=ALU.mult))
        xe_v = xebuf.rearrange("(p r) d -> p r d", p=128)
        for q in range(8):
            P.dma("gpsimd", lambda e, q=q: e.dma_start(out=xe_v[:, q * 16:(q + 1) * 16, :],
                                                        in_=ztile.unsqueeze(1).broadcast_to([128, 16, 1024])), "D2", waits=[tkz])
        P.barrier()

        def carve_front():
            cv.reset()
            B = {}
            B["xt"] = [cv.get([128, D], F32) for _ in range(2)]
            B["junk"] = cv.get([128, D], BF16)
            B["xn"] = [cv.get([128, D], BF16) for _ in range(2)]
            B["xnT"] = [cv.get([128, 8, 128], BF16) for _ in range(2)]
            B["st"] = [cv.get([128, 16], F32) for _ in range(2)]
            B["sq2"] = [cv.get([128, 256], F32) for _ in range(2)]
            B["stq"] = [cv.get([128, 16], F32) for _ in range(2)]
            B["t"] = cv.get([128, 512], F32)
            B["rp"] = cv.get([128, 4, 8, 8], F32)
            B["qb"] = [cv.get([128, 512], BF16) for _ in range(2)]
            B["W"] = cv.get([128, 8, 512], BF16)
            return B

        state = {"n": 0, "save_tok": [None, None]}
        def reset_state():
            for k in ("xt_free", "xn_free", "ptr_free"):
                state[k] = [None, None]
            state["sq2_free"] = [None, None]; state["stq_free"] = [None, None]

        def x_front(B, src_rows, dst, ncol, dst_free, xt=None, xt_free=None, xt_sem=None, defer_copy=False, save_to=None, load_from=None, load_sem=None):
            n = state["n"]; state["n"] += 1
            b = n % 2
            if load_from is not None:
                sem = load_sem or f"D{38 + b}"
                tl = P.dma("sync", lambda e: e.dma_start(out=dst, in_=load_from.rearrange("p (c k) -> p c k", k=128)[:, :, 0:ncol]), sem, waits=[dst_free])
                if defer_copy:
                    return (lambda: tl), None, b
                return tl, None, b
            if xt is None:
                xt = B["xt"][b]; xt_free = state["xt_free"][b]; xt_sem = ["D4", "D5"][b]
            xn, st = B["xn"][b], B["st"][b]
            tl = P.dma("sync", lambda e: e.dma_start(out=xt, in_=src_rows), xt_sem, waits=[xt_free])
            P.op("scalar", lambda e: e.activation(out=B["junk"], in_=xt, func=AF.Square, accum_out=st[:, 0:1]), waits=[tl], sig=False)
            t2 = P.op("scalar", lambda e: e.activation(out=st[:, 1:2], in_=st[:, 0:1], func=AF.Sqrt, scale=1.0 / D, bias=EPS_T[:, 0:1]))
            P.op("vector", lambda e: e.reciprocal(out=st[:, 2:3], in_=st[:, 1:2]), waits=[t2], sig=False)
            t3 = P.op("vector", lambda e: e.scalar_tensor_tensor(out=xn, in0=xt, scalar=st[:, 2:3], in1=g1_t[:], op0=ALU.mult, op1=ALU.mult),
                      waits=[state["xn_free"][b]])
            state["xt_free"][b] = t3
            ptr = pbank_bf(b).rearrange("p (a b) -> p a b", b=128)
            tt = None
            for c in range(8):
                tt = P.op("tensor", lambda e, c=c: e.transpose(out=ptr[:, c, :], in_=xn[:, c * 128:(c + 1) * 128], identity=ident[:]),
                          waits=[t3, state["ptr_free"][b]] if c == 0 else [], sig=(c == 7))
            state["xn_free"][b] = tt
            def do_copy():
                t4 = P.op("scalar", lambda e: e.copy(out=dst, in_=ptr[:, :, 0:ncol]), waits=[tt, dst_free, state["save_tok"][b]])
                state["ptr_free"][b] = t4
                if save_to is not None:
                    state["save_tok"][b] = P.dma("sync", lambda e: e.dma_start(out=save_to.rearrange("p (c k) -> p c k", k=128), in_=dst), f"D{40 + b}", waits=[t4])
                return t4
            if defer_copy:
                return do_copy, t3, b
            return do_copy(), t3, b

        def qk_stats_a(B, pin, ncol, waits, par):
            sq = B["sq2"][par]
            return P.op("scalar", lambda e: e.activation(out=sq[:, 0:ncol], in_=pin, func=AF.Square), waits=list(waits) + [state["sq2_free"][par]])

        def qk_stats_b(B, ncol, ta, par):
            nh = ncol // 64
            sq = B["sq2"][par]; stq = B["stq"][par]
            tb = P.op("vector", lambda e: e.reduce_sum(out=stq[:, 0:nh], in_=sq[:, 0:ncol].rearrange("p (a b) -> p a b", b=64), axis=AX.X), waits=[ta, state["stq_free"][par]])
            state["sq2_free"][par] = tb
            tc_ = P.op("scalar", lambda e: e.activation(out=stq[:, 0:nh], in_=stq[:, 0:nh], func=AF.Sqrt, scale=1.0 / 64, bias=EPS_T[:, 0:1]), waits=[tb])
            return P.op("vector", lambda e: e.reciprocal(out=stq[:, 0:nh], in_=stq[:, 0:nh]), waits=[tc_])

        def qk_apply(B, pin, ncol, g_t, cos_t, sin_t, outb, tr, out_free, par):
            nh = ncol // 64
            t, rp = B["t"], B["rp"]
            stq = B["stq"][par]
            t3v = t[:, 0:ncol].rearrange("p (a b) -> p a b", b=64)
            P.op("vector", lambda e: e.tensor_tensor(out=t3v, in0=pin.rearrange("p (a b) -> p a b", b=64),
                                                     in1=stq[:, 0:nh].unsqueeze(2).broadcast_to([128, nh, 64]), op=ALU.mult), waits=[tr], sig=False)
            state["stq_free"][par] = P.op("vector", lambda e: e.tensor_tensor(out=t3v, in0=t3v, in1=g_t[:].unsqueeze(1).broadcast_to([128, nh, 64]), op=ALU.mult), sig=False)
            ob3 = outb.rearrange("p (a b) -> p a b", b=64)
            P.op("vector", lambda e: e.tensor_copy(out=ob3[:, :, 16:64], in_=t3v[:, :, 16:64]), waits=[out_free], sig=False)
            cosb = cos_t.unsqueeze(1).broadcast_to([128, nh, 8]); sinb = sin_t.unsqueeze(1).broadcast_to([128, nh, 8])
            r1 = t3v[:, :, 0:8]; r2 = t3v[:, :, 8:16]
            P.op("vector", lambda e: e.tensor_tensor(out=rp[:, 0, 0:nh, :], in0=r1, in1=cosb, op=ALU.mult), sig=False)
            P.op("vector", lambda e: e.tensor_tensor(out=rp[:, 1, 0:nh, :], in0=r2, in1=sinb, op=ALU.mult), sig=False)
            P.op("vector", lambda e: e.tensor_tensor(out=rp[:, 2, 0:nh, :], in0=r2, in1=cosb, op=ALU.mult), sig=False)
            P.op("vector", lambda e: e.tensor_tensor(out=rp[:, 3, 0:nh, :], in0=r1, in1=sinb, op=ALU.mult), sig=False)
            P.op("vector", lambda e: e.tensor_tensor(out=ob3[:, :, 0:8], in0=rp[:, 0, 0:nh, :], in1=rp[:, 1, 0:nh, :], op=ALU.subtract), sig=False)
            return P.op("vector", lambda e: e.tensor_tensor(out=ob3[:, :, 8:16], in0=rp[:, 2, 0:nh, :], in1=rp[:, 3, 0:nh, :], op=ALU.add))

        n_pass = 2
        for p in range(n_pass):
            h0 = 2 * p
            B = carve_front()
            KT = cv.get([128, 2, NT_ALL * 128], BF16)
            Vs = cv.get([128, NT_ALL, 2, 130], BF16)
            Ebuf = [cv.get([128, 1024], BF16) for _ in range(2)]
            ev_t1 = cv.get([128, 4, 128], F32)
            ev_o = cv.get([128, 4, 128], F32)
            ev_sq = cv.get([128, 128], F32)
            ev_st = cv.get([128, 16], F32)
            ev_ob = cv.get([128, 4, 128], BF16)
            ot_st = [cv.get([128, GT], BF16) for _ in range(2)]
            reset_state()
            tk_w = P.dma("gpsimd", lambda e: e.dma_start(out=B["W"][:, :, 0:256], in_=w_in_v[:, :, 1536 + h0 * 128:1536 + h0 * 128 + 256]), "D3")
            tk_ones = P.op("gpsimd", lambda e: e.memset(Vs[:, :, :, 128:129], 1.0))
            def proj_phase(T, src, ncw, g_t, cos_t, sin_t, dst, is_kv, cache):
                pf = {"proj": [None, None], "tr": None, "qb": [None, None], "xnT": [None, None]}
                info = {}
                def stageF(t):
                    b = state["n"] % 2
                    xnT = B["xnT"][b]
                    cp, _, b = x_front(B, src[t * 128:(t + 1) * 128, :], xnT, 128, pf["xnT"][b], defer_copy=True,
                                       save_to=(cache[t] if p == 0 else None), load_from=(cache[t] if p == 1 else None))
                    info[t] = {"b": b, "xnT": xnT, "cp": cp}
                def stageF2(t):
                    info[t]["t4"] = info[t]["cp"]()
                def stagePa(t):
                    d = info[t]; b = d["b"]; xnT = d["xnT"]
                    pk = pbank(2 + b)
                    tm = None
                    for c in range(8):
                        tm = P.op("tensor", lambda e, c=c, pk=pk, xnT=xnT: e.matmul(pk[:, 0:ncw], lhsT=xnT[:, c, :], rhs=B["W"][:, c, 0:ncw], start=(c == 0), stop=(c == 7)),
                                  waits=[d["t4"], tk_w, pf["proj"][b]] if c == 0 else [], sig=(c == 7))
                    pf["xnT"][b] = tm
                    d["tm"] = tm; d["pk"] = pk
                    d["tv"] = None
                def stagePb(t):
                    d = info[t]; pk = d["pk"]
                    if is_kv:
                        d["tv"] = P.op("scalar", lambda e, t=t, pk=pk: e.copy(out=Vs[:, t, :, 0:128], in_=pk[:, 256:512].rearrange("p (a b) -> p a b", b=128)), waits=[d["tm"]])
                def stageN1a(t):
                    d = info[t]; b = d["b"]
                    d["ta"] = qk_stats_a(B, d["pk"][:, 0:256], 256, [d["tm"], d["tv"]], b)
                def stageN1b(t):
                    d = info[t]; b = d["b"]
                    d["tr"] = qk_stats_b(B, 256, d["ta"], b)
                def stageN2(t):
                    d = info.pop(t); b = d["b"]; pk = d["pk"]
                    kb = B["qb"][b][:, 0:256]
                    tq = qk_apply(B, pk[:, 0:256], 256, g_t, cos_t[:, t, :], sin_t[:, t, :], kb, d["tr"], pf["qb"][b], b)
                    pf["proj"][b] = [tq, d["tv"]]
                    pkt = pbank_bf(4).rearrange("p (a b) -> p a b", b=128)[:, 0:2, :]
                    tt = None
                    for hl in range(2):
                        tt = P.op("tensor", lambda e, hl=hl, kb=kb: e.transpose(out=pkt[:, hl, :], in_=kb[:, hl * 128:(hl + 1) * 128], identity=ident[:]),
                                  waits=[tq, pf["tr"]] if hl == 0 else [], sig=(hl == 1))
                    pf["qb"][b] = tt
                    pf["tr"] = P.op("vector", lambda e, t=t: e.tensor_copy(out=dst[:, :, t * 128:(t + 1) * 128], in_=pkt), waits=[tt])
                for k in range(T + 2):
                    if 0 <= k - 1 < T:
                        stagePa(k - 1)
                    if k < T:
                        stageF(k)
                    if 0 <= k - 1 < T:
                        stagePb(k - 1)
                    if p == 0:
                        if 0 <= k - 2 < T:
                            stageN1a(k - 2); stageN1b(k - 2); stageN2(k - 2)
                    else:
                        if 0 <= k - 1 < T:
                            stageN1a(k - 1)
                        if 0 <= k - 2 < T:
                            stageN2(k - 2)
                        if 0 <= k - 1 < T:
                            stageN1b(k - 1)
                    if k < T:
                        stageF2(k)
                return pf["tr"]
            qtr_free = proj_phase(NOT_, xown, 256, gq_t, cosO_t, sinO_t, QT, False, cacheO)
            if DEBUG and p == 0:
                out_toks.append(P.dma("sync", lambda e: e.dma_start(out=dbg_qt, in_=QT[:].rearrange("p a b -> p (a b)")), "D6", waits=[qtr_free]))
            P.barrier()
            if STOP_AFTER == "O":
                break
            P.dma("gpsimd", lambda e: e.dma_start(out=B["W"][:, :, 0:256], in_=w_in_v[:, :, 2048 + h0 * 128:2048 + h0 * 128 + 256]), "D3")
            tk_w = P.dma("gpsimd", lambda e: e.dma_start(out=B["W"][:, :, 256:512], in_=w_in_v[:, :, 2560 + h0 * 128:2560 + h0 * 128 + 256]), "D3")
            reset_state()
            proj_phase(int(os.environ.get("HK_NTA", NT_ALL)), xall, 512, gk_t, cosA_t, sinA_t, KT, True, cacheA)
            if DEBUG and p == 0:
                P.barrier()
                out_toks.append(P.dma("sync", lambda e: e.dma_start(out=dbg_kt, in_=KT.rearrange("p a b -> p (a b)")), "D6"))
                out_toks.append(P.dma("sync", lambda e: e.dma_start(out=dbg_v, in_=Vs.rearrange("p a b c -> p (a b c)")), "D6"))
            P.barrier()
            if STOP_AFTER == "A":
                break
            accs = []
            for a in range(8):
                bk, r = divmod(a, 3)
                accs.append(psum[:, (4 + bk) * 512 + r * 132:(4 + bk) * 512 + r * 132 + 129])
            AST = {"S_free": [None, None], "E_free": [None, None], "acc_free": None, "otr_free": None, "n_ot": 0,
                   "ot_st_free": [None, None]}
            def emit_S_exp(i, hl, u, un):
                nkb = 8 * i + 8
                si = un % 2
                nk = 16 if u == 0 else 128
                ps = pbank(2 * si, 2)
                ts = None
                for m in range(2):
                    ts = P.op("tensor", lambda e, m=m, ps=ps, u=u, nk=nk, hl=hl, i=i: e.matmul(
                        ps[0:nk, m * 512:(m + 1) * 512], lhsT=KT[m * 64:(m + 1) * 64, hl, u * 128:u * 128 + nk],
                        rhs=QT[m * 64:(m + 1) * 64, hl, i * GT:(i + 1) * GT], start=True, stop=True),
                        waits=[AST["S_free"][si]] if m == 0 else [], sig=(m == 1))
                Eb = Ebuf[si]
                te = P.op("scalar", lambda e, ps=ps, Eb=Eb, nk=nk: e.activation(out=Eb[0:nk, :], in_=ps[0:nk, :], func=AF.Exp, scale=0.125),
                          waits=[ts, AST["E_free"][si]], noself=True)
                AST["S_free"][si] = te
                if u > nkb - 8:
                    r = u - 1 - (nkb - 8)
                    mi = (i % 2) * 8 + r
                    te = P.op("vector", lambda e, Eb=Eb, mi=mi: e.tensor_tensor(
                        out=Eb.rearrange("p (a b) -> p a b", b=512), in0=Eb.rearrange("p (a b) -> p a b", b=512),
                        in1=masks[:, mi, :].unsqueeze(1).broadcast_to([128, 2, 512]), op=ALU.mult), waits=[te])
                return {"i": i, "hl": hl, "u": u, "si": si, "nk": nk, "Eb": Eb, "te": te, "nkb": nkb}

            def emit_PV(d):
                i, hl, u, si, nk, Eb, te, nkb = d["i"], d["hl"], d["u"], d["si"], d["nk"], d["Eb"], d["te"], d["nkb"]
                tp = None
                for m in range(2):
                    for s_ in range(4):
                        a = m * 4 + s_
                        tp = P.op("tensor", lambda e, a=a, m=m, s_=s_, Eb=Eb, nk=nk, u=u, hl=hl, nkb=nkb: e.matmul(
                            accs[a], lhsT=Eb[0:nk, m * 512 + s_ * 128:m * 512 + (s_ + 1) * 128], rhs=Vs[0:nk, u, hl, 0:129],
                            start=(u == 0 and a % 3 == 0), stop=(u == nkb), skip_group_check=True),
                            waits=[te, AST["acc_free"] if u == 0 else None] if a == 0 else [], sig=(a == 7))
                AST["E_free"][si] = tp
                if u == nkb:
                    emit_evac(i, hl, tp)

            def emit_evac(i, hl, last_pv):
                h = 2 * p + hl
                st = ev_st
                acc_free = None
                for s in range(4):
                    a1, a2 = accs[s], accs[4 + s]
                    P.op("vector", lambda e, a1=a1, s=s: e.reciprocal(out=st[:, s:s + 1], in_=a1[:, 128:129]), waits=[last_pv] if s == 0 else [], sig=False)
                    P.op("vector", lambda e, a2=a2, s=s: e.reciprocal(out=st[:, 4 + s:5 + s], in_=a2[:, 128:129]), sig=False)
                    P.op("vector", lambda e, s=s: e.tensor_tensor(out=st[:, 4 + s:5 + s], in0=st[:, 4 + s:5 + s], in1=nlam[:], op=ALU.mult), sig=False)
                    P.op("vector", lambda e, a1=a1, s=s: e.tensor_scalar(out=ev_t1[:, s, :], in0=a1[:, 0:128], scalar1=st[:, s:s + 1], scalar2=None, op0=ALU.mult), sig=False)
                    acc_free = P.op("vector", lambda e, a2=a2, s=s: e.scalar_tensor_tensor(out=ev_o[:, s, :], in0=a2[:, 0:128], scalar=st[:, 4 + s:5 + s], in1=ev_t1[:, s, :],
                                                                                            op0=ALU.mult, op1=ALU.add), sig=False)
                AST["acc_free"] = acc_free
                tss = None
                for s in range(4):
                    P.op("vector", lambda e, s=s: e.tensor_tensor(out=ev_sq, in0=ev_o[:, s, :], in1=ev_o[:, s, :], op=ALU.mult), sig=False)
                    tss = P.op("vector", lambda e, s=s: e.reduce_sum(out=st[:, 8 + s:9 + s], in_=ev_sq, axis=AX.X))
                tsq = P.op("scalar", lambda e: e.activation(out=st[:, 8:12], in_=st[:, 8:12], func=AF.Sqrt, scale=1.0 / 128, bias=EPS_T[:, 0:1]), waits=[tss])
                P.op("vector", lambda e: e.reciprocal(out=st[:, 8:12], in_=st[:, 8:12]), waits=[tsq], sig=False)
                tob = None
                for s in range(4):
                    tob = P.op("vector", lambda e, s=s: e.scalar_tensor_tensor(out=ev_ob[:, s, :], in0=ev_o[:, s, :], scalar=st[:, 8 + s:9 + s], in1=gsub_t[:],
                                                                                op0=ALU.mult, op1=ALU.mult), waits=[AST["otr_free"]] if s == 0 else [])
                pot = pbank_bf(7).rearrange("p (a b) -> p a b", b=128)[:, 0:4, :]
                tt = None
                for s in range(4):
                    tt = P.op("tensor", lambda e, s=s: e.transpose(out=pot[:, s, :], in_=ev_ob[:, s, :], identity=ident[:]),
                              waits=[tob, AST["otr_free"]] if s == 0 else [], sig=(s == 3))
                n_ot = AST["n_ot"]
                osb = ot_st[n_ot % 2]
                otr = P.op("vector", lambda e, osb=osb: e.tensor_copy(out=osb, in_=pot.rearrange("p a b -> p (a b)")), waits=[tt, AST["ot_st_free"][n_ot % 2]])
                AST["otr_free"] = otr
                AST["ot_st_free"][n_ot % 2] = P.dma("sync", lambda e, osb=osb, h=h, i=i: e.dma_start(out=otbuf[:, h * NOWN + i * GT:h * NOWN + (i + 1) * GT], in_=osb),
                                                    f"D{33 + n_ot % 2}", waits=[otr])
                AST["n_ot"] = n_ot + 1

            units = [(i, hl, u) for i in range(NG) for hl in range(2) for u in range(8 * i + 9)]
            prev = None
            for un, (i, hl, u) in enumerate(units):
                d = emit_S_exp(i, hl, u, un)
                if prev is not None:
                    emit_PV(prev)
                prev = d
            emit_PV(prev)
            P.barrier()
        if STOP_AFTER not in ("O", "A", "B"):
            V = lambda fn, waits=(): P.op("vector", fn, waits)
            A = lambda fn, waits=(): P.op("scalar", fn, waits)
            T = lambda fn, waits=(), sig=True: P.op("tensor", fn, waits, sig)
            cv.reset()
            xt4 = cv.get([128, 4, D], F32)
            B = {}
            B["junk"] = cv.get([128, D], BF16)
            B["xn"] = [cv.get([128, D], BF16) for _ in range(2)]
            B["st"] = [cv.get([128, 16], F32) for _ in range(2)]
            hal_xt = cv.get([128, D], F32)
            xnTg = cv.get([128, 8, 516], BF16)
            Wc = cv.get([128, 8, 1536], BF16)
            Wo = cv.get([128, 8, D], BF16)
            g2_t = cv.get([128, D], F32)
            cc = cv.get([128, 516], F32)
            z = cv.get([128, 516], F32)
            yv = cv.get([128, 512], F32)
            mixc = cv.get([128, 4, 512], BF16)
            h1 = [cv.get([128, D], F32) for _ in range(2)]
            xn2 = [cv.get([128, D], BF16) for _ in range(2)]
            xn2T = cv.get([128, 8, 128], BF16)
            R = cv.get([128, 1024], F32)
            OTg = [cv.get([128, 4, GT], BF16) for _ in range(2)]
            otg_free = [None, None]
            st2 = cv.get([128, 8], F32)
            ohb = cv.get([128, 4, 32], BF16)
            reset_state()
            P.dma("gpsimd", lambda e: e.dma_start(out=Wc, in_=w_in_v[:, :, 0:1536]), "D16")
            P.dma("gpsimd", lambda e: e.dma_start(out=Wo, in_=w_out_v), "D16")
            tk_g2 = P.dma("sync", lambda e: e.dma_start(out=g2_t, in_=norm2_g.partition_broadcast(128)), "D37")
            tk_wc = [("D16", P.cnt["D16"]), tk_g2]
            ps5 = pbank(5)
            ps_rt = pbank_bf(2).rearrange("p (a b) -> p a b", b=128)
            rtc_free = None; xn2T_free = None
            xt4_tok = [None] * 4
            xt4_free = [None] * 4; hal_free = None; grp_free = None; conv_free = None
            ph_free = [None, None]; h1_free = [None, None]; xn2_free = [None, None]; rt_free = None
            nph = 0
            for i in range(NG):
                fr = []
                t4, t3, _ = x_front(B, xhalo[i * 128:(i + 1) * 128, :], xnTg[:, :, 0:2], 2, grp_free, xt=hal_xt, xt_free=hal_free, xt_sem="D11")
                hal_free = t3; fr.append(t4)
                for s in range(4):
                    xt4_tok[s] = P.dma("sync", lambda e, s=s, i=i: e.dma_start(out=xt4[:, s, :], in_=xown[(4 * i + s) * 128:(4 * i + s + 1) * 128, :]),
                                       f"D{7 + s}", waits=[xt4_free[s]])
                    t4, _, _ = x_front(B, None, xnTg[:, :, 2 + s * 128:2 + (s + 1) * 128], 128, grp_free, load_from=cacheO[4 * i + s], load_sem=f"D{42 + s}")
                    fr.append(t4)
                OTc = OTg[i % 2]
                tk_otg = P.dma("sync", lambda e, OTc=OTc, i=i: e.dma_start(out=OTc, in_=otbuf.rearrange("p (h t) -> p h t", h=4)[:, :, i * GT:(i + 1) * GT]),
                               f"D{35 + i % 2}", waits=[otg_free[i % 2]])
                tmix = None
                tm = None
                for q in range(4):
                    for (blkc, dstp, hcol) in ((q, pbank(2), None), (4 + q, pbank(3), 0), (8 + q, pbank(4), 2)):
                        for c in range(8):
                            tm = T(lambda e, c=c, blkc=blkc, dstp=dstp: e.matmul(dstp, lhsT=Wc[:, c, blkc * 128:(blkc + 1) * 128], rhs=xnTg[:, c, 2:514],
                                                                                 start=(c == 0), stop=(c == 7)),
                                   waits=fr + [tk_wc, conv_free, rt_free] if c == 0 else [], sig=(c == 7))
                        if hcol is not None:
                            for c in range(8):
                                tm = T(lambda e, c=c, blkc=blkc, hcol=hcol: e.matmul(ps5[:, hcol:hcol + 2], lhsT=Wc[:, c, blkc * 128:(blkc + 1) * 128], rhs=xnTg[:, c, 0:2],
                                                                                     start=(c == 0), stop=(c == 7)), sig=(c == 7))
                    A(lambda e: e.copy(out=cc[:, 2:514], in_=pbank(3)), waits=[tm])
                    ta = A(lambda e: e.copy(out=cc[:, 0:2], in_=ps5[:, 0:2]))
                    V(lambda e: e.tensor_tensor(out=z[:, 2:514], in0=cc[:, 2:514], in1=pbank(4), op=ALU.mult), waits=[ta, tm])
                    V(lambda e: e.tensor_tensor(out=z[:, 0:2], in0=cc[:, 0:2], in1=ps5[:, 2:4], op=ALU.mult))
                    V(lambda e, q=q: e.tensor_scalar(out=yv, in0=z[:, 0:512], scalar1=convw[:, q, 0:1], scalar2=None, op0=ALU.mult))
                    V(lambda e, q=q: e.scalar_tensor_tensor(out=yv, in0=z[:, 1:513], scalar=convw[:, q, 1:2], in1=yv, op0=ALU.mult, op1=ALU.add))
                    V(lambda e, q=q: e.scalar_tensor_tensor(out=yv, in0=z[:, 2:514], scalar=convw[:, q, 2:3], in1=yv, op0=ALU.mult, op1=ALU.add))
                    conv_free = V(lambda e, q=q: e.tensor_tensor(out=mixc[:, q, :], in0=pbank(2), in1=yv, op=ALU.mult), waits=[grp_free])
                tmix = conv_free
                grp_free = tm
                for s in range(4):
                    ot = 4 * i + s
                    par = ot % 2
                    hb = h1[par]; xb = xn2[par]
                    for hf in range(2):
                        ph = pbank(6 + nph % 2); pfree = ph_free[nph % 2]
                        tw = None
                        for kk in range(8):
                            lhsT = mixc[:, kk, s * 128:(s + 1) * 128] if kk < 4 else OTc[:, kk - 4, s * 128:(s + 1) * 128]
                            tw = T(lambda e, kk=kk, lhsT=lhsT, ph=ph, hf=hf: e.matmul(ph, lhsT=lhsT, rhs=Wo[:, kk, hf * 512:(hf + 1) * 512], start=(kk == 0), stop=(kk == 7)),
                                   waits=[tmix, pfree, tk_otg] if kk == 0 else [], sig=(kk == 7))
                        th = V(lambda e, hb=hb, ph=ph, s=s, hf=hf: e.tensor_tensor(out=hb[:, hf * 512:(hf + 1) * 512], in0=ph, in1=xt4[:, s, hf * 512:(hf + 1) * 512], op=ALU.add),
                               waits=[tw, h1_free[par], xt4_tok[s]])
                        ph_free[nph % 2] = th
                        nph += 1
                    xt4_free[s] = th
                    if s == 3:
                        otg_free[i % 2] = tw
                    A(lambda e, hb=hb: e.activation(out=B["junk"], in_=hb, func=AF.Square, accum_out=st2[:, 0:1]), waits=[th])
                    ta = A(lambda e: e.activation(out=st2[:, 1:2], in_=st2[:, 0:1], func=AF.Sqrt, scale=1.0 / D, bias=EPS_T[:, 0:1]))
                    V(lambda e: e.reciprocal(out=st2[:, 2:3], in_=st2[:, 1:2]), waits=[ta])
                    tx = V(lambda e, hb=hb, xb=xb: e.scalar_tensor_tensor(out=xb, in0=hb, scalar=st2[:, 2:3], in1=g2_t, op0=ALU.mult, op1=ALU.mult), waits=[xn2_free[par]])
                    d1 = P.dma("sync", lambda e, hb=hb, ot=ot: e.dma_start(out=h1buf[ot * 128:(ot + 1) * 128, :], in_=hb), f"D{12 + par}", waits=[th])
                    d2 = P.dma("sync", lambda e, xb=xb, ot=ot: e.dma_start(out=xn2buf[ot * 128:(ot + 1) * 128, :], in_=xb), f"D{14 + par}", waits=[tx])
                    h1_free[par] = [d1, tx]
                    tt = None
                    for c in range(8):
                        tt = T(lambda e, c=c, xb=xb: e.transpose(out=ps_rt[:, c, :], in_=xb[:, c * 128:(c + 1) * 128], identity=ident[:]),
                               waits=[tx, rtc_free, conv_free] if c == 0 else [], sig=(c == 7))
                    rtc_free = A(lambda e: e.copy(out=xn2T, in_=ps_rt), waits=[tt, xn2T_free])
                    xn2_free[par] = [d2, tt]
                    tl = None
                    for c in range(8):
                        tl = T(lambda e, c=c, s=s: e.matmul(ps5[:, 16 + 36 * s:52 + 36 * s], lhsT=xn2T[:, c, :], rhs=Wr[:, c, :], start=(c == 0), stop=(c == 7)),
                               waits=[rtc_free, rt_free] if c == 0 else [], sig=(c == 7))
                    xn2T_free = tl
                o4 = 4 * i
                def R3(a, n, k):
                    return R[:, a:a + 4 * k].rearrange("p (s k) -> p s k", k=k)
                def R2(a):
                    return R[:, a:a + 4]
                def bc(ap2, k):
                    return ap2.unsqueeze(2).broadcast_to([128, 4, k])
                LG = R3(0, 4, 36)
                V(lambda e: e.tensor_tensor(out=LG, in0=ps5[:, 16:160].rearrange("p (s k) -> p s k", k=36), in1=br_t[:].unsqueeze(1).broadcast_to([128, 4, 36]), op=ALU.add), waits=[tl])
                LGg = LG[:, :, 0:4]
                V(lambda e: e.tensor_reduce(out=R2(144), in_=LGg, axis=AX.X, op=ALU.max))
                V(lambda e: e.tensor_tensor(out=R3(148, 4, 4), in0=LGg, in1=bc(R2(144), 4), op=ALU.is_equal))
                tg = V(lambda e: e.tensor_tensor(out=R3(164, 4, 4), in0=LGg, in1=bc(R2(144), 4), op=ALU.subtract))
                tge = A(lambda e: e.activation(out=R[:, 180:196], in_=R[:, 164:180], func=AF.Exp), waits=[tg])
                V(lambda e: e.reduce_sum(out=R2(196), in_=R3(180, 4, 4), axis=AX.X), waits=[tge])
                V(lambda e: e.reciprocal(out=R2(200), in_=R2(196)))
                V(lambda e: e.tensor_tensor(out=R3(164, 4, 4), in0=R3(148, 4, 4), in1=iota_t[:, 0:4].unsqueeze(1).broadcast_to([128, 4, 4]), op=ALU.mult))
                V(lambda e: e.reduce_sum(out=R2(204), in_=R3(164, 4, 4), axis=AX.X))
                PR = R[:, 208:336].rearrange("p (s g j) -> p s g j", g=4, j=8)
                V(lambda e: e.tensor_tensor(out=PR, in0=LG[:, :, 4:36].rearrange("p s (g j) -> p s g j", j=8),
                                            in1=R3(148, 4, 4).unsqueeze(3).broadcast_to([128, 4, 4, 8]), op=ALU.mult))
                ES = R3(336, 4, 8)
                V(lambda e: e.reduce_sum(out=ES, in_=PR.rearrange("p s g j -> p s j g"), axis=AX.X))
                T8 = R3(368, 4, 8)
                for s in range(4):
                    V(lambda e, s=s: e.max(out=T8[:, s, :], in_=ES[:, s, :]))
                OH = [R3(400, 4, 8), R3(432, 4, 8)]
                for k in range(2):
                    V(lambda e, k=k: e.tensor_tensor(out=OH[k], in0=ES, in1=T8[:, :, k:k + 1].broadcast_to([128, 4, 8]), op=ALU.is_equal))
                    V(lambda e, k=k: e.tensor_tensor(out=R3(464, 4, 8), in0=OH[k], in1=iota_t[:, 0:8].unsqueeze(1).broadcast_to([128, 4, 8]), op=ALU.mult))
                    V(lambda e, k=k: e.reduce_sum(out=R2(496 + 4 * k), in_=R3(464, 4, 8), axis=AX.X))
                    V(lambda e, k=k: e.scalar_tensor_tensor(out=eid_all[:, o4:o4 + 4, k], in0=R2(204), scalar=8.0, in1=R2(496 + 4 * k), op0=ALU.mult, op1=ALU.add))
                td = V(lambda e: e.tensor_tensor(out=R2(504), in0=T8[:, :, 1], in1=T8[:, :, 0], op=ALU.subtract))
                tex = A(lambda e: e.activation(out=R2(508), in_=R2(504), func=AF.Exp), waits=[td])
                V(lambda e: e.tensor_scalar(out=R2(512), in0=R2(508), scalar1=1.0, scalar2=None, op0=ALU.add), waits=[tex])
                V(lambda e: e.reciprocal(out=R2(516), in_=R2(512)))
                V(lambda e: e.tensor_tensor(out=gate_all[:, o4:o4 + 4, 0], in0=R2(200), in1=R2(516), op=ALU.mult))
                V(lambda e: e.tensor_tensor(out=gate_all[:, o4:o4 + 4, 1], in0=R2(200), in1=gate_all[:, o4:o4 + 4, 0], op=ALU.subtract))
                O32 = [R3(520, 4, 32), R3(648, 4, 32)]
                for k in range(2):
                    V(lambda e, k=k: e.tensor_tensor(out=O32[k], in0=iota_t[:].unsqueeze(1).broadcast_to([128, 4, 32]),
                                                     in1=eid_all[:, o4:o4 + 4, k:k + 1].broadcast_to([128, 4, 32]), op=ALU.is_equal))
                toh = V(lambda e: e.tensor_tensor(out=ohb, in0=O32[0], in1=O32[1], op=ALU.add))
                tcn = None
                for s in range(4):
                    T(lambda e, s=s: e.matmul(ps5[:, 160 + 32 * s:192 + 32 * s], lhsT=Ubf[:], rhs=ohb[:, s, :], start=True, stop=True), waits=[toh] if s == 0 else [], sig=False)
                    tcn = T(lambda e, s=s: e.matmul(ps5[:, 288 + 32 * s:320 + 32 * s], lhsT=ones_bf[:], rhs=ohb[:, s, :], start=True, stop=True))
                BV = R3(776, 4, 32)
                V(lambda e: e.tensor_copy(out=BV[:, 0, :], in_=base[:]), waits=[tcn])
                for s in range(1, 4):
                    V(lambda e, s=s: e.tensor_tensor(out=BV[:, s, :], in0=BV[:, s - 1, :], in1=ps5[:, 288 + 32 * (s - 1):320 + 32 * (s - 1)], op=ALU.add))
                V(lambda e: e.tensor_tensor(out=base[:], in0=BV[:, 3, :], in1=ps5[:, 384:416], op=ALU.add))
                V(lambda e: e.tensor_tensor(out=BV, in0=BV, in1=ps5[:, 160:288].rearrange("p (s k) -> p s k", k=32), op=ALU.add))
                for k in range(2):
                    V(lambda e, k=k: e.tensor_tensor(out=O32[k], in0=O32[k], in1=BV, op=ALU.mult))
                    rt_free = V(lambda e, k=k: e.reduce_sum(out=rank_all[:, o4:o4 + 4, k], in_=O32[k], axis=AX.X))
            if DEBUG:
                V(lambda e: e.tensor_copy(out=R[:, 0:64], in_=eid_all[:].rearrange("p a b -> p (a b)")))
            cv.reset()
            cmpn = cv.get([128, 32, 32], F32)
            V(lambda e: e.tensor_tensor(out=cmpn, in0=base[:].unsqueeze(2).broadcast_to([128, 32, 32]),
                                        in1=thr_t[:, 0:32].unsqueeze(1).broadcast_to([128, 32, 32]), op=ALU.is_gt))
            V(lambda e: e.reduce_sum(out=pst[:, 3, :], in_=cmpn, axis=AX.X))
            V(lambda e: e.tensor_scalar(out=pst[:, 0, :], in0=pst[:, 3, :], scalar1=256.0, scalar2=None, op0=ALU.mult))
            V(lambda e: e.tensor_copy(out=pst[:, 1, 0:1], in_=pst[:, 0, 0:1]))
            for ee in range(1, 32):
                V(lambda e, ee=ee: e.tensor_tensor(out=pst[:, 1, ee:ee + 1], in0=pst[:, 1, ee - 1:ee], in1=pst[:, 0, ee:ee + 1], op=ALU.add))
            V(lambda e: e.tensor_tensor(out=pst[:, 2, :], in0=pst[:, 1, :], in1=pst[:, 0, :], op=ALU.subtract))
            cmp3 = cv.get([128, NBLK, 32], F32)
            V(lambda e: e.tensor_tensor(out=cmp3, in0=pst[:, 1, :].unsqueeze(1).broadcast_to([128, NBLK, 32]),
                                        in1=thr_t[:].unsqueeze(2).broadcast_to([128, NBLK, 32]), op=ALU.is_le))
            V(lambda e: e.reduce_sum(out=Ej_f[:], in_=cmp3, axis=AX.X))
            V(lambda e: e.tensor_scalar(out=Ej_f[:], in0=Ej_f[:], scalar1=31.0, scalar2=None, op0=ALU.min))
            tk_ej = V(lambda e: e.tensor_copy(out=Ej_i[:], in_=Ej_f[:]))
            unused = cv.get([128, NBLK], F32)
            V(lambda e: e.tensor_scalar(out=unused, in0=thr_t[:], scalar1=pst[:, 1, 31:32], scalar2=None, op0=ALU.is_ge))
            V(lambda e: e.scalar_tensor_tensor(out=Ej_f[:], in0=unused, scalar=8192.0, in1=Ej_f[:], op0=ALU.mult, op1=ALU.add))
            idxW_f = cv.get([128, NBLK, 12], F32)
            for c in range(12):
                V(lambda e, c=c: e.tensor_scalar(out=idxW_f[:, :, c], in0=Ej_f[:], scalar1=(128.0 if c < 8 else 512.0), scalar2=pidx_t[:, c:c + 1],
                                                 op0=ALU.mult, op1=ALU.add))
            tk_ej = V(lambda e: e.tensor_copy(out=idxW_i[:], in_=idxW_f))
            for ot in range(NOT_):
                for k in range(2):
                    V(lambda e, ot=ot, k=k: e.tensor_scalar(out=R[:, 96:128], in0=iota_t[:], scalar1=eid_all[:, ot, k:k + 1], scalar2=None, op0=ALU.is_equal))
                    V(lambda e: e.tensor_tensor(out=R[:, 96:128], in0=R[:, 96:128], in1=pst[:, 2, :], op=ALU.mult))
                    V(lambda e: e.reduce_sum(out=R[:, 240:241], in_=R[:, 96:128], axis=AX.X))
                    V(lambda e, ot=ot, k=k: e.tensor_tensor(out=slot_f[:, ot, k:k + 1], in0=R[:, 240:241], in1=rank_all[:, ot, k:k + 1], op=ALU.add))
            tk_slot = V(lambda e: e.tensor_copy(out=slot_i[:], in_=slot_f[:]))
            if DEBUG:
                V(lambda e: e.tensor_copy(out=R[:, 64:128], in_=slot_f[:].rearrange("p a b -> p (a b)")))
                V(lambda e: e.tensor_copy(out=R[:, 128:192], in_=gate_all[:].rearrange("p a b -> p (a b)")))
                V(lambda e: e.tensor_copy(out=R[:, 192:256], in_=Ej_f[:]))
                out_toks.append(P.dma("sync", lambda e: e.dma_start(out=dbg_rt[:, 0:256], in_=R), "D6", waits=[P.last["vector"]]))
            P.barrier()
            cv.reset()
            NXS = 4
            xs = [cv.get([128, D], BF16) for _ in range(NXS)]
            xs_free = [None] * NXS
            xs_sem = ["D17", "D18", "D38", "D39"]
            sc_sem = ["D19", "D20", "D46", "D47"]
            for ot in range(NOT_):
                par = ot % NXS
                tl = P.dma("sync", lambda e, ot=ot, par=par: e.dma_start(out=xs[par], in_=xn2buf[ot * 128:(ot + 1) * 128, :]), xs_sem[par], waits=[xs_free[par]])
                tsc = None
                for k in range(2):
                    tsc = P.dma("gpsimd", lambda e, ot=ot, k=k, par=par: e.indirect_dma_start(
                        out=xebuf, out_offset=bass.IndirectOffsetOnAxis(ap=slot_i[:, ot, k:k + 1], axis=0), in_=xs[par], in_offset=None,
                        bounds_check=breg(e, NSLOT - 1), oob_is_err=False), sc_sem[par], waits=[tl])
                xs_free[par] = tsc
            P.barrier()
            if STOP_AFTER != "C":
                cv.reset()
                Wg = [cv.get([128, 8, 512], BF16) for _ in range(2)]
                Wu = [cv.get([128, 8, 512], BF16) for _ in range(2)]
                Wd = [cv.get([128, 4, D], BF16) for _ in range(2)]
                xe = [cv.get([128, 2, D], BF16) for _ in range(2)]
                xeT = cv.get([128, 8, 256], BF16)
                sg = [cv.get([128, 256], F32) for _ in range(2)]
                hidT = cv.get([128, 4, 256], BF16)
                ysb = [cv.get([128, D], F32) for _ in range(2)]
                w_free = [None, None]; xe_free = [None, None]; xeT_free = None; pxe_free = None
                sg_free = [None, None]; pgu_free = [None, None]; hid_free = None; py_free = [None, None]; ysb_free = [None, None]
                ny = 0
                wg_v = w_gate.rearrange("e (c p) n -> p e c n", p=128)
                wu_v = w_up.rearrange("e (c p) n -> p e c n", p=128)
                wd_v = w_down.rearrange("e (c p) n -> p e c n", p=128)
                wg_rows = w_gate.rearrange("e (p c) n -> (e p) (c n)", c=8)
                wu_rows = w_up.rearrange("e (p c) n -> (e p) (c n)", c=8)
                wd_rows = w_down.rearrange("e f n -> (e f) n")
                def emit_wload(j):
                    par = j % 2
                    tok = P.dma("gpsimd", lambda e: e.indirect_dma_start(
                        out=Wg[par].rearrange("p c n -> p (c n)"), out_offset=None, in_=wg_rows, in_offset=bass.IndirectOffsetOnAxis(ap=idxW_i[:, j, 0:1], axis=0),
                        bounds_check=breg(e, NE * 128 - 1), oob_is_err=False), f"D{21 + par}", waits=[w_free[par], tk_ej])
                    tok = P.dma("gpsimd", lambda e: e.indirect_dma_start(
                        out=Wu[par].rearrange("p c n -> p (c n)"), out_offset=None, in_=wu_rows, in_offset=bass.IndirectOffsetOnAxis(ap=idxW_i[:, j, 0:1], axis=0),
                        bounds_check=breg(e, NE * 128 - 1), oob_is_err=False), f"D{21 + par}")
                    for c in range(4):
                        tok = P.dma("gpsimd", lambda e, c=c: e.indirect_dma_start(
                            out=Wd[par][:, c, :], out_offset=None, in_=wd_rows, in_offset=bass.IndirectOffsetOnAxis(ap=idxW_i[:, j, 8 + c:9 + c], axis=0),
                            bounds_check=breg(e, NE * 512 - 1), oob_is_err=False), f"D{21 + par}")
                    return tok
                def emit_xload(j):
                    par = j % 2
                    return P.dma("sync", lambda e: e.dma_start(out=xe[par], in_=xebuf[j * BLK:(j + 1) * BLK, :].rearrange("(t p) d -> p t d", p=128)),
                                 f"D{23 + par}", waits=[xe_free[par]])
                tkw = {0: emit_wload(0)}; tkx = {0: emit_xload(0)}
                ptrx = pbank_bf(0, 2).rearrange("p (a b) -> p a b", b=256)
                for j in range(NBLK):
                    par = j % 2
                    if j + 1 < NBLK:
                        tkw[j + 1] = emit_wload(j + 1); tkx[j + 1] = emit_xload(j + 1)
                    tt = None
                    for t2 in range(2):
                        for c in range(8):
                            tt = T(lambda e, t2=t2, c=c, par=par: e.transpose(out=ptrx[:, c, t2 * 128:(t2 + 1) * 128], in_=xe[par][:, t2, c::8], identity=ident[:]),
                                   waits=[tkx[j], pxe_free] if (t2 == 0 and c == 0) else [], sig=(t2 == 1 and c == 7))
                    xe_free[par] = tt
                    pxe_free = A(lambda e: e.copy(out=xeT, in_=ptrx), waits=[tt, xeT_free])
                    for fc in range(4):
                        pp = fc % 2
                        pgu = pbank(2 + pp)
                        tg = None
                        for (Wsrc, c0) in ((Wg[par], 0), (Wu[par], 256)):
                            for c in range(8):
                                tg = T(lambda e, c=c, Wsrc=Wsrc, c0=c0, pgu=pgu, fc=fc: e.matmul(pgu[:, c0:c0 + 256], lhsT=Wsrc[:, c, fc * 128:(fc + 1) * 128], rhs=xeT[:, c, :],
                                                                                                 start=(c == 0), stop=(c == 7)),
                                       waits=[pxe_free, tkw[j], pgu_free[pp]] if (c == 0 and c0 == 0) else [], sig=(c == 7))
                        tsg = A(lambda e, pgu=pgu, pp=pp: e.activation(out=sg[pp], in_=pgu[:, 0:256], func=AF.Silu), waits=[tg, sg_free[pp]])
                        th = V(lambda e, pgu=pgu, pp=pp, fc=fc: e.tensor_tensor(out=hidT[:, fc, :], in0=sg[pp], in1=pgu[:, 256:512], op=ALU.mult), waits=[tsg, hid_free] if fc == 0 else [tsg])
                        sg_free[pp] = th; pgu_free[pp] = th
                    xeT_free = tg
                    tyl = None
                    for t2 in range(2):
                        yb = ysb[t2]
                        tcs = []
                        for hf in range(2):
                            py = pbank(4 + ny % 2)
                            ty = None
                            for fc in range(4):
                                ty = T(lambda e, fc=fc, t2=t2, hf=hf, py=py, par=par: e.matmul(py, lhsT=hidT[:, fc, t2 * 128:(t2 + 1) * 128], rhs=Wd[par][:, fc, hf * 512:(hf + 1) * 512],
                                                                                       start=(fc == 0), stop=(fc == 3)),
                                       waits=[th, py_free[ny % 2]] if fc == 0 else [], sig=(fc == 3))
                            tcp = A(lambda e, yb=yb, py=py, hf=hf: e.copy(out=yb[:, hf * 512:(hf + 1) * 512], in_=py), waits=[ty, ysb_free[t2]])
                            py_free[ny % 2] = tcp
                            tcs.append(tcp)
                            ny += 1
                            tyl = ty
                        ysb_free[t2] = P.dma("sync", lambda e, yb=yb, j=j, t2=t2: e.dma_start(out=ybuf[j * BLK + t2 * 128:j * BLK + (t2 + 1) * 128, :], in_=yb),
                                             f"D{25 + t2}", waits=tcs)
                    hid_free = tyl
                    w_free[par] = tyl
                P.barrier()
                cv.reset()
                NCB = 4
                y0 = [cv.get([128, D], F32) for _ in range(NCB)]
                y1 = [cv.get([128, D], F32) for _ in range(NCB)]
                hc = [cv.get([128, D], F32) for _ in range(NCB)]
                cb_free = [None] * NCB
                g_sem = ["D27", "D28", "D19", "D20"]
                h_sem = ["D29", "D30", "D23", "D24"]
                o_sem = ["D31", "D32", "D25", "D26"]
                for ot in range(NOT_):
                    par = ot % NCB
                    tg0 = P.dma("gpsimd", lambda e, ot=ot, par=par: e.indirect_dma_start(
                        out=y0[par], out_offset=None, in_=ybuf, in_offset=bass.IndirectOffsetOnAxis(ap=slot_i[:, ot, 0:1], axis=0),
                        bounds_check=breg(e, NSLOT - 1), oob_is_err=False), g_sem[par], waits=[cb_free[par]])
                    tg1 = P.dma("gpsimd", lambda e, ot=ot, par=par: e.indirect_dma_start(
                        out=y1[par], out_offset=None, in_=ybuf, in_offset=bass.IndirectOffsetOnAxis(ap=slot_i[:, ot, 1:2], axis=0),
                        bounds_check=breg(e, NSLOT - 1), oob_is_err=False), g_sem[par])
                    tlh = P.dma("sync", lambda e, ot=ot, par=par: e.dma_start(out=hc[par], in_=h1buf[ot * 128:(ot + 1) * 128, :]), h_sem[par], waits=[cb_free[par]])
                    V(lambda e, ot=ot, par=par: e.scalar_tensor_tensor(out=hc[par], in0=y0[par], scalar=gate_all[:, ot, 0:1], in1=hc[par], op0=ALU.mult, op1=ALU.add), waits=[tg1, tlh])
                    tv = V(lambda e, ot=ot, par=par: e.scalar_tensor_tensor(out=hc[par], in0=y1[par], scalar=gate_all[:, ot, 1:2], in1=hc[par], op0=ALU.mult, op1=ALU.add))
                    cb_free[par] = P.dma("sync", lambda e, ot=ot, par=par: e.dma_start(out=out_d[ot * 128:(ot + 1) * 128, :], in_=hc[par]), o_sem[par], waits=[tv])
        P.barrier()
        P.ops["sync"].append((None, P._w("sync", []), None, 0))

        with nc.Block() as blk:
            def mk(engname):
                def body(e):
                    waited = {}
                    for fn, waits, sig, inc in P.ops[engname]:
                        for wv in waits:
                            k, v = wv[0], wv[1]
                            if P.owner.get(k) == engname and len(wv) == 2 and engname == "tensor":
                                continue
                            if waited.get(k, 0) >= v:
                                continue
                            e.wait_ge(P.sems[k], v)
                            waited[k] = v
                        if fn is None:
                            continue
                        ins = fn(e)
                        if sig is not None:
                            ins.then_inc(P.sems[sig], inc)
                return body
            blk.sync(mk("sync"))
            blk.scalar(mk("scalar"))
            blk.vector(mk("vector"))
            blk.gpsimd(mk("gpsimd"))
            blk.tensor(mk("tensor"))
    return nc


def _rope_tables(pos):
    inv = (500000.0 ** (-np.arange(0, 16, 2, dtype=np.float32) / np.float32(16))).astype(np.float32)
    ang = pos.astype(np.float32)[:, None] * inv[None, :]
    return np.cos(ang).astype(np.float32), np.sin(ang).astype(np.float32)


def _masks(variant_large):
    m = np.zeros((8, 128, 512), np.float32)
    tri = (np.arange(128)[:, None] <= np.arange(128)[None, :]).astype(np.float32)
    for r in range(8):
        for s in range(4):
            if variant_large:
                if r < 4: v = 1.0
                elif s > r - 4: v = 1.0
                elif s == r - 4: v = tri
                else: v = 0.0
            else:
                if r >= 4: v = 0.0
                elif s > r: v = 1.0
                elif s == r: v = tri
                else: v = 0.0
            m[r, :, s * 128:(s + 1) * 128] = v
    return m


_NC_CACHE = {}


def kernel(x, meta_tokens, norm1_g, w_in, conv_w, q_norm_g, k_norm_g, lambda_q1, lambda_k1, lambda_q2, lambda_k2,
           subln_g, w_out, norm2_g, w_router_group, b_router_group, w_router_expert, b_router_expert,
           w_gate, w_up, w_down):
    x = np.asarray(x, np.float32)
    f = lambda a: np.ascontiguousarray(np.asarray(a, np.float32))
    if "nc" not in _NC_CACHE:
        _NC_CACHE["nc"] = build()
    nc = _NC_CACHE["nc"]
    meta = f(meta_tokens)
    w_r = np.ascontiguousarray(np.concatenate([f(w_router_group)[0], f(w_router_expert)[0]], axis=1))
    b_r = np.ascontiguousarray(np.concatenate([f(b_router_group)[0], f(b_router_expert)[0]], axis=0)[None, :])
    shared = {
        "norm1_g": f(norm1_g), "norm2_g": f(norm2_g), "w_in": f(w_in)[0], "w_out": f(w_out)[0],
        "conv_wT": np.ascontiguousarray(f(conv_w)[0].T), "q_norm_g": f(q_norm_g), "k_norm_g": f(k_norm_g),
        "lambda_q1": f(lambda_q1), "lambda_k1": f(lambda_k1), "lambda_q2": f(lambda_q2), "lambda_k2": f(lambda_k2),
        "subln_g": f(subln_g), "w_r": w_r, "b_r": b_r,
        "w_gate": f(w_gate)[0], "w_up": f(w_up)[0], "w_down": f(w_down)[0],
        "thr": (np.arange(NBLK, dtype=np.float32) * BLK)[None, :],
        "iota": np.arange(32, dtype=np.float32)[None, :],
        "pidx": np.ascontiguousarray(np.concatenate([np.arange(8)[None, :] * 128 + np.arange(128)[:, None],
                                                     np.arange(4)[None, :] * 128 + np.arange(128)[:, None]], 1).astype(np.float32)),
    }
    posA = np.zeros((NT_ALL, 128), np.float32)
    posA[0, :16] = np.arange(16)
    posA[1:] = 16 + np.arange(SEQ).reshape(64, 128)
    cA, sA = _rope_tables(posA.reshape(-1))
    cosA = np.ascontiguousarray(cA.reshape(NT_ALL, 128, 8).transpose(1, 0, 2).reshape(128, -1))
    sinA = np.ascontiguousarray(sA.reshape(NT_ALL, 128, 8).transpose(1, 0, 2).reshape(128, -1))
    mk_l = _masks(True); mk_s = _masks(False)
    in_maps = []
    for c in range(NCORES):
        b, hf = divmod(c, 2)
        xa = np.zeros((NT_ALL * 128, D), np.float32)
        xa[:NMETA] = meta
        xa[128:] = x[b]
        gl = GROUPS[hf]
        xo = np.concatenate([x[b, g * GT:(g + 1) * GT] for g in gl], 0)
        hal = np.zeros((NG * 128, D), np.float32)
        for i, g in enumerate(gl):
            hal[128 * i:128 * i + 2] = meta[14:16] if g == 0 else x[b, g * GT - 2:g * GT]
        posO = np.concatenate([16 + g * GT + np.arange(GT) for g in gl]).astype(np.float32)
        cO, sO = _rope_tables(posO)
        cosO = np.ascontiguousarray(cO.reshape(NOT_, 128, 8).transpose(1, 0, 2).reshape(128, -1))
        sinO = np.ascontiguousarray(sO.reshape(NOT_, 128, 8).transpose(1, 0, 2).reshape(128, -1))
        mm = np.stack([mk_s if hf == 0 else mk_l, mk_l if hf == 0 else mk_s], 0)
        mm = np.ascontiguousarray(mm.reshape(16, 128, 512).transpose(1, 0, 2).reshape(128, -1)).astype(ml_dtypes.bfloat16)
        d = dict(shared)
        d.update({"xall": xa, "xown": np.ascontiguousarray(xo), "xhalo": hal, "cosA": cosA, "sinA": sinA,
                  "cosO": cosO, "sinO": sinO, "masks": mm})
        in_maps.append(d)
    kernel.last_in_maps = in_maps
    if os.environ.get("HK_NORUN") == "1":
        return None
    res = run_bass_kernel_spmd(nc, in_maps, core_ids=list(range(NCORES)))
    kernel.last_results = res.results
    out = np.zeros((4, SEQ, D), np.float32)
    for c in range(NCORES):
        b, hf = divmod(c, 2)
        o = res.results[c]["out"]
        for i, g in enumerate(GROUPS[hf]):
            out[b, g * GT:(g + 1) * GT] = o[i * GT:(i + 1) * GT]
    return out
```

```python
import os
from contextlib import ExitStack
import numpy as np
import ml_dtypes
import concourse.bass as bass
import concourse.mybir as mybir
from concourse.bass_utils import run_bass_kernel_spmd

F32 = mybir.dt.float32
BF16 = mybir.dt.bfloat16
I32 = mybir.dt.int32
ALU = mybir.AluOpType
AF = mybir.ActivationFunctionType
AX = mybir.AxisListType

NCORES = 8
D = 1024
SEQ = 8192
NMETA = 16
NT_ALL = 65
NG = 8
GT = 512
NOWN = NG * GT
NOT_ = NOWN // 128
EPS = 1e-6
NE = 32
NBLK = 64
BLK = 256
NSLOT = NBLK * BLK
GROUPS = [[0, 3, 4, 7, 8, 11, 12, 15], [1, 2, 5, 6, 9, 10, 13, 14]]

STOP_AFTER = os.environ.get("HK_STOP", "")
DEBUG = os.environ.get("HK_DEBUG", "") == "1"
STATIC_E = os.environ.get("HK_STATIC_E", "") == "1"


class Prog:
    ENGS = ["sync", "scalar", "vector", "gpsimd", "tensor"]

    def __init__(self, nc, es):
        self.nc, self.es = nc, es
        self.ops = {e: [] for e in self.ENGS}
        self.sems, self.cnt, self.owner = {}, {}, {}
        for e in self.ENGS[1:]:
            self.newsem("E_" + e, e)
        self.last = {e: None for e in self.ENGS}
        self.pending = {e: [] for e in self.ENGS}

    def barrier(self):
        toks = [self.last[e] for e in self.ENGS[1:] if self.last[e] is not None]
        toks += [(k, v) for k, v in self.cnt.items() if self.owner.get(k) is None and v > 0]
        for e in self.ENGS:
            self.pending[e] = list(toks)

    def _w(self, eng, waits):
        w = self.pending[eng] + flat(list(waits))
        self.pending[eng] = []
        return w

    def newsem(self, name, owner=None):
        self.sems[name] = self.es.enter_context(self.nc.semaphore(name))
        self.cnt[name] = 0
        self.owner[name] = owner
        return name

    SERIAL = ("scalar", "vector", "gpsimd")

    def op(self, eng, fn, waits=(), sig=True, noself=False):
        tok = None
        name = None
        w = self._w(eng, waits)
        if eng in self.SERIAL:
            sig = True
            if self.cnt["E_" + eng] > 0 and not noself:
                w = w + [("E_" + eng, self.cnt["E_" + eng], "self")]
        if sig:
            name = "E_" + eng
            self.cnt[name] += 1
            tok = (name, self.cnt[name])
        self.ops[eng].append((freeze(fn), w, name, 1))
        if tok is not None:
            self.last[eng] = tok
        return tok

    def dma(self, eng, fn, sem, waits=()):
        self.cnt[sem] += 16
        tok = (sem, self.cnt[sem])
        self.ops[eng].append((freeze(fn), self._w(eng, waits), sem, 16))
        return tok

    def replay(self, eng, e):
        waited = {}
        for fn, waits, sig, inc in self.ops[eng]:
            for (k, v) in waits:
                if self.owner.get(k) == eng:
                    continue
                if waited.get(k, 0) >= v:
                    continue
                e.wait_ge(self.sems[k], v)
                waited[k] = v
            ins = fn(e)
            if sig is not None:
                ins.then_inc(self.sems[sig], inc)


import types


def freeze(fn):
    if fn is None or fn.__closure__ is None:
        return fn
    cells = []
    for c in fn.__closure__:
        try:
            cells.append(types.CellType(c.cell_contents))
        except ValueError:
            cells.append(c)
    return types.FunctionType(fn.__code__, fn.__globals__, fn.__name__, fn.__defaults__, tuple(cells))


def flat(toks):
    out = []
    for t in toks:
        if t is None:
            continue
        if isinstance(t, list):
            out.extend(flat(t))
        else:
            out.append(t)
    return out


def build():
    nc = bass.Bass("TRN2", target_bir_lowering=False)
    dt_in = lambda n, s, d=F32: nc.dram_tensor(n, s, d, kind="ExternalInput").ap()
    xall = dt_in("xall", [NT_ALL * 128, D])
    xown = dt_in("xown", [NOWN, D])
    xhalo = dt_in("xhalo", [NG * 128, D])
    cosA = dt_in("cosA", [128, NT_ALL * 8]); sinA = dt_in("sinA", [128, NT_ALL * 8])
    cosO = dt_in("cosO", [128, NOT_ * 8]); sinO = dt_in("sinO", [128, NOT_ * 8])
    masks_d = dt_in("masks", [128, 16 * 512], BF16)
    thr_d = dt_in("thr", [1, NBLK]); iota_d = dt_in("iota", [1, 32]); pidx_d = dt_in("pidx", [128, 12])
    norm1_g = dt_in("norm1_g", [1, D]); norm2_g = dt_in("norm2_g", [1, D])
    w_in = dt_in("w_in", [D, 3072]); w_out = dt_in("w_out", [D, D])
    conv_wT = dt_in("conv_wT", [512, 3])
    q_g = dt_in("q_norm_g", [1, 64]); k_g = dt_in("k_norm_g", [1, 64])
    lq1 = dt_in("lambda_q1", [1, 64]); lk1 = dt_in("lambda_k1", [1, 64])
    lq2 = dt_in("lambda_q2", [1, 64]); lk2 = dt_in("lambda_k2", [1, 64])
    subln_g = dt_in("subln_g", [1, 128])
    w_r = dt_in("w_r", [D, 36]); b_r = dt_in("b_r", [1, 36])
    w_gate = dt_in("w_gate", [NE, D, 512]); w_up = dt_in("w_up", [NE, D, 512]); w_down = dt_in("w_down", [NE, 512, D])
    out_d = nc.dram_tensor("out", [NOWN, D], F32, kind="ExternalOutput").ap()
    scr_kind = "ExternalOutput" if DEBUG else "Internal"
    h1buf = nc.dram_tensor("h1buf", [NOWN, D], F32, kind=scr_kind).ap()
    xn2buf = nc.dram_tensor("xn2buf", [NOWN, D], BF16, kind="Internal").ap()
    otbuf = nc.dram_tensor("otbuf", [128, 4 * NOWN], BF16, kind=scr_kind).ap()
    cacheA = nc.dram_tensor("cacheA", [NT_ALL, 128, D], BF16, kind="Internal").ap()
    cacheO = nc.dram_tensor("cacheO", [NOT_, 128, D], BF16, kind="Internal").ap()
    xebuf = nc.dram_tensor("xebuf", [NSLOT, D], BF16, kind="Internal").ap()
    ybuf = nc.dram_tensor("ybuf", [NSLOT, D], F32, kind="Internal").ap()
    if DEBUG:
        dbg_qt = nc.dram_tensor("dbg_qt", [128, 2 * NOWN], BF16, kind="ExternalOutput").ap()
        dbg_kt = nc.dram_tensor("dbg_kt", [128, 2 * NT_ALL * 128], BF16, kind="ExternalOutput").ap()
        dbg_v = nc.dram_tensor("dbg_v", [128, NT_ALL * 2 * 130], BF16, kind="ExternalOutput").ap()
        dbg_rt = nc.dram_tensor("dbg_rt", [128, 1024], F32, kind="ExternalOutput").ap()

    with ExitStack() as es:
        def sb(name, shape, dt):
            return es.enter_context(nc.sbuf_tensor("s_" + name, shape, dt))
        P = Prog(nc, es)
        for i in range(48):
            P.newsem(f"D{i}")
        ident = sb("ident", [128, 128], BF16)
        Ubf = sb("Ubf", [128, 128], BF16)
        ones_bf = sb("ones_bf", [128, 128], BF16)
        g1_t = sb("g1_t", [128, D], F32)
        gq_t = sb("gq_t", [128, 64], F32)
        gk_t = sb("gk_t", [128, 64], F32)
        gsub_t = sb("gsub_t", [128, 128], F32)
        lam_w = sb("lam_w", [128, 8], F32)
        nlam = sb("nlam", [128, 1], F32)
        EPS_T = sb("EPS_T", [128, 1], F32)
        masks = sb("masks", [128, 16, 512], BF16)
        cosA_t = sb("cosA_t", [128, NT_ALL, 8], F32); sinA_t = sb("sinA_t", [128, NT_ALL, 8], F32)
        cosO_t = sb("cosO_t", [128, NOT_, 8], F32); sinO_t = sb("sinO_t", [128, NOT_, 8], F32)
        Wr = sb("Wr", [128, 8, 36], BF16)
        br_t = sb("br_t", [128, 36], F32)
        convw = sb("convw", [128, 4, 3], F32)
        thr_t = sb("thr_t", [128, NBLK], F32)
        iota_t = sb("iota_t", [128, 32], F32)
        QT = sb("QT", [128, 2, NOWN], BF16)
        eid_all = sb("eid_all", [128, NOT_, 2], F32)
        rank_all = sb("rank_all", [128, NOT_, 2], F32)
        gate_all = sb("gate_all", [128, NOT_, 2], F32)
        slot_f = sb("slot_f", [128, NOT_, 2], F32)
        slot_i = sb("slot_i", [128, NOT_, 2], I32)
        base = sb("base", [128, 32], F32)
        pst = sb("pst", [128, 4, 32], F32)
        pst_i = sb("pst_i", [128, 32], I32)
        Ej_f = sb("Ej_f", [128, NBLK], F32)
        Ej_i = sb("Ej_i", [128, NBLK], I32)
        pidx_t = sb("pidx_t", [128, 12], F32)
        idxW_i = sb("idxW_i", [128, NBLK, 12], I32)
        regs = {}
        def breg(e, bound):
            key = (id(e), bound)
            if key not in regs:
                regs[key] = e.to_reg(bound)
            return regs[key]
        ARENA_B = 118 * 1024
        arena = sb("arena", [128, ARENA_B // 2], BF16)

        class Carver:
            def __init__(self):
                self.off = 0
            def reset(self):
                self.off = 0
            def get(self, shape, dt):
                n = int(np.prod(shape[1:]))
                nb = n * (4 if dt in (F32, I32) else 2)
                nb_al = (nb + 63) // 64 * 64
                a = arena[:, self.off // 2:(self.off + nb) // 2]
                self.off += nb_al
                assert self.off <= ARENA_B, (self.off, ARENA_B)
                if dt != BF16:
                    a = a.bitcast(dt)
                if len(shape) == 3:
                    a = a.rearrange("p (a b) -> p a b", b=shape[2])
                elif len(shape) == 4:
                    a = a.rearrange("p (a b c) -> p a b c", b=shape[2], c=shape[3])
                return a
        cv = Carver()

        psum = es.enter_context(nc.psum_tensor("psum", [128, 4096], F32))
        def pbank(b, nb=1):
            return psum[:, b * 512:(b + nb) * 512]
        def pbank_bf(b, nb=1):
            return psum[:, b * 512:(b + nb) * 512].bitcast(BF16)

        w_in_v = w_in.rearrange("(c p) n -> p c n", p=128)
        w_out_v = w_out.rearrange("(c p) n -> p c n", p=128)
        out_toks = []

        cv.reset()
        ident_f = cv.get([128, 128], F32)
        U_f = cv.get([128, 128], F32)
        lam_in = cv.get([128, 4, 64], F32)
        ztile = cv.get([128, 1024], BF16)
        P.op("gpsimd", lambda e: e.memset(ident_f, 0.0), sig=False)
        P.op("gpsimd", lambda e: e.affine_select(out=ident_f, in_=ident_f, pattern=[[-1, 128]], compare_op=ALU.not_equal,
                                                  fill=1.0, base=0, channel_multiplier=1), sig=False)
        P.op("gpsimd", lambda e: e.memset(U_f, 1.0), sig=False)
        P.op("gpsimd", lambda e: e.affine_select(out=U_f, in_=U_f, pattern=[[1, 128]], compare_op=ALU.is_gt,
                                                  fill=0.0, base=0, channel_multiplier=-1), sig=False)
        P.op("gpsimd", lambda e: e.tensor_copy(out=ident[:], in_=ident_f), sig=False)
        P.op("gpsimd", lambda e: e.tensor_copy(out=Ubf[:], in_=U_f), sig=False)
        P.op("gpsimd", lambda e: e.memset(base[:], 0.0), sig=False)
        P.op("gpsimd", lambda e: e.memset(EPS_T[:], EPS), sig=False)
        P.op("gpsimd", lambda e: e.memset(ones_bf[:], 1.0), sig=False)
        tkz = P.op("gpsimd", lambda e: e.memset(ztile, 0.0))
        def cdma(out, in_):
            P.dma("sync", lambda e, o=out, i=in_: e.dma_start(out=o, in_=i), "D0")
        cdma(g1_t[:], norm1_g.partition_broadcast(128))
        cdma(gq_t[:], q_g.partition_broadcast(128))
        cdma(gk_t[:], k_g.partition_broadcast(128))
        cdma(gsub_t[:], subln_g.partition_broadcast(128))
        cdma(lam_in[:, 0, :], lq1.partition_broadcast(128))
        cdma(lam_in[:, 1, :], lk1.partition_broadcast(128))
        cdma(lam_in[:, 2, :], lq2.partition_broadcast(128))
        cdma(lam_in[:, 3, :], lk2.partition_broadcast(128))
        cdma(masks[:], masks_d.rearrange("p (a b) -> p a b", b=512))
        cdma(cosA_t[:], cosA.rearrange("p (a b) -> p a b", b=8))
        cdma(sinA_t[:], sinA.rearrange("p (a b) -> p a b", b=8))
        cdma(cosO_t[:], cosO.rearrange("p (a b) -> p a b", b=8))
        cdma(sinO_t[:], sinO.rearrange("p (a b) -> p a b", b=8))
        cdma(br_t[:], b_r.partition_broadcast(128))
        cdma(convw[:], conv_wT.rearrange("(c p) k -> p c k", p=128))
        cdma(thr_t[:], thr_d.partition_broadcast(128))
        cdma(iota_t[:], iota_d.partition_broadcast(128))
        cdma(pidx_t[:], pidx_d)
        tk_cd = ("D0", P.cnt["D0"])
        P.dma("gpsimd", lambda e: e.dma_start(out=Wr[:], in_=w_r.rearrange("(c p) n -> p c n", p=128)), "D1")
        P.op("vector", lambda e: e.tensor_tensor(out=lam_in[:, 0, :], in0=lam_in[:, 0, :], in1=lam_in[:, 1, :], op=ALU.mult), waits=[tk_cd], sig=False)
        P.op("vector", lambda e: e.tensor_tensor(out=lam_in[:, 2, :], in0=lam_in[:, 2, :], in1=lam_in[:, 3, :], op=ALU.mult), sig=False)
        P.op("vector", lambda e: e.reduce_sum(out=lam_w[:, 0:1], in_=lam_in[:, 0, :], axis=AX.X), sig=False)
        tk = P.op("vector", lambda e: e.reduce_sum(out=lam_w[:, 1:2], in_=lam_in[:, 2, :], axis=AX.X))
        tk = P.op("scalar", lambda e: e.activation(out=lam_w[:, 2:4], in_=lam_w[:, 0:2], func=AF.Exp), waits=[tk])
        P.op("vector", lambda e: e.tensor_tensor(out=lam_w[:, 4:5], in0=lam_w[:, 3:4], in1=lam_w[:, 2:3], op=ALU.subtract), waits=[tk], sig=False)
        P.op("vector", lambda e: e.tensor_scalar(out=nlam[:], in0=lam_w[:, 4:5], scalar1=-0.2, scalar2=None, op0=ALU.add), sig=False)
        P.op("vector", lambda e: e.tensor_scalar(out=gsub_t[:], in0=gsub_t[:], scalar1=0.8, scalar2=None, op0=ALU.mult))
        xe_v = xebuf.rearrange("(p r) d -> p r d", p=128)
        for q in range(8):
            P.dma("gpsimd", lambda e, q=q: e.dma_start(out=xe_v[:, q * 16:(q + 1) * 16, :],
                                                        in_=ztile.unsqueeze(1).broadcast_to([128, 16, 1024])), "D2", waits=[tkz])
        P.barrier()

        def carve_front():
            cv.reset()
            B = {}
            B["xt"] = [cv.get([128, D], F32) for _ in range(2)]
            B["junk"] = cv.get([128, D], BF16)
            B["xn"] = [cv.get([128, D], BF16) for _ in range(2)]
            B["xnT"] = [cv.get([128, 8, 128], BF16) for _ in range(2)]
            B["st"] = [cv.get([128, 16], F32) for _ in range(2)]
            B["sq2"] = [cv.get([128, 256], F32) for _ in range(2)]
            B["stq"] = [cv.get([128, 16], F32) for _ in range(2)]
            B["t"] = cv.get([128, 512], F32)
            B["rp"] = cv.get([128, 4, 8, 8], F32)
            B["qb"] = [cv.get([128, 512], BF16) for _ in range(2)]
            B["W"] = cv.get([128, 8, 512], BF16)
            return B

        state = {"n": 0, "save_tok": [None, None]}
        def reset_state():
            for k in ("xt_free", "xn_free", "ptr_free"):
                state[k] = [None, None]
            state["sq2_free"] = [None, None]; state["stq_free"] = [None, None]

        def x_front(B, src_rows, dst, ncol, dst_free, xt=None, xt_free=None, xt_sem=None, defer_copy=False, save_to=None, load_from=None, load_sem=None):
            n = state["n"]; state["n"] += 1
            b = n % 2
            if load_from is not None:
                sem = load_sem or f"D{38 + b}"
                tl = P.dma("sync", lambda e: e.dma_start(out=dst, in_=load_from.rearrange("p (c k) -> p c k", k=128)[:, :, 0:ncol]), sem, waits=[dst_free])
                if defer_copy:
                    return (lambda: tl), None, b
                return tl, None, b
            if xt is None:
                xt = B["xt"][b]; xt_free = state["xt_free"][b]; xt_sem = ["D4", "D5"][b]
            xn, st = B["xn"][b], B["st"][b]
            tl = P.dma("sync", lambda e: e.dma_start(out=xt, in_=src_rows), xt_sem, waits=[xt_free])
            P.op("scalar", lambda e: e.activation(out=B["junk"], in_=xt, func=AF.Square, accum_out=st[:, 0:1]), waits=[tl], sig=False)
            t2 = P.op("scalar", lambda e: e.activation(out=st[:, 1:2], in_=st[:, 0:1], func=AF.Sqrt, scale=1.0 / D, bias=EPS_T[:, 0:1]))
            P.op("vector", lambda e: e.reciprocal(out=st[:, 2:3], in_=st[:, 1:2]), waits=[t2], sig=False)
            t3 = P.op("vector", lambda e: e.scalar_tensor_tensor(out=xn, in0=xt, scalar=st[:, 2:3], in1=g1_t[:], op0=ALU.mult, op1=ALU.mult),
                      waits=[state["xn_free"][b]])
            state["xt_free"][b] = t3
            ptr = pbank_bf(b).rearrange("p (a b) -> p a b", b=128)
            tt = None
            for c in range(8):
                tt = P.op("tensor", lambda e, c=c: e.transpose(out=ptr[:, c, :], in_=xn[:, c * 128:(c + 1) * 128], identity=ident[:]),
                          waits=[t3, state["ptr_free"][b]] if c == 0 else [], sig=(c == 7))
            state["xn_free"][b] = tt
            def do_copy():
                t4 = P.op("scalar", lambda e: e.copy(out=dst, in_=ptr[:, :, 0:ncol]), waits=[tt, dst_free, state["save_tok"][b]])
                state["ptr_free"][b] = t4
                if save_to is not None:
                    state["save_tok"][b] = P.dma("sync", lambda e: e.dma_start(out=save_to.rearrange("p (c k) -> p c k", k=128), in_=dst), f"D{40 + b}", waits=[t4])
                return t4
            if defer_copy:
                return do_copy, t3, b
            return do_copy(), t3, b

        def qk_stats_a(B, pin, ncol, waits, par):
            sq = B["sq2"][par]
            return P.op("scalar", lambda e: e.activation(out=sq[:, 0:ncol], in_=pin, func=AF.Square), waits=list(waits) + [state["sq2_free"][par]])

        def qk_stats_b(B, ncol, ta, par):
            nh = ncol // 64
            sq = B["sq2"][par]; stq = B["stq"][par]
            tb = P.op("vector", lambda e: e.reduce_sum(out=stq[:, 0:nh], in_=sq[:, 0:ncol].rearrange("p (a b) -> p a b", b=64), axis=AX.X), waits=[ta, state["stq_free"][par]])
            state["sq2_free"][par] = tb
            tc_ = P.op("scalar", lambda e: e.activation(out=stq[:, 0:nh], in_=stq[:, 0:nh], func=AF.Sqrt, scale=1.0 / 64, bias=EPS_T[:, 0:1]), waits=[tb])
            return P.op("vector", lambda e: e.reciprocal(out=stq[:, 0:nh], in_=stq[:, 0:nh]), waits=[tc_])

        def qk_apply(B, pin, ncol, g_t, cos_t, sin_t, outb, tr, out_free, par):
            nh = ncol // 64
            t, rp = B["t"], B["rp"]
            stq = B["stq"][par]
            t3v = t[:, 0:ncol].rearrange("p (a b) -> p a b", b=64)
            P.op("vector", lambda e: e.tensor_tensor(out=t3v, in0=pin.rearrange("p (a b) -> p a b", b=64),
                                                     in1=stq[:, 0:nh].unsqueeze(2).broadcast_to([128, nh, 64]), op=ALU.mult), waits=[tr], sig=False)
            state["stq_free"][par] = P.op("vector", lambda e: e.tensor_tensor(out=t3v, in0=t3v, in1=g_t[:].unsqueeze(1).broadcast_to([128, nh, 64]), op=ALU.mult), sig=False)
            ob3 = outb.rearrange("p (a b) -> p a b", b=64)
            P.op("vector", lambda e: e.tensor_copy(out=ob3[:, :, 16:64], in_=t3v[:, :, 16:64]), waits=[out_free], sig=False)
            cosb = cos_t.unsqueeze(1).broadcast_to([128, nh, 8]); sinb = sin_t.unsqueeze(1).broadcast_to([128, nh, 8])
            r1 = t3v[:, :, 0:8]; r2 = t3v[:, :, 8:16]
            P.op("vector", lambda e: e.tensor_tensor(out=rp[:, 0, 0:nh, :], in0=r1, in1=cosb, op=ALU.mult), sig=False)
            P.op("vector", lambda e: e.tensor_tensor(out=rp[:, 1, 0:nh, :], in0=r2, in1=sinb, op=ALU.mult), sig=False)
            P.op("vector", lambda e: e.tensor_tensor(out=rp[:, 2, 0:nh, :], in0=r2, in1=cosb, op=ALU.mult), sig=False)
            P.op("vector", lambda e: e.tensor_tensor(out=rp[:, 3, 0:nh, :], in0=r1, in1=sinb, op=ALU.mult), sig=False)
            P.op("vector", lambda e: e.tensor_tensor(out=ob3[:, :, 0:8], in0=rp[:, 0, 0:nh, :], in1=rp[:, 1, 0:nh, :], op=ALU.subtract), sig=False)
            return P.op("vector", lambda e: e.tensor_tensor(out=ob3[:, :, 8:16], in0=rp[:, 2, 0:nh, :], in1=rp[:, 3, 0:nh, :], op=ALU.add))

        n_pass = 2
        for p in range(n_pass):
            h0 = 2 * p
            B = carve_front()
            KT = cv.get([128, 2, NT_ALL * 128], BF16)
            Vs = cv.get([128, NT_ALL, 2, 130], BF16)
            Ebuf = [cv.get([128, 1024], BF16) for _ in range(2)]
            ev_t1 = cv.get([128, 4, 128], F32)
            ev_o = cv.get([128, 4, 128], F32)
            ev_sq = cv.get([128, 128], F32)
            ev_st = cv.get([128, 16], F32)
            ev_ob = cv.get([128, 4, 128], BF16)
            ot_st = [cv.get([128, GT], BF16) for _ in range(2)]
            reset_state()
            tk_w = P.dma("gpsimd", lambda e: e.dma_start(out=B["W"][:, :, 0:256], in_=w_in_v[:, :, 1536 + h0 * 128:1536 + h0 * 128 + 256]), "D3")
            tk_ones = P.op("gpsimd", lambda e: e.memset(Vs[:, :, :, 128:129], 1.0))
            def proj_phase(T, src, ncw, g_t, cos_t, sin_t, dst, is_kv, cache):
                pf = {"proj": [None, None], "tr": None, "qb": [None, None], "xnT": [None, None]}
                info = {}
                def stageF(t):
                    b = state["n"] % 2
                    xnT = B["xnT"][b]
                    cp, _, b = x_front(B, src[t * 128:(t + 1) * 128, :], xnT, 128, pf["xnT"][b], defer_copy=True,
                                       save_to=(cache[t] if p == 0 else None), load_from=(cache[t] if p == 1 else None))
                    info[t] = {"b": b, "xnT": xnT, "cp": cp}
                def stageF2(t):
                    info[t]["t4"] = info[t]["cp"]()
                def stagePa(t):
                    d = info[t]; b = d["b"]; xnT = d["xnT"]
                    pk = pbank(2 + b)
                    tm = None
                    for c in range(8):
                        tm = P.op("tensor", lambda e, c=c, pk=pk, xnT=xnT: e.matmul(pk[:, 0:ncw], lhsT=xnT[:, c, :], rhs=B["W"][:, c, 0:ncw], start=(c == 0), stop=(c == 7)),
                                  waits=[d["t4"], tk_w, pf["proj"][b]] if c == 0 else [], sig=(c == 7))
                    pf["xnT"][b] = tm
                    d["tm"] = tm; d["pk"] = pk
                    d["tv"] = None
                def stagePb(t):
                    d = info[t]; pk = d["pk"]
                    if is_kv:
                        d["tv"] = P.op("scalar", lambda e, t=t, pk=pk: e.copy(out=Vs[:, t, :, 0:128], in_=pk[:, 256:512].rearrange("p (a b) -> p a b", b=128)), waits=[d["tm"]])
                def stageN1a(t):
                    d = info[t]; b = d["b"]
                    d["ta"] = qk_stats_a(B, d["pk"][:, 0:256], 256, [d["tm"], d["tv"]], b)
                def stageN1b(t):
                    d = info[t]; b = d["b"]
                    d["tr"] = qk_stats_b(B, 256, d["ta"], b)
                def stageN2(t):
                    d = info.pop(t); b = d["b"]; pk = d["pk"]
                    kb = B["qb"][b][:, 0:256]
                    tq = qk_apply(B, pk[:, 0:256], 256, g_t, cos_t[:, t, :], sin_t[:, t, :], kb, d["tr"], pf["qb"][b], b)
                    pf["proj"][b] = [tq, d["tv"]]
                    pkt = pbank_bf(4).rearrange("p (a b) -> p a b", b=128)[:, 0:2, :]
                    tt = None
                    for hl in range(2):
                        tt = P.op("tensor", lambda e, hl=hl, kb=kb: e.transpose(out=pkt[:, hl, :], in_=kb[:, hl * 128:(hl + 1) * 128], identity=ident[:]),
                                  waits=[tq, pf["tr"]] if hl == 0 else [], sig=(hl == 1))
                    pf["qb"][b] = tt
                    pf["tr"] = P.op("vector", lambda e, t=t: e.tensor_copy(out=dst[:, :, t * 128:(t + 1) * 128], in_=pkt), waits=[tt])
                for k in range(T + 2):
                    if 0 <= k - 1 < T:
                        stagePa(k - 1)
                    if k < T:
                        stageF(k)
                    if 0 <= k - 1 < T:
                        stagePb(k - 1)
                    if p == 0:
                        if 0 <= k - 2 < T:
                            stageN1a(k - 2); stageN1b(k - 2); stageN2(k - 2)
                    else:
                        if 0 <= k - 1 < T:
                            stageN1a(k - 1)
                        if 0 <= k - 2 < T:
                            stageN2(k - 2)
                        if 0 <= k - 1 < T:
                            stageN1b(k - 1)
                    if k < T:
                        stageF2(k)
                return pf["tr"]
            qtr_free = proj_phase(NOT_, xown, 256, gq_t, cosO_t, sinO_t, QT, False, cacheO)
            if DEBUG and p == 0:
                out_toks.append(P.dma("sync", lambda e: e.dma_start(out=dbg_qt, in_=QT[:].rearrange("p a b -> p (a b)")), "D6", waits=[qtr_free]))
            P.barrier()
            if STOP_AFTER == "O":
                break
            P.dma("gpsimd", lambda e: e.dma_start(out=B["W"][:, :, 0:256], in_=w_in_v[:, :, 2048 + h0 * 128:2048 + h0 * 128 + 256]), "D3")
            tk_w = P.dma("gpsimd", lambda e: e.dma_start(out=B["W"][:, :, 256:512], in_=w_in_v[:, :, 2560 + h0 * 128:2560 + h0 * 128 + 256]), "D3")
            reset_state()
            proj_phase(int(os.environ.get("HK_NTA", NT_ALL)), xall, 512, gk_t, cosA_t, sinA_t, KT, True, cacheA)
            if DEBUG and p == 0:
                P.barrier()
                out_toks.append(P.dma("sync", lambda e: e.dma_start(out=dbg_kt, in_=KT.rearrange("p a b -> p (a b)")), "D6"))
                out_toks.append(P.dma("sync", lambda e: e.dma_start(out=dbg_v, in_=Vs.rearrange("p a b c -> p (a b c)")), "D6"))
            P.barrier()
            if STOP_AFTER == "A":
                break
            accs = []
            for a in range(8):
                bk, r = divmod(a, 3)
                accs.append(psum[:, (4 + bk) * 512 + r * 132:(4 + bk) * 512 + r * 132 + 129])
            AST = {"S_free": [None, None], "E_free": [None, None], "acc_free": None, "otr_free": None, "n_ot": 0,
                   "ot_st_free": [None, None]}
            def emit_S_exp(i, hl, u, un):
                nkb = 8 * i + 8
                si = un % 2
                nk = 16 if u == 0 else 128
                ps = pbank(2 * si, 2)
                ts = None
                for m in range(2):
                    ts = P.op("tensor", lambda e, m=m, ps=ps, u=u, nk=nk, hl=hl, i=i: e.matmul(
                        ps[0:nk, m * 512:(m + 1) * 512], lhsT=KT[m * 64:(m + 1) * 64, hl, u * 128:u * 128 + nk],
                        rhs=QT[m * 64:(m + 1) * 64, hl, i * GT:(i + 1) * GT], start=True, stop=True),
                        waits=[AST["S_free"][si]] if m == 0 else [], sig=(m == 1))
                Eb = Ebuf[si]
                te = P.op("scalar", lambda e, ps=ps, Eb=Eb, nk=nk: e.activation(out=Eb[0:nk, :], in_=ps[0:nk, :], func=AF.Exp, scale=0.125),
                          waits=[ts, AST["E_free"][si]], noself=True)
                AST["S_free"][si] = te
                if u > nkb - 8:
                    r = u - 1 - (nkb - 8)
                    mi = (i % 2) * 8 + r
                    te = P.op("vector", lambda e, Eb=Eb, mi=mi: e.tensor_tensor(
                        out=Eb.rearrange("p (a b) -> p a b", b=512), in0=Eb.rearrange("p (a b) -> p a b", b=512),
                        in1=masks[:, mi, :].unsqueeze(1).broadcast_to([128, 2, 512]), op=ALU.mult), waits=[te])
                return {"i": i, "hl": hl, "u": u, "si": si, "nk": nk, "Eb": Eb, "te": te, "nkb": nkb}

            def emit_PV(d):
                i, hl, u, si, nk, Eb, te, nkb = d["i"], d["hl"], d["u"], d["si"], d["nk"], d["Eb"], d["te"], d["nkb"]
                tp = None
                for m in range(2):
                    for s_ in range(4):
                        a = m * 4 + s_
                        tp = P.op("tensor", lambda e, a=a, m=m, s_=s_, Eb=Eb, nk=nk, u=u, hl=hl, nkb=nkb: e.matmul(
                            accs[a], lhsT=Eb[0:nk, m * 512 + s_ * 128:m * 512 + (s_ + 1) * 128], rhs=Vs[0:nk, u, hl, 0:129],
                            start=(u == 0 and a % 3 == 0), stop=(u == nkb), skip_group_check=True),
                            waits=[te, AST["acc_free"] if u == 0 else None] if a == 0 else [], sig=(a == 7))
                AST["E_free"][si] = tp
                if u == nkb:
                    emit_evac(i, hl, tp)

            def emit_evac(i, hl, last_pv):
                h = 2 * p + hl
                st = ev_st
                acc_free = None
                for s in range(4):
                    a1, a2 = accs[s], accs[4 + s]
                    P.op("vector", lambda e, a1=a1, s=s: e.reciprocal(out=st[:, s:s + 1], in_=a1[:, 128:129]), waits=[last_pv] if s == 0 else [], sig=False)
                    P.op("vector", lambda e, a2=a2, s=s: e.reciprocal(out=st[:, 4 + s:5 + s], in_=a2[:, 128:129]), sig=False)
                    P.op("vector", lambda e, s=s: e.tensor_tensor(out=st[:, 4 + s:5 + s], in0=st[:, 4 + s:5 + s], in1=nlam[:], op=ALU.mult), sig=False)
                    P.op("vector", lambda e, a1=a1, s=s: e.tensor_scalar(out=ev_t1[:, s, :], in0=a1[:, 0:128], scalar1=st[:, s:s + 1], scalar2=None, op0=ALU.mult), sig=False)
                    acc_free = P.op("vector", lambda e, a2=a2, s=s: e.scalar_tensor_tensor(out=ev_o[:, s, :], in0=a2[:, 0:128], scalar=st[:, 4 + s:5 + s], in1=ev_t1[:, s, :],
                                                                                            op0=ALU.mult, op1=ALU.add), sig=False)
                AST["acc_free"] = acc_free
                tss = None
                for s in range(4):
                    P.op("vector", lambda e, s=s: e.tensor_tensor(out=ev_sq, in0=ev_o[:, s, :], in1=ev_o[:, s, :], op=ALU.mult), sig=False)
                    tss = P.op("vector", lambda e, s=s: e.reduce_sum(out=st[:, 8 + s:9 + s], in_=ev_sq, axis=AX.X))
                tsq = P.op("scalar", lambda e: e.activation(out=st[:, 8:12], in_=st[:, 8:12], func=AF.Sqrt, scale=1.0 / 128, bias=EPS_T[:, 0:1]), waits=[tss])
                P.op("vector", lambda e: e.reciprocal(out=st[:, 8:12], in_=st[:, 8:12]), waits=[tsq], sig=False)
                tob = None
                for s in range(4):
                    tob = P.op("vector", lambda e, s=s: e.scalar_tensor_tensor(out=ev_ob[:, s, :], in0=ev_o[:, s, :], scalar=st[:, 8 + s:9 + s], in1=gsub_t[:],
                                                                                op0=ALU.mult, op1=ALU.mult), waits=[AST["otr_free"]] if s == 0 else [])
                pot = pbank_bf(7).rearrange("p (a b) -> p a b", b=128)[:, 0:4, :]
                tt = None
                for s in range(4):
                    tt = P.op("tensor", lambda e, s=s: e.transpose(out=pot[:, s, :], in_=ev_ob[:, s, :], identity=ident[:]),
                              waits=[tob, AST["otr_free"]] if s == 0 else [], sig=(s == 3))
                n_ot = AST["n_ot"]
                osb = ot_st[n_ot % 2]
                otr = P.op("vector", lambda e, osb=osb: e.tensor_copy(out=osb, in_=pot.rearrange("p a b -> p (a b)")), waits=[tt, AST["ot_st_free"][n_ot % 2]])
                AST["otr_free"] = otr
                AST["ot_st_free"][n_ot % 2] = P.dma("sync", lambda e, osb=osb, h=h, i=i: e.dma_start(out=otbuf[:, h * NOWN + i * GT:h * NOWN + (i + 1) * GT], in_=osb),
                                                    f"D{33 + n_ot % 2}", waits=[otr])
                AST["n_ot"] = n_ot + 1

            units = [(i, hl, u) for i in range(NG) for hl in range(2) for u in range(8 * i + 9)]
            prev = None
            for un, (i, hl, u) in enumerate(units):
                d = emit_S_exp(i, hl, u, un)
                if prev is not None:
                    emit_PV(prev)
                prev = d
            emit_PV(prev)
            P.barrier()
        if STOP_AFTER not in ("O", "A", "B"):
            V = lambda fn, waits=(): P.op("vector", fn, waits)
            A = lambda fn, waits=(): P.op("scalar", fn, waits)
            T = lambda fn, waits=(), sig=True: P.op("tensor", fn, waits, sig)
            cv.reset()
            xt4 = cv.get([128, 4, D], F32)
            B = {}
            B["junk"] = cv.get([128, D], BF16)
            B["xn"] = [cv.get([128, D], BF16) for _ in range(2)]
            B["st"] = [cv.get([128, 16], F32) for _ in range(2)]
            hal_xt = cv.get([128, D], F32)
            xnTg = cv.get([128, 8, 516], BF16)
            Wc = cv.get([128, 8, 1536], BF16)
            Wo = cv.get([128, 8, D], BF16)
            g2_t = cv.get([128, D], F32)
            cc = cv.get([128, 516], F32)
            z = cv.get([128, 516], F32)
            yv = cv.get([128, 512], F32)
            mixc = cv.get([128, 4, 512], BF16)
            h1 = [cv.get([128, D], F32) for _ in range(2)]
            xn2 = [cv.get([128, D], BF16) for _ in range(2)]
            xn2T = cv.get([128, 8, 128], BF16)
            R = cv.get([128, 1024], F32)
            OTg = [cv.get([128, 4, GT], BF16) for _ in range(2)]
            otg_free = [None, None]
            st2 = cv.get([128, 8], F32)
            ohb = cv.get([128, 4, 32], BF16)
            reset_state()
            P.dma("gpsimd", lambda e: e.dma_start(out=Wc, in_=w_in_v[:, :, 0:1536]), "D16")
            P.dma("gpsimd", lambda e: e.dma_start(out=Wo, in_=w_out_v), "D16")
            tk_g2 = P.dma("sync", lambda e: e.dma_start(out=g2_t, in_=norm2_g.partition_broadcast(128)), "D37")
            tk_wc = [("D16", P.cnt["D16"]), tk_g2]
            ps5 = pbank(5)
            ps_rt = pbank_bf(2).rearrange("p (a b) -> p a b", b=128)
            rtc_free = None; xn2T_free = None
            xt4_tok = [None] * 4
            xt4_free = [None] * 4; hal_free = None; grp_free = None; conv_free = None
            ph_free = [None, None]; h1_free = [None, None]; xn2_free = [None, None]; rt_free = None
            nph = 0
            tl_last = None
            for i in range(NG):
                fr = []
                t4, t3, _ = x_front(B, xhalo[i * 128:(i + 1) * 128, :], xnTg[:, :, 0:2], 2, grp_free, xt=hal_xt, xt_free=hal_free, xt_sem="D11")
                hal_free = t3; fr.append(t4)
                for s in range(4):
                    xt4_tok[s] = P.dma("sync", lambda e, s=s, i=i: e.dma_start(out=xt4[:, s, :], in_=xown[(4 * i + s) * 128:(4 * i + s + 1) * 128, :]),
                                       f"D{7 + s}", waits=[xt4_free[s]])
                    t4, _, _ = x_front(B, None, xnTg[:, :, 2 + s * 128:2 + (s + 1) * 128], 128, grp_free, load_from=cacheO[4 * i + s], load_sem=f"D{42 + s}")
                    fr.append(t4)
                OTc = OTg[i % 2]
                tk_otg = P.dma("sync", lambda e, OTc=OTc, i=i: e.dma_start(out=OTc, in_=otbuf.rearrange("p (h t) -> p h t", h=4)[:, :, i * GT:(i + 1) * GT]),
                               f"D{35 + i % 2}", waits=[otg_free[i % 2]])
                tmix = None
                tm = None
                for q in range(4):
                    for (blkc, dstp, hcol) in ((q, pbank(2), None), (4 + q, pbank(3), 0), (8 + q, pbank(4), 2)):
                        for c in range(8):
                            tm = T(lambda e, c=c, blkc=blkc, dstp=dstp: e.matmul(dstp, lhsT=Wc[:, c, blkc * 128:(blkc + 1) * 128], rhs=xnTg[:, c, 2:514],
                                                                                 start=(c == 0), stop=(c == 7)),
                                   waits=fr + [tk_wc, conv_free, rt_free] if c == 0 else [], sig=(c == 7))
                        if hcol is not None:
                            for c in range(8):
                                tm = T(lambda e, c=c, blkc=blkc, hcol=hcol: e.matmul(ps5[:, hcol:hcol + 2], lhsT=Wc[:, c, blkc * 128:(blkc + 1) * 128], rhs=xnTg[:, c, 0:2],
                                                                                     start=(c == 0), stop=(c == 7)), sig=(c == 7))
                    A(lambda e: e.copy(out=cc[:, 2:514], in_=pbank(3)), waits=[tm])
                    ta = A(lambda e: e.copy(out=cc[:, 0:2], in_=ps5[:, 0:2]))
                    V(lambda e: e.tensor_tensor(out=z[:, 2:514], in0=cc[:, 2:514], in1=pbank(4), op=ALU.mult), waits=[ta, tm])
                    V(lambda e: e.tensor_tensor(out=z[:, 0:2], in0=cc[:, 0:2], in1=ps5[:, 2:4], op=ALU.mult))
                    V(lambda e, q=q: e.tensor_scalar(out=yv, in0=z[:, 0:512], scalar1=convw[:, q, 0:1], scalar2=None, op0=ALU.mult))
                    V(lambda e, q=q: e.scalar_tensor_tensor(out=yv, in0=z[:, 1:513], scalar=convw[:, q, 1:2], in1=yv, op0=ALU.mult, op1=ALU.add))
                    V(lambda e, q=q: e.scalar_tensor_tensor(out=yv, in0=z[:, 2:514], scalar=convw[:, q, 2:3], in1=yv, op0=ALU.mult, op1=ALU.add))
                    conv_free = V(lambda e, q=q: e.tensor_tensor(out=mixc[:, q, :], in0=pbank(2), in1=yv, op=ALU.mult), waits=[grp_free])
                tmix = conv_free
                grp_free = tm
                CS = {}
                def Wstage(s):
                    nonlocal nph
                    ot = 4 * i + s
                    par = ot % 2
                    hb = h1[par]; xb = xn2[par]
                    for hf in range(2):
                        ph = pbank(6 + nph % 2); pfree = ph_free[nph % 2]
                        tw = None
                        for kk in range(8):
                            lhsT = mixc[:, kk, s * 128:(s + 1) * 128] if kk < 4 else OTc[:, kk - 4, s * 128:(s + 1) * 128]
                            tw = T(lambda e, kk=kk, lhsT=lhsT, ph=ph, hf=hf: e.matmul(ph, lhsT=lhsT, rhs=Wo[:, kk, hf * 512:(hf + 1) * 512], start=(kk == 0), stop=(kk == 7)),
                                   waits=[tmix, pfree, tk_otg] if kk == 0 else [], sig=(kk == 7))
                        th = V(lambda e, hb=hb, ph=ph, s=s, hf=hf: e.tensor_tensor(out=hb[:, hf * 512:(hf + 1) * 512], in0=ph, in1=xt4[:, s, hf * 512:(hf + 1) * 512], op=ALU.add),
                               waits=[tw, h1_free[par], xt4_tok[s]])
                        ph_free[nph % 2] = th
                        nph += 1
                    xt4_free[s] = th
                    if s == 3:
                        otg_free[i % 2] = tw
                    A(lambda e, hb=hb: e.activation(out=B["junk"], in_=hb, func=AF.Square, accum_out=st2[:, 0:1]), waits=[th])
                    ta = A(lambda e: e.activation(out=st2[:, 1:2], in_=st2[:, 0:1], func=AF.Sqrt, scale=1.0 / D, bias=EPS_T[:, 0:1]))
                    V(lambda e: e.reciprocal(out=st2[:, 2:3], in_=st2[:, 1:2]), waits=[ta])
                    tx = V(lambda e, hb=hb, xb=xb: e.scalar_tensor_tensor(out=xb, in0=hb, scalar=st2[:, 2:3], in1=g2_t, op0=ALU.mult, op1=ALU.mult), waits=[xn2_free[par]])
                    d1 = P.dma("sync", lambda e, hb=hb, ot=ot: e.dma_start(out=h1buf[ot * 128:(ot + 1) * 128, :], in_=hb), f"D{12 + par}", waits=[th])
                    d2 = P.dma("sync", lambda e, xb=xb, ot=ot: e.dma_start(out=xn2buf[ot * 128:(ot + 1) * 128, :], in_=xb), f"D{14 + par}", waits=[tx])
                    h1_free[par] = [d1, tx]
                    CS[s] = {'tx': tx, 'xb': xb, 'par': par, 'd2': d2}
                def Rstage(s):
                    nonlocal rtc_free, xn2T_free, tl_last
                    c_ = CS[s]; tx = c_['tx']; xb = c_['xb']; par = c_['par']; d2 = c_['d2']
                    tt = None
                    for c in range(8):
                        tt = T(lambda e, c=c, xb=xb: e.transpose(out=ps_rt[:, c, :], in_=xb[:, c * 128:(c + 1) * 128], identity=ident[:]),
                               waits=[tx, rtc_free, conv_free] if c == 0 else [], sig=(c == 7))
                    rtc_free = A(lambda e: e.copy(out=xn2T, in_=ps_rt), waits=[tt, xn2T_free])
                    xn2_free[par] = [d2, tt]
                    tl = None
                    for c in range(8):
                        tl = T(lambda e, c=c, s=s: e.matmul(ps5[:, 16 + 36 * s:52 + 36 * s], lhsT=xn2T[:, c, :], rhs=Wr[:, c, :], start=(c == 0), stop=(c == 7)),
                               waits=[rtc_free, rt_free] if c == 0 else [], sig=(c == 7))
                    xn2T_free = tl
                    tl_last = tl
                for step in (('W', 0), ('W', 1), ('R', 0), ('W', 2), ('R', 1), ('W', 3), ('R', 2), ('R', 3)):
                    (Wstage if step[0] == 'W' else Rstage)(step[1])
                tl = tl_last
                o4 = 4 * i
                def R3(a, n, k):
                    return R[:, a:a + 4 * k].rearrange("p (s k) -> p s k", k=k)
                def R2(a):
                    return R[:, a:a + 4]
                def bc(ap2, k):
                    return ap2.unsqueeze(2).broadcast_to([128, 4, k])
                LG = R3(0, 4, 36)
                V(lambda e: e.tensor_tensor(out=LG, in0=ps5[:, 16:160].rearrange("p (s k) -> p s k", k=36), in1=br_t[:].unsqueeze(1).broadcast_to([128, 4, 36]), op=ALU.add), waits=[tl])
                LGg = LG[:, :, 0:4]
                V(lambda e: e.tensor_reduce(out=R2(144), in_=LGg, axis=AX.X, op=ALU.max))
                V(lambda e: e.tensor_tensor(out=R3(148, 4, 4), in0=LGg, in1=bc(R2(144), 4), op=ALU.is_equal))
                tg = V(lambda e: e.tensor_tensor(out=R3(164, 4, 4), in0=LGg, in1=bc(R2(144), 4), op=ALU.subtract))
                tge = A(lambda e: e.activation(out=R[:, 180:196], in_=R[:, 164:180], func=AF.Exp), waits=[tg])
                V(lambda e: e.reduce_sum(out=R2(196), in_=R3(180, 4, 4), axis=AX.X), waits=[tge])
                V(lambda e: e.reciprocal(out=R2(200), in_=R2(196)))
                V(lambda e: e.tensor_tensor(out=R3(164, 4, 4), in0=R3(148, 4, 4), in1=iota_t[:, 0:4].unsqueeze(1).broadcast_to([128, 4, 4]), op=ALU.mult))
                V(lambda e: e.reduce_sum(out=R2(204), in_=R3(164, 4, 4), axis=AX.X))
                PR = R[:, 208:336].rearrange("p (s g j) -> p s g j", g=4, j=8)
                V(lambda e: e.tensor_tensor(out=PR, in0=LG[:, :, 4:36].rearrange("p s (g j) -> p s g j", j=8),
                                            in1=R3(148, 4, 4).unsqueeze(3).broadcast_to([128, 4, 4, 8]), op=ALU.mult))
                ES = R3(336, 4, 8)
                V(lambda e: e.reduce_sum(out=ES, in_=PR.rearrange("p s g j -> p s j g"), axis=AX.X))
                T8 = R3(368, 4, 8)
                for s in range(4):
                    V(lambda e, s=s: e.max(out=T8[:, s, :], in_=ES[:, s, :]))
                OH = [R3(400, 4, 8), R3(432, 4, 8)]
                for k in range(2):
                    V(lambda e, k=k: e.tensor_tensor(out=OH[k], in0=ES, in1=T8[:, :, k:k + 1].broadcast_to([128, 4, 8]), op=ALU.is_equal))
                    V(lambda e, k=k: e.tensor_tensor(out=R3(464, 4, 8), in0=OH[k], in1=iota_t[:, 0:8].unsqueeze(1).broadcast_to([128, 4, 8]), op=ALU.mult))
                    V(lambda e, k=k: e.reduce_sum(out=R2(496 + 4 * k), in_=R3(464, 4, 8), axis=AX.X))
                    V(lambda e, k=k: e.scalar_tensor_tensor(out=eid_all[:, o4:o4 + 4, k], in0=R2(204), scalar=8.0, in1=R2(496 + 4 * k), op0=ALU.mult, op1=ALU.add))
                td = V(lambda e: e.tensor_tensor(out=R2(504), in0=T8[:, :, 1], in1=T8[:, :, 0], op=ALU.subtract))
                tex = A(lambda e: e.activation(out=R2(508), in_=R2(504), func=AF.Exp), waits=[td])
                V(lambda e: e.tensor_scalar(out=R2(512), in0=R2(508), scalar1=1.0, scalar2=None, op0=ALU.add), waits=[tex])
                V(lambda e: e.reciprocal(out=R2(516), in_=R2(512)))
                V(lambda e: e.tensor_tensor(out=gate_all[:, o4:o4 + 4, 0], in0=R2(200), in1=R2(516), op=ALU.mult))
                V(lambda e: e.tensor_tensor(out=gate_all[:, o4:o4 + 4, 1], in0=R2(200), in1=gate_all[:, o4:o4 + 4, 0], op=ALU.subtract))
                O32 = [R3(520, 4, 32), R3(648, 4, 32)]
                for k in range(2):
                    V(lambda e, k=k: e.tensor_tensor(out=O32[k], in0=iota_t[:].unsqueeze(1).broadcast_to([128, 4, 32]),
                                                     in1=eid_all[:, o4:o4 + 4, k:k + 1].broadcast_to([128, 4, 32]), op=ALU.is_equal))
                toh = V(lambda e: e.tensor_tensor(out=ohb, in0=O32[0], in1=O32[1], op=ALU.add))
                tcn = None
                for s in range(4):
                    T(lambda e, s=s: e.matmul(ps5[:, 160 + 32 * s:192 + 32 * s], lhsT=Ubf[:], rhs=ohb[:, s, :], start=True, stop=True), waits=[toh] if s == 0 else [], sig=False)
                    tcn = T(lambda e, s=s: e.matmul(ps5[:, 288 + 32 * s:320 + 32 * s], lhsT=ones_bf[:], rhs=ohb[:, s, :], start=True, stop=True))
                BV = R3(776, 4, 32)
                V(lambda e: e.tensor_copy(out=BV[:, 0, :], in_=base[:]), waits=[tcn])
                for s in range(1, 4):
                    V(lambda e, s=s: e.tensor_tensor(out=BV[:, s, :], in0=BV[:, s - 1, :], in1=ps5[:, 288 + 32 * (s - 1):320 + 32 * (s - 1)], op=ALU.add))
                V(lambda e: e.tensor_tensor(out=base[:], in0=BV[:, 3, :], in1=ps5[:, 384:416], op=ALU.add))
                V(lambda e: e.tensor_tensor(out=BV, in0=BV, in1=ps5[:, 160:288].rearrange("p (s k) -> p s k", k=32), op=ALU.add))
                for k in range(2):
                    V(lambda e, k=k: e.tensor_tensor(out=O32[k], in0=O32[k], in1=BV, op=ALU.mult))
                    rt_free = V(lambda e, k=k: e.reduce_sum(out=rank_all[:, o4:o4 + 4, k], in_=O32[k], axis=AX.X))
            if DEBUG:
                V(lambda e: e.tensor_copy(out=R[:, 0:64], in_=eid_all[:].rearrange("p a b -> p (a b)")))
            cv.reset()
            cmpn = cv.get([128, 32, 32], F32)
            V(lambda e: e.tensor_tensor(out=cmpn, in0=base[:].unsqueeze(2).broadcast_to([128, 32, 32]),
                                        in1=thr_t[:, 0:32].unsqueeze(1).broadcast_to([128, 32, 32]), op=ALU.is_gt))
            V(lambda e: e.reduce_sum(out=pst[:, 3, :], in_=cmpn, axis=AX.X))
            V(lambda e: e.tensor_scalar(out=pst[:, 0, :], in0=pst[:, 3, :], scalar1=256.0, scalar2=None, op0=ALU.mult))
            V(lambda e: e.tensor_copy(out=pst[:, 1, 0:1], in_=pst[:, 0, 0:1]))
            for ee in range(1, 32):
                V(lambda e, ee=ee: e.tensor_tensor(out=pst[:, 1, ee:ee + 1], in0=pst[:, 1, ee - 1:ee], in1=pst[:, 0, ee:ee + 1], op=ALU.add))
            V(lambda e: e.tensor_tensor(out=pst[:, 2, :], in0=pst[:, 1, :], in1=pst[:, 0, :], op=ALU.subtract))
            cmp3 = cv.get([128, NBLK, 32], F32)
            V(lambda e: e.tensor_tensor(out=cmp3, in0=pst[:, 1, :].unsqueeze(1).broadcast_to([128, NBLK, 32]),
                                        in1=thr_t[:].unsqueeze(2).broadcast_to([128, NBLK, 32]), op=ALU.is_le))
            V(lambda e: e.reduce_sum(out=Ej_f[:], in_=cmp3, axis=AX.X))
            V(lambda e: e.tensor_scalar(out=Ej_f[:], in0=Ej_f[:], scalar1=31.0, scalar2=None, op0=ALU.min))
            tk_ej = V(lambda e: e.tensor_copy(out=Ej_i[:], in_=Ej_f[:]))
            unused = cv.get([128, NBLK], F32)
            V(lambda e: e.tensor_scalar(out=unused, in0=thr_t[:], scalar1=pst[:, 1, 31:32], scalar2=None, op0=ALU.is_ge))
            V(lambda e: e.scalar_tensor_tensor(out=Ej_f[:], in0=unused, scalar=8192.0, in1=Ej_f[:], op0=ALU.mult, op1=ALU.add))
            idxW_f = cv.get([128, NBLK, 12], F32)
            for c in range(12):
                V(lambda e, c=c: e.tensor_scalar(out=idxW_f[:, :, c], in0=Ej_f[:], scalar1=(128.0 if c < 8 else 512.0), scalar2=pidx_t[:, c:c + 1],
                                                 op0=ALU.mult, op1=ALU.add))
            tk_ej = V(lambda e: e.tensor_copy(out=idxW_i[:], in_=idxW_f))
            for ot in range(NOT_):
                for k in range(2):
                    V(lambda e, ot=ot, k=k: e.tensor_scalar(out=R[:, 96:128], in0=iota_t[:], scalar1=eid_all[:, ot, k:k + 1], scalar2=None, op0=ALU.is_equal))
                    V(lambda e: e.tensor_tensor(out=R[:, 96:128], in0=R[:, 96:128], in1=pst[:, 2, :], op=ALU.mult))
                    V(lambda e: e.reduce_sum(out=R[:, 240:241], in_=R[:, 96:128], axis=AX.X))
                    V(lambda e, ot=ot, k=k: e.tensor_tensor(out=slot_f[:, ot, k:k + 1], in0=R[:, 240:241], in1=rank_all[:, ot, k:k + 1], op=ALU.add))
            tk_slot = V(lambda e: e.tensor_copy(out=slot_i[:], in_=slot_f[:]))
            if DEBUG:
                V(lambda e: e.tensor_copy(out=R[:, 64:128], in_=slot_f[:].rearrange("p a b -> p (a b)")))
                V(lambda e: e.tensor_copy(out=R[:, 128:192], in_=gate_all[:].rearrange("p a b -> p (a b)")))
                V(lambda e: e.tensor_copy(out=R[:, 192:256], in_=Ej_f[:]))
                out_toks.append(P.dma("sync", lambda e: e.dma_start(out=dbg_rt[:, 0:256], in_=R), "D6", waits=[P.last["vector"]]))
            P.barrier()
            cv.reset()
            NXS = 4
            xs = [cv.get([128, D], BF16) for _ in range(NXS)]
            xs_free = [None] * NXS
            xs_sem = ["D17", "D18", "D38", "D39"]
            sc_sem = ["D19", "D20", "D46", "D47"]
            for ot in range(NOT_):
                par = ot % NXS
                tl = P.dma("sync", lambda e, ot=ot, par=par: e.dma_start(out=xs[par], in_=xn2buf[ot * 128:(ot + 1) * 128, :]), xs_sem[par], waits=[xs_free[par]])
                tsc = None
                for k in range(2):
                    tsc = P.dma("gpsimd", lambda e, ot=ot, k=k, par=par: e.indirect_dma_start(
                        out=xebuf, out_offset=bass.IndirectOffsetOnAxis(ap=slot_i[:, ot, k:k + 1], axis=0), in_=xs[par], in_offset=None,
                        bounds_check=breg(e, NSLOT - 1), oob_is_err=False), sc_sem[par], waits=[tl])
                xs_free[par] = tsc
            P.barrier()
            if STOP_AFTER != "C":
                cv.reset()
                Wg = [cv.get([128, 8, 512], BF16) for _ in range(2)]
                Wu = [cv.get([128, 8, 512], BF16) for _ in range(2)]
                Wd = [cv.get([128, 4, D], BF16) for _ in range(2)]
                xe = [cv.get([128, 2, D], BF16) for _ in range(2)]
                xeT = cv.get([128, 8, 256], BF16)
                sg = [cv.get([128, 256], F32) for _ in range(2)]
                hidT = cv.get([128, 4, 256], BF16)
                ysb = [cv.get([128, D], F32) for _ in range(2)]
                w_free = [None, None]; xe_free = [None, None]; xeT_free = None; pxe_free = None
                sg_free = [None, None]; pgu_free = [None, None]; hid_free = None; py_free = [None, None]; ysb_free = [None, None]
                ny = 0
                wg_v = w_gate.rearrange("e (c p) n -> p e c n", p=128)
                wu_v = w_up.rearrange("e (c p) n -> p e c n", p=128)
                wd_v = w_down.rearrange("e (c p) n -> p e c n", p=128)
                wg_rows = w_gate.rearrange("e (p c) n -> (e p) (c n)", c=8)
                wu_rows = w_up.rearrange("e (p c) n -> (e p) (c n)", c=8)
                wd_rows = w_down.rearrange("e f n -> (e f) n")
                def emit_wload(j):
                    par = j % 2
                    tok = P.dma("gpsimd", lambda e: e.indirect_dma_start(
                        out=Wg[par].rearrange("p c n -> p (c n)"), out_offset=None, in_=wg_rows, in_offset=bass.IndirectOffsetOnAxis(ap=idxW_i[:, j, 0:1], axis=0),
                        bounds_check=breg(e, NE * 128 - 1), oob_is_err=False), f"D{21 + par}", waits=[w_free[par], tk_ej])
                    tok = P.dma("gpsimd", lambda e: e.indirect_dma_start(
                        out=Wu[par].rearrange("p c n -> p (c n)"), out_offset=None, in_=wu_rows, in_offset=bass.IndirectOffsetOnAxis(ap=idxW_i[:, j, 0:1], axis=0),
                        bounds_check=breg(e, NE * 128 - 1), oob_is_err=False), f"D{21 + par}")
                    for c in range(4):
                        tok = P.dma("gpsimd", lambda e, c=c: e.indirect_dma_start(
                            out=Wd[par][:, c, :], out_offset=None, in_=wd_rows, in_offset=bass.IndirectOffsetOnAxis(ap=idxW_i[:, j, 8 + c:9 + c], axis=0),
                            bounds_check=breg(e, NE * 512 - 1), oob_is_err=False), f"D{21 + par}")
                    return tok
                def emit_xload(j):
                    par = j % 2
                    return P.dma("sync", lambda e: e.dma_start(out=xe[par], in_=xebuf[j * BLK:(j + 1) * BLK, :].rearrange("(t p) d -> p t d", p=128)),
                                 f"D{23 + par}", waits=[xe_free[par]])
                tkw = {0: emit_wload(0)}; tkx = {0: emit_xload(0)}
                ptrx = pbank_bf(0, 2).rearrange("p (a b) -> p a b", b=256)
                for j in range(NBLK):
                    par = j % 2
                    if j + 1 < NBLK:
                        tkw[j + 1] = emit_wload(j + 1); tkx[j + 1] = emit_xload(j + 1)
                    tt = None
                    for t2 in range(2):
                        for c in range(8):
                            tt = T(lambda e, t2=t2, c=c, par=par: e.transpose(out=ptrx[:, c, t2 * 128:(t2 + 1) * 128], in_=xe[par][:, t2, c::8], identity=ident[:]),
                                   waits=[tkx[j], pxe_free] if (t2 == 0 and c == 0) else [], sig=(t2 == 1 and c == 7))
                    xe_free[par] = tt
                    pxe_free = A(lambda e: e.copy(out=xeT, in_=ptrx), waits=[tt, xeT_free])
                    for fc in range(4):
                        pp = fc % 2
                        pgu = pbank(2 + pp)
                        tg = None
                        for (Wsrc, c0) in ((Wg[par], 0), (Wu[par], 256)):
                            for c in range(8):
                                tg = T(lambda e, c=c, Wsrc=Wsrc, c0=c0, pgu=pgu, fc=fc: e.matmul(pgu[:, c0:c0 + 256], lhsT=Wsrc[:, c, fc * 128:(fc + 1) * 128], rhs=xeT[:, c, :],
                                                                                                 start=(c == 0), stop=(c == 7)),
                                       waits=[pxe_free, tkw[j], pgu_free[pp]] if (c == 0 and c0 == 0) else [], sig=(c == 7))
                        tsg = A(lambda e, pgu=pgu, pp=pp: e.activation(out=sg[pp], in_=pgu[:, 0:256], func=AF.Silu), waits=[tg, sg_free[pp]])
                        th = V(lambda e, pgu=pgu, pp=pp, fc=fc: e.tensor_tensor(out=hidT[:, fc, :], in0=sg[pp], in1=pgu[:, 256:512], op=ALU.mult), waits=[tsg, hid_free] if fc == 0 else [tsg])
                        sg_free[pp] = th; pgu_free[pp] = th
                    xeT_free = tg
                    tyl = None
                    for t2 in range(2):
                        yb = ysb[t2]
                        tcs = []
                        for hf in range(2):
                            py = pbank(4 + ny % 2)
                            ty = None
                            for fc in range(4):
                                ty = T(lambda e, fc=fc, t2=t2, hf=hf, py=py, par=par: e.matmul(py, lhsT=hidT[:, fc, t2 * 128:(t2 + 1) * 128], rhs=Wd[par][:, fc, hf * 512:(hf + 1) * 512],
                                                                                       start=(fc == 0), stop=(fc == 3)),
                                       waits=[th, py_free[ny % 2]] if fc == 0 else [], sig=(fc == 3))
                            tcp = A(lambda e, yb=yb, py=py, hf=hf: e.copy(out=yb[:, hf * 512:(hf + 1) * 512], in_=py), waits=[ty, ysb_free[t2]])
                            py_free[ny % 2] = tcp
                            tcs.append(tcp)
                            ny += 1
                            tyl = ty
                        ysb_free[t2] = P.dma("sync", lambda e, yb=yb, j=j, t2=t2: e.dma_start(out=ybuf[j * BLK + t2 * 128:j * BLK + (t2 + 1) * 128, :], in_=yb),
                                             f"D{25 + t2}", waits=tcs)
                    hid_free = tyl
                    w_free[par] = tyl
                P.barrier()
                cv.reset()
                NCB = 4
                y0 = [cv.get([128, D], F32) for _ in range(NCB)]
                y1 = [cv.get([128, D], F32) for _ in range(NCB)]
                hc = [cv.get([128, D], F32) for _ in range(NCB)]
                cb_free = [None] * NCB
                g_sem = ["D27", "D28", "D19", "D20"]
                h_sem = ["D29", "D30", "D23", "D24"]
                o_sem = ["D31", "D32", "D25", "D26"]
                for ot in range(NOT_):
                    par = ot % NCB
                    tg0 = P.dma("gpsimd", lambda e, ot=ot, par=par: e.indirect_dma_start(
                        out=y0[par], out_offset=None, in_=ybuf, in_offset=bass.IndirectOffsetOnAxis(ap=slot_i[:, ot, 0:1], axis=0),
                        bounds_check=breg(e, NSLOT - 1), oob_is_err=False), g_sem[par], waits=[cb_free[par]])
                    tg1 = P.dma("gpsimd", lambda e, ot=ot, par=par: e.indirect_dma_start(
                        out=y1[par], out_offset=None, in_=ybuf, in_offset=bass.IndirectOffsetOnAxis(ap=slot_i[:, ot, 1:2], axis=0),
                        bounds_check=breg(e, NSLOT - 1), oob_is_err=False), g_sem[par])
                    tlh = P.dma("sync", lambda e, ot=ot, par=par: e.dma_start(out=hc[par], in_=h1buf[ot * 128:(ot + 1) * 128, :]), h_sem[par], waits=[cb_free[par]])
                    V(lambda e, ot=ot, par=par: e.scalar_tensor_tensor(out=hc[par], in0=y0[par], scalar=gate_all[:, ot, 0:1], in1=hc[par], op0=ALU.mult, op1=ALU.add), waits=[tg1, tlh])
                    tv = V(lambda e, ot=ot, par=par: e.scalar_tensor_tensor(out=hc[par], in0=y1[par], scalar=gate_all[:, ot, 1:2], in1=hc[par], op0=ALU.mult, op1=ALU.add))
                    cb_free[par] = P.dma("sync", lambda e, ot=ot, par=par: e.dma_start(out=out_d[ot * 128:(ot + 1) * 128, :], in_=hc[par]), o_sem[par], waits=[tv])
        P.barrier()
        P.ops["sync"].append((None, P._w("sync", []), None, 0))

        with nc.Block() as blk:
            def mk(engname):
                def body(e):
                    waited = {}
                    for fn, waits, sig, inc in P.ops[engname]:
                        for wv in waits:
                            k, v = wv[0], wv[1]
                            if P.owner.get(k) == engname and len(wv) == 2 and engname == "tensor":
                                continue
                            if waited.get(k, 0) >= v:
                                continue
                            e.wait_ge(P.sems[k], v)
                            waited[k] = v
                        if fn is None:
                            continue
                        ins = fn(e)
                        if sig is not None:
                            ins.then_inc(P.sems[sig], inc)
                return body
            blk.sync(mk("sync"))
            blk.scalar(mk("scalar"))
            blk.vector(mk("vector"))
            blk.gpsimd(mk("gpsimd"))
            blk.tensor(mk("tensor"))
    return nc


def _rope_tables(pos):
    inv = (500000.0 ** (-np.arange(0, 16, 2, dtype=np.float32) / np.float32(16))).astype(np.float32)
    ang = pos.astype(np.float32)[:, None] * inv[None, :]
    return np.cos(ang).astype(np.float32), np.sin(ang).astype(np.float32)


def _masks(variant_large):
    m = np.zeros((8, 128, 512), np.float32)
    tri = (np.arange(128)[:, None] <= np.arange(128)[None, :]).astype(np.float32)
    for r in range(8):
        for s in range(4):
            if variant_large:
                if r < 4: v = 1.0
                elif s > r - 4: v = 1.0
                elif s == r - 4: v = tri
                else: v = 0.0
            else:
                if r >= 4: v = 0.0
                elif s > r: v = 1.0
                elif s == r: v = tri
                else: v = 0.0
            m[r, :, s * 128:(s + 1) * 128] = v
    return m


_NC_CACHE = {}


def kernel(x, meta_tokens, norm1_g, w_in, conv_w, q_norm_g, k_norm_g, lambda_q1, lambda_k1, lambda_q2, lambda_k2,
           subln_g, w_out, norm2_g, w_router_group, b_router_group, w_router_expert, b_router_expert,
           w_gate, w_up, w_down):
    x = np.asarray(x, np.float32)
    f = lambda a: np.ascontiguousarray(np.asarray(a, np.float32))
    if "nc" not in _NC_CACHE:
        _NC_CACHE["nc"] = build()
    nc = _NC_CACHE["nc"]
    meta = f(meta_tokens)
    w_r = np.ascontiguousarray(np.concatenate([f(w_router_group)[0], f(w_router_expert)[0]], axis=1))
    b_r = np.ascontiguousarray(np.concatenate([f(b_router_group)[0], f(b_router_expert)[0]], axis=0)[None, :])
    shared = {
        "norm1_g": f(norm1_g), "norm2_g": f(norm2_g), "w_in": f(w_in)[0], "w_out": f(w_out)[0],
        "conv_wT": np.ascontiguousarray(f(conv_w)[0].T), "q_norm_g": f(q_norm_g), "k_norm_g": f(k_norm_g),
        "lambda_q1": f(lambda_q1), "lambda_k1": f(lambda_k1), "lambda_q2": f(lambda_q2), "lambda_k2": f(lambda_k2),
        "subln_g": f(subln_g), "w_r": w_r, "b_r": b_r,
        "w_gate": f(w_gate)[0], "w_up": f(w_up)[0], "w_down": f(w_down)[0],
        "thr": (np.arange(NBLK, dtype=np.float32) * BLK)[None, :],
        "iota": np.arange(32, dtype=np.float32)[None, :],
        "pidx": np.ascontiguousarray(np.concatenate([np.arange(8)[None, :] * 128 + np.arange(128)[:, None],
                                                     np.arange(4)[None, :] * 128 + np.arange(128)[:, None]], 1).astype(np.float32)),
    }
    posA = np.zeros((NT_ALL, 128), np.float32)
    posA[0, :16] = np.arange(16)
    posA[1:] = 16 + np.arange(SEQ).reshape(64, 128)
    cA, sA = _rope_tables(posA.reshape(-1))
    cosA = np.ascontiguousarray(cA.reshape(NT_ALL, 128, 8).transpose(1, 0, 2).reshape(128, -1))
    sinA = np.ascontiguousarray(sA.reshape(NT_ALL, 128, 8).transpose(1, 0, 2).reshape(128, -1))
    mk_l = _masks(True); mk_s = _masks(False)
    in_maps = []
    for c in range(NCORES):
        b, hf = divmod(c, 2)
        xa = np.zeros((NT_ALL * 128, D), np.float32)
        xa[:NMETA] = meta
        xa[128:] = x[b]
        gl = GROUPS[hf]
        xo = np.concatenate([x[b, g * GT:(g + 1) * GT] for g in gl], 0)
        hal = np.zeros((NG * 128, D), np.float32)
        for i, g in enumerate(gl):
            hal[128 * i:128 * i + 2] = meta[14:16] if g == 0 else x[b, g * GT - 2:g * GT]
        posO = np.concatenate([16 + g * GT + np.arange(GT) for g in gl]).astype(np.float32)
        cO, sO = _rope_tables(posO)
        cosO = np.ascontiguousarray(cO.reshape(NOT_, 128, 8).transpose(1, 0, 2).reshape(128, -1))
        sinO = np.ascontiguousarray(sO.reshape(NOT_, 128, 8).transpose(1, 0, 2).reshape(128, -1))
        mm = np.stack([mk_s if hf == 0 else mk_l, mk_l if hf == 0 else mk_s], 0)
        mm = np.ascontiguousarray(mm.reshape(16, 128, 512).transpose(1, 0, 2).reshape(128, -1)).astype(ml_dtypes.bfloat16)
        d = dict(shared)
        d.update({"xall": xa, "xown": np.ascontiguousarray(xo), "xhalo": hal, "cosA": cosA, "sinA": sinA,
                  "cosO": cosO, "sinO": sinO, "masks": mm})
        in_maps.append(d)
    kernel.last_in_maps = in_maps
    if os.environ.get("HK_NORUN") == "1":
        return None
    res = run_bass_kernel_spmd(nc, in_maps, core_ids=list(range(NCORES)))
    kernel.last_results = res.results
    out = np.zeros((4, SEQ, D), np.float32)
    for c in range(NCORES):
        b, hf = divmod(c, 2)
        o = res.results[c]["out"]
        for i, g in enumerate(GROUPS[hf]):
            out[b, g * GT:(g + 1) * GT] = o[i * GT:(i + 1) * GT]
    return out
```

```python
import os
from contextlib import ExitStack
import numpy as np
import ml_dtypes
import concourse.bass as bass
import concourse.mybir as mybir
from concourse.bass_utils import run_bass_kernel_spmd

F32 = mybir.dt.float32
BF16 = mybir.dt.bfloat16
I32 = mybir.dt.int32
ALU = mybir.AluOpType
AF = mybir.ActivationFunctionType
AX = mybir.AxisListType

NCORES = 8
D = 1024
SEQ = 8192
NMETA = 16
NT_ALL = 65
NG = 8
GT = 512
NOWN = NG * GT
NOT_ = NOWN // 128
EPS = 1e-6
NE = 32
NBLK = 64
BLK = 256
NSLOT = NBLK * BLK
GROUPS = [[0, 3, 4, 7, 8, 11, 12, 15], [1, 2, 5, 6, 9, 10, 13, 14]]

STOP_AFTER = os.environ.get("HK_STOP", "")
DEBUG = os.environ.get("HK_DEBUG", "") == "1"
STATIC_E = os.environ.get("HK_STATIC_E", "") == "1"


class Prog:
    ENGS = ["sync", "scalar", "vector", "gpsimd", "tensor"]

    def __init__(self, nc, es):
        self.nc, self.es = nc, es
        self.ops = {e: [] for e in self.ENGS}
        self.sems, self.cnt, self.owner = {}, {}, {}
        for e in self.ENGS[1:]:
            self.newsem("E_" + e, e)
        self.last = {e: None for e in self.ENGS}
        self.pending = {e: [] for e in self.ENGS}

    def barrier(self):
        toks = [self.last[e] for e in self.ENGS[1:] if self.last[e] is not None]
        toks += [(k, v) for k, v in self.cnt.items() if self.owner.get(k) is None and v > 0]
        for e in self.ENGS:
            self.pending[e] = list(toks)

    def _w(self, eng, waits):
        w = self.pending[eng] + flat(list(waits))
        self.pending[eng] = []
        return w

    def newsem(self, name, owner=None):
        self.sems[name] = self.es.enter_context(self.nc.semaphore(name))
        self.cnt[name] = 0
        self.owner[name] = owner
        return name

    SERIAL = ("scalar", "vector", "gpsimd")

    def op(self, eng, fn, waits=(), sig=True, noself=False):
        tok = None
        name = None
        w = self._w(eng, waits)
        if eng in self.SERIAL:
            sig = True
            if self.cnt["E_" + eng] > 0 and not noself:
                w = w + [("E_" + eng, self.cnt["E_" + eng], "self")]
        if sig:
            name = "E_" + eng
            self.cnt[name] += 1
            tok = (name, self.cnt[name])
        self.ops[eng].append((freeze(fn), w, name, 1))
        if tok is not None:
            self.last[eng] = tok
        return tok

    def dma(self, eng, fn, sem, waits=()):
        self.cnt[sem] += 16
        tok = (sem, self.cnt[sem])
        self.ops[eng].append((freeze(fn), self._w(eng, waits), sem, 16))
        return tok

    def replay(self, eng, e):
        waited = {}
        for fn, waits, sig, inc in self.ops[eng]:
            for (k, v) in waits:
                if self.owner.get(k) == eng:
                    continue
                if waited.get(k, 0) >= v:
                    continue
                e.wait_ge(self.sems[k], v)
                waited[k] = v
            ins = fn(e)
            if sig is not None:
                ins.then_inc(self.sems[sig], inc)


import types


def freeze(fn):
    if fn is None or fn.__closure__ is None:
        return fn
    cells = []
    for c in fn.__closure__:
        try:
            cells.append(types.CellType(c.cell_contents))
        except ValueError:
            cells.append(c)
    return types.FunctionType(fn.__code__, fn.__globals__, fn.__name__, fn.__defaults__, tuple(cells))


def flat(toks):
    out = []
    for t in toks:
        if t is None:
            continue
        if isinstance(t, list):
            out.extend(flat(t))
        else:
            out.append(t)
    return out


def build():
    nc = bass.Bass("TRN2", target_bir_lowering=False)
    dt_in = lambda n, s, d=F32: nc.dram_tensor(n, s, d, kind="ExternalInput").ap()
    xall = dt_in("xall", [NT_ALL * 128, D])
    xown = dt_in("xown", [NOWN, D])
    xhalo = dt_in("xhalo", [NG * 128, D])
    cosA = dt_in("cosA", [128, NT_ALL * 8]); sinA = dt_in("sinA", [128, NT_ALL * 8])
    cosO = dt_in("cosO", [128, NOT_ * 8]); sinO = dt_in("sinO", [128, NOT_ * 8])
    masks_d = dt_in("masks", [128, 16 * 512], BF16)
    thr_d = dt_in("thr", [1, NBLK]); iota_d = dt_in("iota", [1, 32]); pidx_d = dt_in("pidx", [128, 12])
    norm1_g = dt_in("norm1_g", [1, D]); norm2_g = dt_in("norm2_g", [1, D])
    w_in = dt_in("w_in", [D, 3072]); w_out = dt_in("w_out", [D, D])
    conv_wT = dt_in("conv_wT", [512, 3])
    q_g = dt_in("q_norm_g", [1, 64]); k_g = dt_in("k_norm_g", [1, 64])
    lq1 = dt_in("lambda_q1", [1, 64]); lk1 = dt_in("lambda_k1", [1, 64])
    lq2 = dt_in("lambda_q2", [1, 64]); lk2 = dt_in("lambda_k2", [1, 64])
    subln_g = dt_in("subln_g", [1, 128])
    w_r = dt_in("w_r", [D, 36]); b_r = dt_in("b_r", [1, 36])
    w_gate = dt_in("w_gate", [NE, D, 512]); w_up = dt_in("w_up", [NE, D, 512]); w_down = dt_in("w_down", [NE, 512, D])
    out_d = nc.dram_tensor("out", [NOWN, D], F32, kind="ExternalOutput").ap()
    scr_kind = "ExternalOutput" if DEBUG else "Internal"
    h1buf = nc.dram_tensor("h1buf", [NOWN, D], F32, kind=scr_kind).ap()
    xn2buf = nc.dram_tensor("xn2buf", [NOWN, D], BF16, kind="Internal").ap()
    otbuf = nc.dram_tensor("otbuf", [128, 4 * NOWN], BF16, kind=scr_kind).ap()
    cacheA = nc.dram_tensor("cacheA", [NT_ALL, 128, D], BF16, kind="Internal").ap()
    cacheO = nc.dram_tensor("cacheO", [NOT_, 128, D], BF16, kind="Internal").ap()
    xebuf = nc.dram_tensor("xebuf", [NSLOT, D], BF16, kind="Internal").ap()
    ybuf = nc.dram_tensor("ybuf", [NSLOT, D], F32, kind="Internal").ap()
    if DEBUG:
        dbg_qt = nc.dram_tensor("dbg_qt", [128, 2 * NOWN], BF16, kind="ExternalOutput").ap()
        dbg_kt = nc.dram_tensor("dbg_kt", [128, 2 * NT_ALL * 128], BF16, kind="ExternalOutput").ap()
        dbg_v = nc.dram_tensor("dbg_v", [128, NT_ALL * 2 * 130], BF16, kind="ExternalOutput").ap()
        dbg_rt = nc.dram_tensor("dbg_rt", [128, 1024], F32, kind="ExternalOutput").ap()

    with ExitStack() as es:
        def sb(name, shape, dt):
            return es.enter_context(nc.sbuf_tensor("s_" + name, shape, dt))
        P = Prog(nc, es)
        for i in range(48):
            P.newsem(f"D{i}")
        ident = sb("ident", [128, 128], BF16)
        Ubf = sb("Ubf", [128, 128], BF16)
        ones_bf = sb("ones_bf", [128, 128], BF16)
        g1_t = sb("g1_t", [128, D], F32)
        gq_t = sb("gq_t", [128, 64], F32)
        gk_t = sb("gk_t", [128, 64], F32)
        gsub_t = sb("gsub_t", [128, 128], F32)
        lam_w = sb("lam_w", [128, 8], F32)
        nlam = sb("nlam", [128, 1], F32)
        EPS_T = sb("EPS_T", [128, 1], F32)
        masks = sb("masks", [128, 16, 512], BF16)
        cosA_t = sb("cosA_t", [128, NT_ALL, 8], F32); sinA_t = sb("sinA_t", [128, NT_ALL, 8], F32)
        cosO_t = sb("cosO_t", [128, NOT_, 8], F32); sinO_t = sb("sinO_t", [128, NOT_, 8], F32)
        Wr = sb("Wr", [128, 8, 36], BF16)
        br_t = sb("br_t", [128, 36], F32)
        convw = sb("convw", [128, 4, 3], F32)
        thr_t = sb("thr_t", [128, NBLK], F32)
        iota_t = sb("iota_t", [128, 32], F32)
        QT = sb("QT", [128, 2, NOWN], BF16)
        eid_all = sb("eid_all", [128, NOT_, 2], F32)
        rank_all = sb("rank_all", [128, NOT_, 2], F32)
        gate_all = sb("gate_all", [128, NOT_, 2], F32)
        slot_f = sb("slot_f", [128, NOT_, 2], F32)
        slot_i = sb("slot_i", [128, NOT_, 2], I32)
        base = sb("base", [128, 32], F32)
        pst = sb("pst", [128, 4, 32], F32)
        pst_i = sb("pst_i", [128, 32], I32)
        Ej_f = sb("Ej_f", [128, NBLK], F32)
        Ej_i = sb("Ej_i", [128, NBLK], I32)
        pidx_t = sb("pidx_t", [128, 12], F32)
        idxW_i = sb("idxW_i", [128, NBLK, 12], I32)
        regs = {}
        def breg(e, bound):
            key = (id(e), bound)
            if key not in regs:
                regs[key] = e.to_reg(bound)
            return regs[key]
        ARENA_B = 118 * 1024
        arena = sb("arena", [128, ARENA_B // 2], BF16)

        class Carver:
            def __init__(self):
                self.off = 0
            def reset(self):
                self.off = 0
            def get(self, shape, dt):
                n = int(np.prod(shape[1:]))
                nb = n * (4 if dt in (F32, I32) else 2)
                nb_al = (nb + 63) // 64 * 64
                a = arena[:, self.off // 2:(self.off + nb) // 2]
                self.off += nb_al
                assert self.off <= ARENA_B, (self.off, ARENA_B)
                if dt != BF16:
                    a = a.bitcast(dt)
                if len(shape) == 3:
                    a = a.rearrange("p (a b) -> p a b", b=shape[2])
                elif len(shape) == 4:
                    a = a.rearrange("p (a b c) -> p a b c", b=shape[2], c=shape[3])
                return a
        cv = Carver()

        psum = es.enter_context(nc.psum_tensor("psum", [128, 4096], F32))
        def pbank(b, nb=1):
            return psum[:, b * 512:(b + nb) * 512]
        def pbank_bf(b, nb=1):
            return psum[:, b * 512:(b + nb) * 512].bitcast(BF16)

        w_in_v = w_in.rearrange("(c p) n -> p c n", p=128)
        w_out_v = w_out.rearrange("(c p) n -> p c n", p=128)
        out_toks = []

        cv.reset()
        ident_f = cv.get([128, 128], F32)
        U_f = cv.get([128, 128], F32)
        lam_in = cv.get([128, 4, 64], F32)
        ztile = cv.get([128, 1024], BF16)
        P.op("gpsimd", lambda e: e.memset(ident_f, 0.0), sig=False)
        P.op("gpsimd", lambda e: e.affine_select(out=ident_f, in_=ident_f, pattern=[[-1, 128]], compare_op=ALU.not_equal,
                                                  fill=1.0, base=0, channel_multiplier=1), sig=False)
        P.op("gpsimd", lambda e: e.memset(U_f, 1.0), sig=False)
        P.op("gpsimd", lambda e: e.affine_select(out=U_f, in_=U_f, pattern=[[1, 128]], compare_op=ALU.is_gt,
                                                  fill=0.0, base=0, channel_multiplier=-1), sig=False)
        P.op("gpsimd", lambda e: e.tensor_copy(out=ident[:], in_=ident_f), sig=False)
        P.op("gpsimd", lambda e: e.tensor_copy(out=Ubf[:], in_=U_f), sig=False)
        P.op("gpsimd", lambda e: e.memset(base[:], 0.0), sig=False)
        P.op("gpsimd", lambda e: e.memset(EPS_T[:], EPS), sig=False)
        P.op("gpsimd", lambda e: e.memset(ones_bf[:], 1.0), sig=False)
        tkz = P.op("gpsimd", lambda e: e.memset(ztile, 0.0))
        def cdma(out, in_):
            P.dma("sync", lambda e, o=out, i=in_: e.dma_start(out=o, in_=i), "D0")
        cdma(g1_t[:], norm1_g.partition_broadcast(128))
        cdma(gq_t[:], q_g.partition_broadcast(128))
        cdma(gk_t[:], k_g.partition_broadcast(128))
        cdma(gsub_t[:], subln_g.partition_broadcast(128))
        cdma(lam_in[:, 0, :], lq1.partition_broadcast(128))
        cdma(lam_in[:, 1, :], lk1.partition_broadcast(128))
        cdma(lam_in[:, 2, :], lq2.partition_broadcast(128))
        cdma(lam_in[:, 3, :], lk2.partition_broadcast(128))
        cdma(masks[:], masks_d.rearrange("p (a b) -> p a b", b=512))
        cdma(cosA_t[:], cosA.rearrange("p (a b) -> p a b", b=8))
        cdma(sinA_t[:], sinA.rearrange("p (a b) -> p a b", b=8))
        cdma(cosO_t[:], cosO.rearrange("p (a b) -> p a b", b=8))
        cdma(sinO_t[:], sinO.rearrange("p (a b) -> p a b", b=8))
        cdma(br_t[:], b_r.partition_broadcast(128))
        cdma(convw[:], conv_wT.rearrange("(c p) k -> p c k", p=128))
        cdma(thr_t[:], thr_d.partition_broadcast(128))
        cdma(iota_t[:], iota_d.partition_broadcast(128))
        cdma(pidx_t[:], pidx_d)
        tk_cd = ("D0", P.cnt["D0"])
        P.dma("gpsimd", lambda e: e.dma_start(out=Wr[:], in_=w_r.rearrange("(c p) n -> p c n", p=128)), "D1")
        P.op("vector", lambda e: e.tensor_tensor(out=lam_in[:, 0, :], in0=lam_in[:, 0, :], in1=lam_in[:, 1, :], op=ALU.mult), waits=[tk_cd], sig=False)
        P.op("vector", lambda e: e.tensor_tensor(out=lam_in[:, 2, :], in0=lam_in[:, 2, :], in1=lam_in[:, 3, :], op=ALU.mult), sig=False)
        P.op("vector", lambda e: e.reduce_sum(out=lam_w[:, 0:1], in_=lam_in[:, 0, :], axis=AX.X), sig=False)
        tk = P.op("vector", lambda e: e.reduce_sum(out=lam_w[:, 1:2], in_=lam_in[:, 2, :], axis=AX.X))
        tk = P.op("scalar", lambda e: e.activation(out=lam_w[:, 2:4], in_=lam_w[:, 0:2], func=AF.Exp), waits=[tk])
        P.op("vector", lambda e: e.tensor_tensor(out=lam_w[:, 4:5], in0=lam_w[:, 3:4], in1=lam_w[:, 2:3], op=ALU.subtract), waits=[tk], sig=False)
        P.op("vector", lambda e: e.tensor_scalar(out=nlam[:], in0=lam_w[:, 4:5], scalar1=-0.2, scalar2=None, op0=ALU.add), sig=False)
        P.op("vector", lambda e: e.tensor_scalar(out=gsub_t[:], in0=gsub_t[:], scalar1=0.8, scalar2=None, op0=ALU.mult))
        xe_v = xebuf.rearrange("(p r) d -> p r d", p=128)
        for q in range(8):
            P.dma("gpsimd", lambda e, q=q: e.dma_start(out=xe_v[:, q * 16:(q + 1) * 16, :],
                                                        in_=ztile.unsqueeze(1).broadcast_to([128, 16, 1024])), "D2", waits=[tkz])
        P.barrier()

        def carve_front():
            cv.reset()
            B = {}
            B["xt"] = [cv.get([128, D], F32) for _ in range(2)]
            B["junk"] = cv.get([128, D], BF16)
            B["xn"] = [cv.get([128, D], BF16) for _ in range(2)]
            B["xnT"] = [cv.get([128, 8, 128], BF16) for _ in range(2)]
            B["st"] = [cv.get([128, 16], F32) for _ in range(2)]
            B["sq2"] = [cv.get([128, 256], F32) for _ in range(2)]
            B["stq"] = [cv.get([128, 16], F32) for _ in range(2)]
            B["t"] = cv.get([128, 512], F32)
            B["rp"] = cv.get([128, 4, 8, 8], F32)
            B["qb"] = [cv.get([128, 512], BF16) for _ in range(2)]
            B["W"] = cv.get([128, 8, 512], BF16)
            return B

        state = {"n": 0, "save_tok": [None, None]}
        def reset_state():
            for k in ("xt_free", "xn_free", "ptr_free"):
                state[k] = [None, None]
            state["sq2_free"] = [None, None]; state["stq_free"] = [None, None]

        def x_front(B, src_rows, dst, ncol, dst_free, xt=None, xt_free=None, xt_sem=None, defer_copy=False, save_to=None, load_from=None, load_sem=None):
            n = state["n"]; state["n"] += 1
            b = n % 2
            if load_from is not None:
                sem = load_sem or f"D{38 + b}"
                tl = P.dma("sync", lambda e: e.dma_start(out=dst, in_=load_from.rearrange("p (c k) -> p c k", k=128)[:, :, 0:ncol]), sem, waits=[dst_free])
                if defer_copy:
                    return (lambda: tl), None, b
                return tl, None, b
            if xt is None:
                xt = B["xt"][b]; xt_free = state["xt_free"][b]; xt_sem = ["D4", "D5"][b]
            xn, st = B["xn"][b], B["st"][b]
            tl = P.dma("sync", lambda e: e.dma_start(out=xt, in_=src_rows), xt_sem, waits=[xt_free])
            P.op("scalar", lambda e: e.activation(out=B["junk"], in_=xt, func=AF.Square, accum_out=st[:, 0:1]), waits=[tl], sig=False)
            t2 = P.op("scalar", lambda e: e.activation(out=st[:, 1:2], in_=st[:, 0:1], func=AF.Sqrt, scale=1.0 / D, bias=EPS_T[:, 0:1]))
            P.op("vector", lambda e: e.reciprocal(out=st[:, 2:3], in_=st[:, 1:2]), waits=[t2], sig=False)
            t3 = P.op("vector", lambda e: e.scalar_tensor_tensor(out=xn, in0=xt, scalar=st[:, 2:3], in1=g1_t[:], op0=ALU.mult, op1=ALU.mult),
                      waits=[state["xn_free"][b]])
            state["xt_free"][b] = t3
            ptr = pbank_bf(b).rearrange("p (a b) -> p a b", b=128)
            tt = None
            for c in range(8):
                tt = P.op("tensor", lambda e, c=c: e.transpose(out=ptr[:, c, :], in_=xn[:, c * 128:(c + 1) * 128], identity=ident[:]),
                          waits=[t3, state["ptr_free"][b]] if c == 0 else [], sig=(c == 7))
            state["xn_free"][b] = tt
            def do_copy():
                t4 = P.op("scalar", lambda e: e.copy(out=dst, in_=ptr[:, :, 0:ncol]), waits=[tt, dst_free, state["save_tok"][b]])
                state["ptr_free"][b] = t4
                if save_to is not None:
                    state["save_tok"][b] = P.dma("sync", lambda e: e.dma_start(out=save_to.rearrange("p (c k) -> p c k", k=128), in_=dst), f"D{40 + b}", waits=[t4])
                return t4
            if defer_copy:
                return do_copy, t3, b
            return do_copy(), t3, b

        def qk_stats_a(B, pin, ncol, waits, par):
            sq = B["sq2"][par]
            return P.op("scalar", lambda e: e.activation(out=sq[:, 0:ncol], in_=pin, func=AF.Square), waits=list(waits) + [state["sq2_free"][par]])

        def qk_stats_b(B, ncol, ta, par):
            nh = ncol // 64
            sq = B["sq2"][par]; stq = B["stq"][par]
            tb = P.op("vector", lambda e: e.reduce_sum(out=stq[:, 0:nh], in_=sq[:, 0:ncol].rearrange("p (a b) -> p a b", b=64), axis=AX.X), waits=[ta, state["stq_free"][par]])
            state["sq2_free"][par] = tb
            tc_ = P.op("scalar", lambda e: e.activation(out=stq[:, 0:nh], in_=stq[:, 0:nh], func=AF.Sqrt, scale=1.0 / 64, bias=EPS_T[:, 0:1]), waits=[tb])
            return P.op("vector", lambda e: e.reciprocal(out=stq[:, 0:nh], in_=stq[:, 0:nh]), waits=[tc_])

        def qk_apply(B, pin, ncol, g_t, cos_t, sin_t, outb, tr, out_free, par):
            nh = ncol // 64
            t, rp = B["t"], B["rp"]
            stq = B["stq"][par]
            t3v = t[:, 0:ncol].rearrange("p (a b) -> p a b", b=64)
            P.op("vector", lambda e: e.tensor_tensor(out=t3v, in0=pin.rearrange("p (a b) -> p a b", b=64),
                                                     in1=stq[:, 0:nh].unsqueeze(2).broadcast_to([128, nh, 64]), op=ALU.mult), waits=[tr], sig=False)
            state["stq_free"][par] = P.op("vector", lambda e: e.tensor_tensor(out=t3v, in0=t3v, in1=g_t[:].unsqueeze(1).broadcast_to([128, nh, 64]), op=ALU.mult), sig=False)
            ob3 = outb.rearrange("p (a b) -> p a b", b=64)
            P.op("vector", lambda e: e.tensor_copy(out=ob3[:, :, 16:64], in_=t3v[:, :, 16:64]), waits=[out_free], sig=False)
            cosb = cos_t.unsqueeze(1).broadcast_to([128, nh, 8]); sinb = sin_t.unsqueeze(1).broadcast_to([128, nh, 8])
            r1 = t3v[:, :, 0:8]; r2 = t3v[:, :, 8:16]
            P.op("vector", lambda e: e.tensor_tensor(out=rp[:, 0, 0:nh, :], in0=r1, in1=cosb, op=ALU.mult), sig=False)
            P.op("vector", lambda e: e.tensor_tensor(out=rp[:, 1, 0:nh, :], in0=r2, in1=sinb, op=ALU.mult), sig=False)
            P.op("vector", lambda e: e.tensor_tensor(out=rp[:, 2, 0:nh, :], in0=r2, in1=cosb, op=ALU.mult), sig=False)
            P.op("vector", lambda e: e.tensor_tensor(out=rp[:, 3, 0:nh, :], in0=r1, in1=sinb, op=ALU.mult), sig=False)
            P.op("vector", lambda e: e.tensor_tensor(out=ob3[:, :, 0:8], in0=rp[:, 0, 0:nh, :], in1=rp[:, 1, 0:nh, :], op=ALU.subtract), sig=False)
            return P.op("vector", lambda e: e.tensor_tensor(out=ob3[:, :, 8:16], in0=rp[:, 2, 0:nh, :], in1=rp[:, 3, 0:nh, :], op=ALU.add))

        n_pass = 2
        for p in range(n_pass):
            h0 = 2 * p
            B = carve_front()
            KT = cv.get([128, 2, NT_ALL * 128], BF16)
            Vs = cv.get([128, NT_ALL, 2, 130], BF16)
            Ebuf = [cv.get([128, 1024], BF16) for _ in range(2)]
            ev_t1 = cv.get([128, 4, 128], F32)
            ev_o = cv.get([128, 4, 128], F32)
            ev_sq = cv.get([128, 128], F32)
            ev_st = cv.get([128, 16], F32)
            ev_ob = cv.get([128, 4, 128], BF16)
            ot_st = [cv.get([128, GT], BF16) for _ in range(2)]
            reset_state()
            tk_w = P.dma("gpsimd", lambda e: e.dma_start(out=B["W"][:, :, 0:256], in_=w_in_v[:, :, 1536 + h0 * 128:1536 + h0 * 128 + 256]), "D3")
            tk_ones = P.op("gpsimd", lambda e: e.memset(Vs[:, :, :, 128:129], 1.0))
            def proj_phase(T, src, ncw, g_t, cos_t, sin_t, dst, is_kv, cache):
                pf = {"proj": [None, None], "tr": None, "qb": [None, None], "xnT": [None, None]}
                info = {}
                def stageF(t):
                    b = state["n"] % 2
                    xnT = B["xnT"][b]
                    cp, _, b = x_front(B, src[t * 128:(t + 1) * 128, :], xnT, 128, pf["xnT"][b], defer_copy=True,
                                       save_to=(cache[t] if p == 0 else None), load_from=(cache[t] if p == 1 else None))
                    info[t] = {"b": b, "xnT": xnT, "cp": cp}
                def stageF2(t):
                    info[t]["t4"] = info[t]["cp"]()
                def stagePa(t):
                    d = info[t]; b = d["b"]; xnT = d["xnT"]
                    pk = pbank(2 + b)
                    tm = None
                    for c in range(8):
                        tm = P.op("tensor", lambda e, c=c, pk=pk, xnT=xnT: e.matmul(pk[:, 0:ncw], lhsT=xnT[:, c, :], rhs=B["W"][:, c, 0:ncw], start=(c == 0), stop=(c == 7)),
                                  waits=[d["t4"], tk_w, pf["proj"][b]] if c == 0 else [], sig=(c == 7))
                    pf["xnT"][b] = tm
                    d["tm"] = tm; d["pk"] = pk
                    d["tv"] = None
                def stagePb(t):
                    d = info[t]; pk = d["pk"]
                    if is_kv:
                        d["tv"] = P.op("scalar", lambda e, t=t, pk=pk: e.copy(out=Vs[:, t, :, 0:128], in_=pk[:, 256:512].rearrange("p (a b) -> p a b", b=128)), waits=[d["tm"]])
                def stageN1a(t):
                    d = info[t]; b = d["b"]
                    d["ta"] = qk_stats_a(B, d["pk"][:, 0:256], 256, [d["tm"], d["tv"]], b)
                def stageN1b(t):
                    d = info[t]; b = d["b"]
                    d["tr"] = qk_stats_b(B, 256, d["ta"], b)
                def stageN2(t):
                    d = info.pop(t); b = d["b"]; pk = d["pk"]
                    kb = B["qb"][b][:, 0:256]
                    tq = qk_apply(B, pk[:, 0:256], 256, g_t, cos_t[:, t, :], sin_t[:, t, :], kb, d["tr"], pf["qb"][b], b)
                    pf["proj"][b] = [tq, d["tv"]]
                    pkt = pbank_bf(4).rearrange("p (a b) -> p a b", b=128)[:, 0:2, :]
                    tt = None
                    for hl in range(2):
                        tt = P.op("tensor", lambda e, hl=hl, kb=kb: e.transpose(out=pkt[:, hl, :], in_=kb[:, hl * 128:(hl + 1) * 128], identity=ident[:]),
                                  waits=[tq, pf["tr"]] if hl == 0 else [], sig=(hl == 1))
                    pf["qb"][b] = tt
                    pf["tr"] = P.op("vector", lambda e, t=t: e.tensor_copy(out=dst[:, :, t * 128:(t + 1) * 128], in_=pkt), waits=[tt])
                for k in range(T + 2):
                    if 0 <= k - 1 < T:
                        stagePa(k - 1)
                    if k < T:
                        stageF(k)
                    if 0 <= k - 1 < T:
                        stagePb(k - 1)
                    if p == 0:
                        if 0 <= k - 2 < T:
                            stageN1a(k - 2); stageN1b(k - 2); stageN2(k - 2)
                    else:
                        if 0 <= k - 1 < T:
                            stageN1a(k - 1)
                        if 0 <= k - 2 < T:
                            stageN2(k - 2)
                        if 0 <= k - 1 < T:
                            stageN1b(k - 1)
                    if k < T:
                        stageF2(k)
                return pf["tr"]
            qtr_free = proj_phase(NOT_, xown, 256, gq_t, cosO_t, sinO_t, QT, False, cacheO)
            if DEBUG and p == 0:
                out_toks.append(P.dma("sync", lambda e: e.dma_start(out=dbg_qt, in_=QT[:].rearrange("p a b -> p (a b)")), "D6", waits=[qtr_free]))
            P.barrier()
            if STOP_AFTER == "O":
                break
            P.dma("gpsimd", lambda e: e.dma_start(out=B["W"][:, :, 0:256], in_=w_in_v[:, :, 2048 + h0 * 128:2048 + h0 * 128 + 256]), "D3")
            tk_w = P.dma("gpsimd", lambda e: e.dma_start(out=B["W"][:, :, 256:512], in_=w_in_v[:, :, 2560 + h0 * 128:2560 + h0 * 128 + 256]), "D3")
            reset_state()
            proj_phase(int(os.environ.get("HK_NTA", NT_ALL)), xall, 512, gk_t, cosA_t, sinA_t, KT, True, cacheA)
            if DEBUG and p == 0:
                P.barrier()
                out_toks.append(P.dma("sync", lambda e: e.dma_start(out=dbg_kt, in_=KT.rearrange("p a b -> p (a b)")), "D6"))
                out_toks.append(P.dma("sync", lambda e: e.dma_start(out=dbg_v, in_=Vs.rearrange("p a b c -> p (a b c)")), "D6"))
            P.barrier()
            if STOP_AFTER == "A":
                break
            accs = []
            for a in range(8):
                bk, r = divmod(a, 3)
                accs.append(psum[:, (4 + bk) * 512 + r * 132:(4 + bk) * 512 + r * 132 + 129])
            AST = {"S_free": [None, None], "E_free": [None, None], "acc_free": None, "otr_free": None, "n_ot": 0,
                   "ot_st_free": [None, None]}
            def emit_S_exp(i, hl, u, un):
                nkb = 8 * i + 8
                si = un % 2
                nk = 16 if u == 0 else 128
                ps = pbank(2 * si, 2)
                ts = None
                for m in range(2):
                    ts = P.op("tensor", lambda e, m=m, ps=ps, u=u, nk=nk, hl=hl, i=i: e.matmul(
                        ps[0:nk, m * 512:(m + 1) * 512], lhsT=KT[m * 64:(m + 1) * 64, hl, u * 128:u * 128 + nk],
                        rhs=QT[m * 64:(m + 1) * 64, hl, i * GT:(i + 1) * GT], start=True, stop=True),
                        waits=[AST["S_free"][si]] if m == 0 else [], sig=(m == 1))
                Eb = Ebuf[si]
                te = P.op("scalar", lambda e, ps=ps, Eb=Eb, nk=nk: e.activation(out=Eb[0:nk, :], in_=ps[0:nk, :], func=AF.Exp, scale=0.125),
                          waits=[ts, AST["E_free"][si]], noself=True)
                AST["S_free"][si] = te
                if u > nkb - 8:
                    r = u - 1 - (nkb - 8)
                    mi = (i % 2) * 8 + r
                    te = P.op("vector", lambda e, Eb=Eb, mi=mi: e.tensor_tensor(
                        out=Eb.rearrange("p (a b) -> p a b", b=512), in0=Eb.rearrange("p (a b) -> p a b", b=512),
                        in1=masks[:, mi, :].unsqueeze(1).broadcast_to([128, 2, 512]), op=ALU.mult), waits=[te])
                return {"i": i, "hl": hl, "u": u, "si": si, "nk": nk, "Eb": Eb, "te": te, "nkb": nkb}

            def emit_PV(d):
                i, hl, u, si, nk, Eb, te, nkb = d["i"], d["hl"], d["u"], d["si"], d["nk"], d["Eb"], d["te"], d["nkb"]
                tp = None
                for m in range(2):
                    for s_ in range(4):
                        a = m * 4 + s_
                        tp = P.op("tensor", lambda e, a=a, m=m, s_=s_, Eb=Eb, nk=nk, u=u, hl=hl, nkb=nkb: e.matmul(
                            accs[a], lhsT=Eb[0:nk, m * 512 + s_ * 128:m * 512 + (s_ + 1) * 128], rhs=Vs[0:nk, u, hl, 0:129],
                            start=(u == 0 and a % 3 == 0), stop=(u == nkb), skip_group_check=True),
                            waits=[te, AST["acc_free"] if u == 0 else None] if a == 0 else [], sig=(a == 7))
                AST["E_free"][si] = tp
                if u == nkb:
                    emit_evac(i, hl, tp)

            def emit_evac(i, hl, last_pv):
                h = 2 * p + hl
                st = ev_st
                acc_free = None
                for s in range(4):
                    a1, a2 = accs[s], accs[4 + s]
                    P.op("vector", lambda e, a1=a1, s=s: e.reciprocal(out=st[:, s:s + 1], in_=a1[:, 128:129]), waits=[last_pv] if s == 0 else [], sig=False)
                    P.op("vector", lambda e, a2=a2, s=s: e.reciprocal(out=st[:, 4 + s:5 + s], in_=a2[:, 128:129]), sig=False)
                    P.op("vector", lambda e, s=s: e.tensor_tensor(out=st[:, 4 + s:5 + s], in0=st[:, 4 + s:5 + s], in1=nlam[:], op=ALU.mult), sig=False)
                    P.op("vector", lambda e, a1=a1, s=s: e.tensor_scalar(out=ev_t1[:, s, :], in0=a1[:, 0:128], scalar1=st[:, s:s + 1], scalar2=None, op0=ALU.mult), sig=False)
                    acc_free = P.op("vector", lambda e, a2=a2, s=s: e.scalar_tensor_tensor(out=ev_o[:, s, :], in0=a2[:, 0:128], scalar=st[:, 4 + s:5 + s], in1=ev_t1[:, s, :],
                                                                                            op0=ALU.mult, op1=ALU.add), sig=False)
                AST["acc_free"] = acc_free
                tss = None
                for s in range(4):
                    P.op("vector", lambda e, s=s: e.tensor_tensor(out=ev_sq, in0=ev_o[:, s, :], in1=ev_o[:, s, :], op=ALU.mult), sig=False)
                    tss = P.op("vector", lambda e, s=s: e.reduce_sum(out=st[:, 8 + s:9 + s], in_=ev_sq, axis=AX.X))
                tsq = P.op("scalar", lambda e: e.activation(out=st[:, 8:12], in_=st[:, 8:12], func=AF.Sqrt, scale=1.0 / 128, bias=EPS_T[:, 0:1]), waits=[tss])
                P.op("vector", lambda e: e.reciprocal(out=st[:, 8:12], in_=st[:, 8:12]), waits=[tsq], sig=False)
                tob = None
                for s in range(4):
                    tob = P.op("vector", lambda e, s=s: e.scalar_tensor_tensor(out=ev_ob[:, s, :], in0=ev_o[:, s, :], scalar=st[:, 8 + s:9 + s], in1=gsub_t[:],
                                                                                op0=ALU.mult, op1=ALU.mult), waits=[AST["otr_free"]] if s == 0 else [])
                pot = pbank_bf(7).rearrange("p (a b) -> p a b", b=128)[:, 0:4, :]
                tt = None
                for s in range(4):
                    tt = P.op("tensor", lambda e, s=s: e.transpose(out=pot[:, s, :], in_=ev_ob[:, s, :], identity=ident[:]),
                              waits=[tob, AST["otr_free"]] if s == 0 else [], sig=(s == 3))
                n_ot = AST["n_ot"]
                osb = ot_st[n_ot % 2]
                otr = P.op("vector", lambda e, osb=osb: e.tensor_copy(out=osb, in_=pot.rearrange("p a b -> p (a b)")), waits=[tt, AST["ot_st_free"][n_ot % 2]])
                AST["otr_free"] = otr
                AST["ot_st_free"][n_ot % 2] = P.dma("sync", lambda e, osb=osb, h=h, i=i: e.dma_start(out=otbuf[:, h * NOWN + i * GT:h * NOWN + (i + 1) * GT], in_=osb),
                                                    f"D{33 + n_ot % 2}", waits=[otr])
                AST["n_ot"] = n_ot + 1

            units = [(i, hl, u) for i in range(NG) for hl in range(2) for u in range(8 * i + 9)]
            prev = None
            for un, (i, hl, u) in enumerate(units):
                d = emit_S_exp(i, hl, u, un)
                if prev is not None:
                    emit_PV(prev)
                prev = d
            emit_PV(prev)
            P.barrier()
        if STOP_AFTER not in ("O", "A", "B"):
            V = lambda fn, waits=(): P.op("vector", fn, waits)
            A = lambda fn, waits=(): P.op("scalar", fn, waits)
            T = lambda fn, waits=(), sig=True: P.op("tensor", fn, waits, sig)
            cv.reset()
            xt4 = cv.get([128, 4, D], F32)
            B = {}
            B["junk"] = cv.get([128, D], BF16)
            B["xn"] = [cv.get([128, D], BF16) for _ in range(2)]
            B["st"] = [cv.get([128, 16], F32) for _ in range(2)]
            hal_xt = cv.get([128, D], F32)
            xnTg = cv.get([128, 8, 516], BF16)
            Wc = cv.get([128, 8, 1536], BF16)
            Wo = cv.get([128, 8, D], BF16)
            g2_t = cv.get([128, D], F32)
            cc = cv.get([128, 516], F32)
            z = cv.get([128, 516], F32)
            yv = cv.get([128, 512], F32)
            mixc = cv.get([128, 4, 512], BF16)
            h1 = [cv.get([128, D], F32) for _ in range(2)]
            xn2 = [cv.get([128, D], BF16) for _ in range(2)]
            xn2T = cv.get([128, 8, 128], BF16)
            R = cv.get([128, 1024], F32)
            OTg = [cv.get([128, 4, GT], BF16) for _ in range(2)]
            otg_free = [None, None]
            st2 = cv.get([128, 8], F32)
            ohb = cv.get([128, 4, 32], BF16)
            reset_state()
            P.dma("gpsimd", lambda e: e.dma_start(out=Wc, in_=w_in_v[:, :, 0:1536]), "D16")
            P.dma("gpsimd", lambda e: e.dma_start(out=Wo, in_=w_out_v), "D16")
            tk_g2 = P.dma("sync", lambda e: e.dma_start(out=g2_t, in_=norm2_g.partition_broadcast(128)), "D37")
            tk_wc = [("D16", P.cnt["D16"]), tk_g2]
            ps5 = pbank(5)
            ps_rt = pbank_bf(2).rearrange("p (a b) -> p a b", b=128)
            rtc_free = None; xn2T_free = None
            xt4_tok = [None] * 4
            xt4_free = [None] * 4; hal_free = None; grp_free = None; conv_free = None
            ph_free = [None, None]; h1_free = [None, None]; xn2_free = [None, None]; rt_free = None
            nph = 0
            tl_last = None
            def emit_front(i):
                nonlocal hal_free
                fr = []
                t4, t3, _ = x_front(B, xhalo[i * 128:(i + 1) * 128, :], xnTg[:, :, 0:2], 2, grp_free, xt=hal_xt, xt_free=hal_free, xt_sem="D11")
                hal_free = t3; fr.append(t4)
                for s in range(4):
                    t4, _, _ = x_front(B, None, xnTg[:, :, 2 + s * 128:2 + (s + 1) * 128], 128, grp_free, load_from=cacheO[4 * i + s], load_sem=f"D{42 + s}")
                    fr.append(t4)
                return fr
            fr_next = emit_front(0)
            for i in range(NG):
                fr = fr_next
                for s in range(4):
                    xt4_tok[s] = P.dma("sync", lambda e, s=s, i=i: e.dma_start(out=xt4[:, s, :], in_=xown[(4 * i + s) * 128:(4 * i + s + 1) * 128, :]),
                                       f"D{7 + s}", waits=[xt4_free[s]])
                OTc = OTg[i % 2]
                tk_otg = P.dma("sync", lambda e, OTc=OTc, i=i: e.dma_start(out=OTc, in_=otbuf.rearrange("p (h t) -> p h t", h=4)[:, :, i * GT:(i + 1) * GT]),
                               f"D{35 + i % 2}", waits=[otg_free[i % 2]])
                tmix = None
                tm = None
                for q in range(4):
                    for (blkc, dstp, hcol) in ((q, pbank(2), None), (4 + q, pbank(3), 0), (8 + q, pbank(4), 2)):
                        for c in range(8):
                            tm = T(lambda e, c=c, blkc=blkc, dstp=dstp: e.matmul(dstp, lhsT=Wc[:, c, blkc * 128:(blkc + 1) * 128], rhs=xnTg[:, c, 2:514],
                                                                                 start=(c == 0), stop=(c == 7)),
                                   waits=fr + [tk_wc, conv_free, rt_free] if c == 0 else [], sig=(c == 7))
                        if hcol is not None:
                            for c in range(8):
                                tm = T(lambda e, c=c, blkc=blkc, hcol=hcol: e.matmul(ps5[:, hcol:hcol + 2], lhsT=Wc[:, c, blkc * 128:(blkc + 1) * 128], rhs=xnTg[:, c, 0:2],
                                                                                     start=(c == 0), stop=(c == 7)), sig=(c == 7))
                    A(lambda e: e.copy(out=cc[:, 2:514], in_=pbank(3)), waits=[tm])
                    ta = A(lambda e: e.copy(out=cc[:, 0:2], in_=ps5[:, 0:2]))
                    V(lambda e: e.tensor_tensor(out=z[:, 2:514], in0=cc[:, 2:514], in1=pbank(4), op=ALU.mult), waits=[ta, tm])
                    V(lambda e: e.tensor_tensor(out=z[:, 0:2], in0=cc[:, 0:2], in1=ps5[:, 2:4], op=ALU.mult))
                    V(lambda e, q=q: e.tensor_scalar(out=yv, in0=z[:, 0:512], scalar1=convw[:, q, 0:1], scalar2=None, op0=ALU.mult))
                    V(lambda e, q=q: e.scalar_tensor_tensor(out=yv, in0=z[:, 1:513], scalar=convw[:, q, 1:2], in1=yv, op0=ALU.mult, op1=ALU.add))
                    V(lambda e, q=q: e.scalar_tensor_tensor(out=yv, in0=z[:, 2:514], scalar=convw[:, q, 2:3], in1=yv, op0=ALU.mult, op1=ALU.add))
                    conv_free = V(lambda e, q=q: e.tensor_tensor(out=mixc[:, q, :], in0=pbank(2), in1=yv, op=ALU.mult), waits=[grp_free])
                tmix = conv_free
                grp_free = tm
                if i + 1 < NG:
                    fr_next = emit_front(i + 1)
                CS = {}
                def Wstage(s):
                    nonlocal nph
                    ot = 4 * i + s
                    par = ot % 2
                    hb = h1[par]; xb = xn2[par]
                    for hf in range(2):
                        ph = pbank(6 + nph % 2); pfree = ph_free[nph % 2]
                        tw = None
                        for kk in range(8):
                            lhsT = mixc[:, kk, s * 128:(s + 1) * 128] if kk < 4 else OTc[:, kk - 4, s * 128:(s + 1) * 128]
                            tw = T(lambda e, kk=kk, lhsT=lhsT, ph=ph, hf=hf: e.matmul(ph, lhsT=lhsT, rhs=Wo[:, kk, hf * 512:(hf + 1) * 512], start=(kk == 0), stop=(kk == 7)),
                                   waits=[tmix, pfree, tk_otg] if kk == 0 else [], sig=(kk == 7))
                        th = V(lambda e, hb=hb, ph=ph, s=s, hf=hf: e.tensor_tensor(out=hb[:, hf * 512:(hf + 1) * 512], in0=ph, in1=xt4[:, s, hf * 512:(hf + 1) * 512], op=ALU.add),
                               waits=[tw, h1_free[par], xt4_tok[s]])
                        ph_free[nph % 2] = th
                        nph += 1
                    xt4_free[s] = th
                    if s == 3:
                        otg_free[i % 2] = tw
                    A(lambda e, hb=hb: e.activation(out=B["junk"], in_=hb, func=AF.Square, accum_out=st2[:, 0:1]), waits=[th])
                    ta = A(lambda e: e.activation(out=st2[:, 1:2], in_=st2[:, 0:1], func=AF.Sqrt, scale=1.0 / D, bias=EPS_T[:, 0:1]))
                    V(lambda e: e.reciprocal(out=st2[:, 2:3], in_=st2[:, 1:2]), waits=[ta])
                    tx = V(lambda e, hb=hb, xb=xb: e.scalar_tensor_tensor(out=xb, in0=hb, scalar=st2[:, 2:3], in1=g2_t, op0=ALU.mult, op1=ALU.mult), waits=[xn2_free[par]])
                    d1 = P.dma("sync", lambda e, hb=hb, ot=ot: e.dma_start(out=h1buf[ot * 128:(ot + 1) * 128, :], in_=hb), f"D{12 + par}", waits=[th])
                    d2 = P.dma("sync", lambda e, xb=xb, ot=ot: e.dma_start(out=xn2buf[ot * 128:(ot + 1) * 128, :], in_=xb), f"D{14 + par}", waits=[tx])
                    h1_free[par] = [d1, tx]
                    CS[s] = {'tx': tx, 'xb': xb, 'par': par, 'd2': d2}
                def Rstage(s):
                    nonlocal rtc_free, xn2T_free, tl_last
                    c_ = CS[s]; tx = c_['tx']; xb = c_['xb']; par = c_['par']; d2 = c_['d2']
                    tt = None
                    for c in range(8):
                        tt = T(lambda e, c=c, xb=xb: e.transpose(out=ps_rt[:, c, :], in_=xb[:, c * 128:(c + 1) * 128], identity=ident[:]),
                               waits=[tx, rtc_free, conv_free] if c == 0 else [], sig=(c == 7))
                    rtc_free = A(lambda e: e.copy(out=xn2T, in_=ps_rt), waits=[tt, xn2T_free])
                    xn2_free[par] = [d2, tt]
                    tl = None
                    for c in range(8):
                        tl = T(lambda e, c=c, s=s: e.matmul(ps5[:, 16 + 36 * s:52 + 36 * s], lhsT=xn2T[:, c, :], rhs=Wr[:, c, :], start=(c == 0), stop=(c == 7)),
                               waits=[rtc_free, rt_free] if c == 0 else [], sig=(c == 7))
                    xn2T_free = tl
                    tl_last = tl
                for step in (('W', 0), ('W', 1), ('R', 0), ('W', 2), ('R', 1), ('W', 3), ('R', 2), ('R', 3)):
                    (Wstage if step[0] == 'W' else Rstage)(step[1])
                tl = tl_last
                o4 = 4 * i
                def R3(a, n, k):
                    return R[:, a:a + 4 * k].rearrange("p (s k) -> p s k", k=k)
                def R2(a):
                    return R[:, a:a + 4]
                def bc(ap2, k):
                    return ap2.unsqueeze(2).broadcast_to([128, 4, k])
                LG = R3(0, 4, 36)
                V(lambda e: e.tensor_tensor(out=LG, in0=ps5[:, 16:160].rearrange("p (s k) -> p s k", k=36), in1=br_t[:].unsqueeze(1).broadcast_to([128, 4, 36]), op=ALU.add), waits=[tl])
                LGg = LG[:, :, 0:4]
                V(lambda e: e.tensor_reduce(out=R2(144), in_=LGg, axis=AX.X, op=ALU.max))
                V(lambda e: e.tensor_tensor(out=R3(148, 4, 4), in0=LGg, in1=bc(R2(144), 4), op=ALU.is_equal))
                tg = V(lambda e: e.tensor_tensor(out=R3(164, 4, 4), in0=LGg, in1=bc(R2(144), 4), op=ALU.subtract))
                tge = A(lambda e: e.activation(out=R[:, 180:196], in_=R[:, 164:180], func=AF.Exp), waits=[tg])
                V(lambda e: e.reduce_sum(out=R2(196), in_=R3(180, 4, 4), axis=AX.X), waits=[tge])
                V(lambda e: e.reciprocal(out=R2(200), in_=R2(196)))
                V(lambda e: e.tensor_tensor(out=R3(164, 4, 4), in0=R3(148, 4, 4), in1=iota_t[:, 0:4].unsqueeze(1).broadcast_to([128, 4, 4]), op=ALU.mult))
                V(lambda e: e.reduce_sum(out=R2(204), in_=R3(164, 4, 4), axis=AX.X))
                PR = R[:, 208:336].rearrange("p (s g j) -> p s g j", g=4, j=8)
                V(lambda e: e.tensor_tensor(out=PR, in0=LG[:, :, 4:36].rearrange("p s (g j) -> p s g j", j=8),
                                            in1=R3(148, 4, 4).unsqueeze(3).broadcast_to([128, 4, 4, 8]), op=ALU.mult))
                ES = R3(336, 4, 8)
                V(lambda e: e.reduce_sum(out=ES, in_=PR.rearrange("p s g j -> p s j g"), axis=AX.X))
                T8 = R3(368, 4, 8)
                for s in range(4):
                    V(lambda e, s=s: e.max(out=T8[:, s, :], in_=ES[:, s, :]))
                OH = [R3(400, 4, 8), R3(432, 4, 8)]
                for k in range(2):
                    V(lambda e, k=k: e.tensor_tensor(out=OH[k], in0=ES, in1=T8[:, :, k:k + 1].broadcast_to([128, 4, 8]), op=ALU.is_equal))
                    V(lambda e, k=k: e.tensor_tensor(out=R3(464, 4, 8), in0=OH[k], in1=iota_t[:, 0:8].unsqueeze(1).broadcast_to([128, 4, 8]), op=ALU.mult))
                    V(lambda e, k=k: e.reduce_sum(out=R2(496 + 4 * k), in_=R3(464, 4, 8), axis=AX.X))
                    V(lambda e, k=k: e.scalar_tensor_tensor(out=eid_all[:, o4:o4 + 4, k], in0=R2(204), scalar=8.0, in1=R2(496 + 4 * k), op0=ALU.mult, op1=ALU.add))
                td = V(lambda e: e.tensor_tensor(out=R2(504), in0=T8[:, :, 1], in1=T8[:, :, 0], op=ALU.subtract))
                tex = A(lambda e: e.activation(out=R2(508), in_=R2(504), func=AF.Exp), waits=[td])
                V(lambda e: e.tensor_scalar(out=R2(512), in0=R2(508), scalar1=1.0, scalar2=None, op0=ALU.add), waits=[tex])
                V(lambda e: e.reciprocal(out=R2(516), in_=R2(512)))
                V(lambda e: e.tensor_tensor(out=gate_all[:, o4:o4 + 4, 0], in0=R2(200), in1=R2(516), op=ALU.mult))
                V(lambda e: e.tensor_tensor(out=gate_all[:, o4:o4 + 4, 1], in0=R2(200), in1=gate_all[:, o4:o4 + 4, 0], op=ALU.subtract))
                O32 = [R3(520, 4, 32), R3(648, 4, 32)]
                for k in range(2):
                    V(lambda e, k=k: e.tensor_tensor(out=O32[k], in0=iota_t[:].unsqueeze(1).broadcast_to([128, 4, 32]),
                                                     in1=eid_all[:, o4:o4 + 4, k:k + 1].broadcast_to([128, 4, 32]), op=ALU.is_equal))
                toh = V(lambda e: e.tensor_tensor(out=ohb, in0=O32[0], in1=O32[1], op=ALU.add))
                tcn = None
                for s in range(4):
                    T(lambda e, s=s: e.matmul(ps5[:, 160 + 32 * s:192 + 32 * s], lhsT=Ubf[:], rhs=ohb[:, s, :], start=True, stop=True), waits=[toh] if s == 0 else [], sig=False)
                    tcn = T(lambda e, s=s: e.matmul(ps5[:, 288 + 32 * s:320 + 32 * s], lhsT=ones_bf[:], rhs=ohb[:, s, :], start=True, stop=True))
                BV = R3(776, 4, 32)
                V(lambda e: e.tensor_copy(out=BV[:, 0, :], in_=base[:]), waits=[tcn])
                for s in range(1, 4):
                    V(lambda e, s=s: e.tensor_tensor(out=BV[:, s, :], in0=BV[:, s - 1, :], in1=ps5[:, 288 + 32 * (s - 1):320 + 32 * (s - 1)], op=ALU.add))
                V(lambda e: e.tensor_tensor(out=base[:], in0=BV[:, 3, :], in1=ps5[:, 384:416], op=ALU.add))
                V(lambda e: e.tensor_tensor(out=BV, in0=BV, in1=ps5[:, 160:288].rearrange("p (s k) -> p s k", k=32), op=ALU.add))
                for k in range(2):
                    V(lambda e, k=k: e.tensor_tensor(out=O32[k], in0=O32[k], in1=BV, op=ALU.mult))
                    rt_free = V(lambda e, k=k: e.reduce_sum(out=rank_all[:, o4:o4 + 4, k], in_=O32[k], axis=AX.X))
            if DEBUG:
                V(lambda e: e.tensor_copy(out=R[:, 0:64], in_=eid_all[:].rearrange("p a b -> p (a b)")))
            cv.reset()
            cmpn = cv.get([128, 32, 32], F32)
            V(lambda e: e.tensor_tensor(out=cmpn, in0=base[:].unsqueeze(2).broadcast_to([128, 32, 32]),
                                        in1=thr_t[:, 0:32].unsqueeze(1).broadcast_to([128, 32, 32]), op=ALU.is_gt))
            V(lambda e: e.reduce_sum(out=pst[:, 3, :], in_=cmpn, axis=AX.X))
            V(lambda e: e.tensor_scalar(out=pst[:, 0, :], in0=pst[:, 3, :], scalar1=256.0, scalar2=None, op0=ALU.mult))
            V(lambda e: e.tensor_copy(out=pst[:, 1, 0:1], in_=pst[:, 0, 0:1]))
            for ee in range(1, 32):
                V(lambda e, ee=ee: e.tensor_tensor(out=pst[:, 1, ee:ee + 1], in0=pst[:, 1, ee - 1:ee], in1=pst[:, 0, ee:ee + 1], op=ALU.add))
            V(lambda e: e.tensor_tensor(out=pst[:, 2, :], in0=pst[:, 1, :], in1=pst[:, 0, :], op=ALU.subtract))
            cmp3 = cv.get([128, NBLK, 32], F32)
            V(lambda e: e.tensor_tensor(out=cmp3, in0=pst[:, 1, :].unsqueeze(1).broadcast_to([128, NBLK, 32]),
                                        in1=thr_t[:].unsqueeze(2).broadcast_to([128, NBLK, 32]), op=ALU.is_le))
            V(lambda e: e.reduce_sum(out=Ej_f[:], in_=cmp3, axis=AX.X))
            V(lambda e: e.tensor_scalar(out=Ej_f[:], in0=Ej_f[:], scalar1=31.0, scalar2=None, op0=ALU.min))
            tk_ej = V(lambda e: e.tensor_copy(out=Ej_i[:], in_=Ej_f[:]))
            unused = cv.get([128, NBLK], F32)
            V(lambda e: e.tensor_scalar(out=unused, in0=thr_t[:], scalar1=pst[:, 1, 31:32], scalar2=None, op0=ALU.is_ge))
            V(lambda e: e.scalar_tensor_tensor(out=Ej_f[:], in0=unused, scalar=8192.0, in1=Ej_f[:], op0=ALU.mult, op1=ALU.add))
            idxW_f = cv.get([128, NBLK, 12], F32)
            for c in range(12):
                V(lambda e, c=c: e.tensor_scalar(out=idxW_f[:, :, c], in0=Ej_f[:], scalar1=(128.0 if c < 8 else 512.0), scalar2=pidx_t[:, c:c + 1],
                                                 op0=ALU.mult, op1=ALU.add))
            tk_ej = V(lambda e: e.tensor_copy(out=idxW_i[:], in_=idxW_f))
            for ot in range(NOT_):
                for k in range(2):
                    V(lambda e, ot=ot, k=k: e.tensor_scalar(out=R[:, 96:128], in0=iota_t[:], scalar1=eid_all[:, ot, k:k + 1], scalar2=None, op0=ALU.is_equal))
                    V(lambda e: e.tensor_tensor(out=R[:, 96:128], in0=R[:, 96:128], in1=pst[:, 2, :], op=ALU.mult))
                    V(lambda e: e.reduce_sum(out=R[:, 240:241], in_=R[:, 96:128], axis=AX.X))
                    V(lambda e, ot=ot, k=k: e.tensor_tensor(out=slot_f[:, ot, k:k + 1], in0=R[:, 240:241], in1=rank_all[:, ot, k:k + 1], op=ALU.add))
            tk_slot = V(lambda e: e.tensor_copy(out=slot_i[:], in_=slot_f[:]))
            if DEBUG:
                V(lambda e: e.tensor_copy(out=R[:, 64:128], in_=slot_f[:].rearrange("p a b -> p (a b)")))
                V(lambda e: e.tensor_copy(out=R[:, 128:192], in_=gate_all[:].rearrange("p a b -> p (a b)")))
                V(lambda e: e.tensor_copy(out=R[:, 192:256], in_=Ej_f[:]))
                out_toks.append(P.dma("sync", lambda e: e.dma_start(out=dbg_rt[:, 0:256], in_=R), "D6", waits=[P.last["vector"]]))
            P.barrier()
            cv.reset()
            NXS = 4
            xs = [cv.get([128, D], BF16) for _ in range(NXS)]
            xs_free = [None] * NXS
            xs_sem = ["D17", "D18", "D38", "D39"]
            sc_sem = ["D19", "D20", "D46", "D47"]
            for ot in range(NOT_):
                par = ot % NXS
                tl = P.dma("sync", lambda e, ot=ot, par=par: e.dma_start(out=xs[par], in_=xn2buf[ot * 128:(ot + 1) * 128, :]), xs_sem[par], waits=[xs_free[par]])
                tsc = None
                for k in range(2):
                    tsc = P.dma("gpsimd", lambda e, ot=ot, k=k, par=par: e.indirect_dma_start(
                        out=xebuf, out_offset=bass.IndirectOffsetOnAxis(ap=slot_i[:, ot, k:k + 1], axis=0), in_=xs[par], in_offset=None,
                        bounds_check=breg(e, NSLOT - 1), oob_is_err=False), sc_sem[par], waits=[tl])
                xs_free[par] = tsc
            P.barrier()
            if STOP_AFTER != "C":
                cv.reset()
                Wg = [cv.get([128, 8, 512], BF16) for _ in range(2)]
                Wu = [cv.get([128, 8, 512], BF16) for _ in range(2)]
                Wd = [cv.get([128, 4, D], BF16) for _ in range(2)]
                xe = [cv.get([128, 2, D], BF16) for _ in range(2)]
                xeT = cv.get([128, 8, 256], BF16)
                sg = [cv.get([128, 256], F32) for _ in range(2)]
                hidT = cv.get([128, 4, 256], BF16)
                ysb = [cv.get([128, D], F32) for _ in range(2)]
                w_free = [None, None]; xe_free = [None, None]; xeT_free = None; pxe_free = None
                sg_free = [None, None]; pgu_free = [None, None]; hid_free = None; py_free = [None, None]; ysb_free = [None, None]
                ny = 0
                wg_v = w_gate.rearrange("e (c p) n -> p e c n", p=128)
                wu_v = w_up.rearrange("e (c p) n -> p e c n", p=128)
                wd_v = w_down.rearrange("e (c p) n -> p e c n", p=128)
                wg_rows = w_gate.rearrange("e (p c) n -> (e p) (c n)", c=8)
                wu_rows = w_up.rearrange("e (p c) n -> (e p) (c n)", c=8)
                wd_rows = w_down.rearrange("e f n -> (e f) n")
                def emit_wload(j):
                    par = j % 2
                    tok = P.dma("gpsimd", lambda e: e.indirect_dma_start(
                        out=Wg[par].rearrange("p c n -> p (c n)"), out_offset=None, in_=wg_rows, in_offset=bass.IndirectOffsetOnAxis(ap=idxW_i[:, j, 0:1], axis=0),
                        bounds_check=breg(e, NE * 128 - 1), oob_is_err=False), f"D{21 + par}", waits=[w_free[par], tk_ej])
                    tok = P.dma("gpsimd", lambda e: e.indirect_dma_start(
                        out=Wu[par].rearrange("p c n -> p (c n)"), out_offset=None, in_=wu_rows, in_offset=bass.IndirectOffsetOnAxis(ap=idxW_i[:, j, 0:1], axis=0),
                        bounds_check=breg(e, NE * 128 - 1), oob_is_err=False), f"D{21 + par}")
                    for c in range(4):
                        tok = P.dma("gpsimd", lambda e, c=c: e.indirect_dma_start(
                            out=Wd[par][:, c, :], out_offset=None, in_=wd_rows, in_offset=bass.IndirectOffsetOnAxis(ap=idxW_i[:, j, 8 + c:9 + c], axis=0),
                            bounds_check=breg(e, NE * 512 - 1), oob_is_err=False), f"D{21 + par}")
                    return tok
                def emit_xload(j):
                    par = j % 2
                    return P.dma("sync", lambda e: e.dma_start(out=xe[par], in_=xebuf[j * BLK:(j + 1) * BLK, :].rearrange("(t p) d -> p t d", p=128)),
                                 f"D{23 + par}", waits=[xe_free[par]])
                tkw = {0: emit_wload(0)}; tkx = {0: emit_xload(0)}
                ptrx = pbank_bf(0, 2).rearrange("p (a b) -> p a b", b=256)
                for j in range(NBLK):
                    par = j % 2
                    if j + 1 < NBLK:
                        tkw[j + 1] = emit_wload(j + 1); tkx[j + 1] = emit_xload(j + 1)
                    tt = None
                    for t2 in range(2):
                        for c in range(8):
                            tt = T(lambda e, t2=t2, c=c, par=par: e.transpose(out=ptrx[:, c, t2 * 128:(t2 + 1) * 128], in_=xe[par][:, t2, c::8], identity=ident[:]),
                                   waits=[tkx[j], pxe_free] if (t2 == 0 and c == 0) else [], sig=(t2 == 1 and c == 7))
                    xe_free[par] = tt
                    pxe_free = A(lambda e: e.copy(out=xeT, in_=ptrx), waits=[tt, xeT_free])
                    for fc in range(4):
                        pp = fc % 2
                        pgu = pbank(2 + pp)
                        tg = None
                        for (Wsrc, c0) in ((Wg[par], 0), (Wu[par], 256)):
                            for c in range(8):
                                tg = T(lambda e, c=c, Wsrc=Wsrc, c0=c0, pgu=pgu, fc=fc: e.matmul(pgu[:, c0:c0 + 256], lhsT=Wsrc[:, c, fc * 128:(fc + 1) * 128], rhs=xeT[:, c, :],
                                                                                                 start=(c == 0), stop=(c == 7)),
                                       waits=[pxe_free, tkw[j], pgu_free[pp]] if (c == 0 and c0 == 0) else [], sig=(c == 7))
                        tsg = A(lambda e, pgu=pgu, pp=pp: e.activation(out=sg[pp], in_=pgu[:, 0:256], func=AF.Silu), waits=[tg, sg_free[pp]])
                        th = V(lambda e, pgu=pgu, pp=pp, fc=fc: e.tensor_tensor(out=hidT[:, fc, :], in0=sg[pp], in1=pgu[:, 256:512], op=ALU.mult), waits=[tsg, hid_free] if fc == 0 else [tsg])
                        sg_free[pp] = th; pgu_free[pp] = th
                    xeT_free = tg
                    tyl = None
                    for t2 in range(2):
                        yb = ysb[t2]
                        tcs = []
                        for hf in range(2):
                            py = pbank(4 + ny % 2)
                            ty = None
                            for fc in range(4):
                                ty = T(lambda e, fc=fc, t2=t2, hf=hf, py=py, par=par: e.matmul(py, lhsT=hidT[:, fc, t2 * 128:(t2 + 1) * 128], rhs=Wd[par][:, fc, hf * 512:(hf + 1) * 512],
                                                                                       start=(fc == 0), stop=(fc == 3)),
                                       waits=[th, py_free[ny % 2]] if fc == 0 else [], sig=(fc == 3))
                            tcp = A(lambda e, yb=yb, py=py, hf=hf: e.copy(out=yb[:, hf * 512:(hf + 1) * 512], in_=py), waits=[ty, ysb_free[t2]])
                            py_free[ny % 2] = tcp
                            tcs.append(tcp)
                            ny += 1
                            tyl = ty
                        ysb_free[t2] = P.dma("sync", lambda e, yb=yb, j=j, t2=t2: e.dma_start(out=ybuf[j * BLK + t2 * 128:j * BLK + (t2 + 1) * 128, :], in_=yb),
                                             f"D{25 + t2}", waits=tcs)
                    hid_free = tyl
                    w_free[par] = tyl
                P.barrier()
                cv.reset()
                NCB = 4
                y0 = [cv.get([128, D], F32) for _ in range(NCB)]
                y1 = [cv.get([128, D], F32) for _ in range(NCB)]
                hc = [cv.get([128, D], F32) for _ in range(NCB)]
                cb_free = [None] * NCB
                g_sem = ["D27", "D28", "D19", "D20"]
                h_sem = ["D29", "D30", "D23", "D24"]
                o_sem = ["D31", "D32", "D25", "D26"]
                for ot in range(NOT_):
                    par = ot % NCB
                    tg0 = P.dma("gpsimd", lambda e, ot=ot, par=par: e.indirect_dma_start(
                        out=y0[par], out_offset=None, in_=ybuf, in_offset=bass.IndirectOffsetOnAxis(ap=slot_i[:, ot, 0:1], axis=0),
                        bounds_check=breg(e, NSLOT - 1), oob_is_err=False), g_sem[par], waits=[cb_free[par]])
                    tg1 = P.dma("gpsimd", lambda e, ot=ot, par=par: e.indirect_dma_start(
                        out=y1[par], out_offset=None, in_=ybuf, in_offset=bass.IndirectOffsetOnAxis(ap=slot_i[:, ot, 1:2], axis=0),
                        bounds_check=breg(e, NSLOT - 1), oob_is_err=False), g_sem[par])
                    tlh = P.dma("sync", lambda e, ot=ot, par=par: e.dma_start(out=hc[par], in_=h1buf[ot * 128:(ot + 1) * 128, :]), h_sem[par], waits=[cb_free[par]])
                    V(lambda e, ot=ot, par=par: e.scalar_tensor_tensor(out=hc[par], in0=y0[par], scalar=gate_all[:, ot, 0:1], in1=hc[par], op0=ALU.mult, op1=ALU.add), waits=[tg1, tlh])
                    tv = V(lambda e, ot=ot, par=par: e.scalar_tensor_tensor(out=hc[par], in0=y1[par], scalar=gate_all[:, ot, 1:2], in1=hc[par], op0=ALU.mult, op1=ALU.add))
                    cb_free[par] = P.dma("sync", lambda e, ot=ot, par=par: e.dma_start(out=out_d[ot * 128:(ot + 1) * 128, :], in_=hc[par]), o_sem[par], waits=[tv])
        P.barrier()
        P.ops["sync"].append((None, P._w("sync", []), None, 0))

        with nc.Block() as blk:
            def mk(engname):
                def body(e):
                    waited = {}
                    for fn, waits, sig, inc in P.ops[engname]:
                        for wv in waits:
                            k, v = wv[0], wv[1]
                            if P.owner.get(k) == engname and len(wv) == 2 and engname == "tensor":
                                continue
                            if waited.get(k, 0) >= v:
                                continue
                            e.wait_ge(P.sems[k], v)
                            waited[k] = v
                        if fn is None:
                            continue
                        ins = fn(e)
                        if sig is not None:
                            ins.then_inc(P.sems[sig], inc)
                return body
            blk.sync(mk("sync"))
            blk.scalar(mk("scalar"))
            blk.vector(mk("vector"))
            blk.gpsimd(mk("gpsimd"))
            blk.tensor(mk("tensor"))
    return nc


def _rope_tables(pos):
    inv = (500000.0 ** (-np.arange(0, 16, 2, dtype=np.float32) / np.float32(16))).astype(np.float32)
    ang = pos.astype(np.float32)[:, None] * inv[None, :]
    return np.cos(ang).astype(np.float32), np.sin(ang).astype(np.float32)


def _masks(variant_large):
    m = np.zeros((8, 128, 512), np.float32)
    tri = (np.arange(128)[:, None] <= np.arange(128)[None, :]).astype(np.float32)
    for r in range(8):
        for s in range(4):
            if variant_large:
                if r < 4: v = 1.0
                elif s > r - 4: v = 1.0
                elif s == r - 4: v = tri
                else: v = 0.0
            else:
                if r >= 4: v = 0.0
                elif s > r: v = 1.0
                elif s == r: v = tri
                else: v = 0.0
            m[r, :, s * 128:(s + 1) * 128] = v
    return m


_NC_CACHE = {}


def kernel(x, meta_tokens, norm1_g, w_in, conv_w, q_norm_g, k_norm_g, lambda_q1, lambda_k1, lambda_q2, lambda_k2,
           subln_g, w_out, norm2_g, w_router_group, b_router_group, w_router_expert, b_router_expert,
           w_gate, w_up, w_down):
    x = np.asarray(x, np.float32)
    f = lambda a: np.ascontiguousarray(np.asarray(a, np.float32))
    if "nc" not in _NC_CACHE:
        _NC_CACHE["nc"] = build()
    nc = _NC_CACHE["nc"]
    meta = f(meta_tokens)
    w_r = np.ascontiguousarray(np.concatenate([f(w_router_group)[0], f(w_router_expert)[0]], axis=1))
    b_r = np.ascontiguousarray(np.concatenate([f(b_router_group)[0], f(b_router_expert)[0]], axis=0)[None, :])
    shared = {
        "norm1_g": f(norm1_g), "norm2_g": f(norm2_g), "w_in": f(w_in)[0], "w_out": f(w_out)[0],
        "conv_wT": np.ascontiguousarray(f(conv_w)[0].T), "q_norm_g": f(q_norm_g), "k_norm_g": f(k_norm_g),
        "lambda_q1": f(lambda_q1), "lambda_k1": f(lambda_k1), "lambda_q2": f(lambda_q2), "lambda_k2": f(lambda_k2),
        "subln_g": f(subln_g), "w_r": w_r, "b_r": b_r,
        "w_gate": f(w_gate)[0], "w_up": f(w_up)[0], "w_down": f(w_down)[0],
        "thr": (np.arange(NBLK, dtype=np.float32) * BLK)[None, :],
        "iota": np.arange(32, dtype=np.float32)[None, :],
        "pidx": np.ascontiguousarray(np.concatenate([np.arange(8)[None, :] * 128 + np.arange(128)[:, None],
                                                     np.arange(4)[None, :] * 128 + np.arange(128)[:, None]], 1).astype(np.float32)),
    }
    posA = np.zeros((NT_ALL, 128), np.float32)
    posA[0, :16] = np.arange(16)
    posA[1:] = 16 + np.arange(SEQ).reshape(64, 128)
    cA, sA = _rope_tables(posA.reshape(-1))
    cosA = np.ascontiguousarray(cA.reshape(NT_ALL, 128, 8).transpose(1, 0, 2).reshape(128, -1))
    sinA = np.ascontiguousarray(sA.reshape(NT_ALL, 128, 8).transpose(1, 0, 2).reshape(128, -1))
    mk_l = _masks(True); mk_s = _masks(False)
    in_maps = []
    for c in range(NCORES):
        b, hf = divmod(c, 2)
        xa = np.zeros((NT_ALL * 128, D), np.float32)
        xa[:NMETA] = meta
        xa[128:] = x[b]
        gl = GROUPS[hf]
        xo = np.concatenate([x[b, g * GT:(g + 1) * GT] for g in gl], 0)
        hal = np.zeros((NG * 128, D), np.float32)
        for i, g in enumerate(gl):
            hal[128 * i:128 * i + 2] = meta[14:16] if g == 0 else x[b, g * GT - 2:g * GT]
        posO = np.concatenate([16 + g * GT + np.arange(GT) for g in gl]).astype(np.float32)
        cO, sO = _rope_tables(posO)
        cosO = np.ascontiguousarray(cO.reshape(NOT_, 128, 8).transpose(1, 0, 2).reshape(128, -1))
        sinO = np.ascontiguousarray(sO.reshape(NOT_, 128, 8).transpose(1, 0, 2).reshape(128, -1))
        mm = np.stack([mk_s if hf == 0 else mk_l, mk_l if hf == 0 else mk_s], 0)
        mm = np.ascontiguousarray(mm.reshape(16, 128, 512).transpose(1, 0, 2).reshape(128, -1)).astype(ml_dtypes.bfloat16)
        d = dict(shared)
        d.update({"xall": xa, "xown": np.ascontiguousarray(xo), "xhalo": hal, "cosA": cosA, "sinA": sinA,
                  "cosO": cosO, "sinO": sinO, "masks": mm})
        in_maps.append(d)
    kernel.last_in_maps = in_maps
    if os.environ.get("HK_NORUN") == "1":
        return None
    res = run_bass_kernel_spmd(nc, in_maps, core_ids=list(range(NCORES)))
    kernel.last_results = res.results
    out = np.zeros((4, SEQ, D), np.float32)
    for c in range(NCORES):
        b, hf = divmod(c, 2)
        o = res.results[c]["out"]
        for i, g in enumerate(GROUPS[hf]):
            out[b, g * GT:(g + 1) * GT] = o[i * GT:(i + 1) * GT]
    return out
```
